# Optimizing a Trainium2 kernel written in Bass

```python
import math
import jax, jax.numpy as jnp
from jax import lax
import numpy as np

D_MODEL = 1024
BATCH = 8
SEQ = 4096
DEPTH = 1

MEM_LEN = 256
EPS = 1e-6
ROPE_THETA = 10000.0
A_HEADS = 8
A_HEAD_DIM = 64
A_WIDTH = A_HEADS * A_HEAD_DIM
MOBA_BLOCK = 256
MOBA_TOPK = 3
Q_BLOCK = 128
R_HEADS = 4
R_QK_DIM = 128
R_V_DIM = 256
R_QK_WIDTH = R_HEADS * R_QK_DIM
R_V_WIDTH = R_HEADS * R_V_DIM
R_CHUNK = 128
X_HEADS = 4
X_HEAD_DIM = D_MODEL // X_HEADS
D_FF = -(-8 * D_MODEL // (3 * 256)) * 256
SPLIT_SIZES = (A_WIDTH, A_WIDTH, A_WIDTH, R_QK_WIDTH, R_QK_WIDTH, R_V_WIDTH, R_V_WIDTH, D_MODEL, D_MODEL)
IN_COLS = sum(SPLIT_SIZES)

kernel_name = "moba_retention_gated_hybrid"


def rms_norm(x, g):
    xf = x.astype(jnp.float32)
    y = xf * lax.rsqrt(jnp.mean(xf * xf, axis=-1, keepdims=True) + EPS)
    return (y * g.astype(jnp.float32)).astype(x.dtype)


def rotary(x, pos):
    d = x.shape[-1]
    inv = ROPE_THETA ** (-jnp.arange(0, d, 2, dtype=jnp.float32) / d)
    ang = pos.astype(jnp.float32)[:, None] * inv[None, :]
    cos, sin = jnp.cos(ang), jnp.sin(ang)
    xf = x.astype(jnp.float32)
    x1, x2 = xf[..., : d // 2], xf[..., d // 2:]
    return jnp.concatenate([x1 * cos - x2 * sin, x2 * cos + x1 * sin], axis=-1).astype(x.dtype)


def to_heads(t, n_heads, head_dim):
    b, s, _ = t.shape
    return t.reshape(b, s, n_heads, head_dim).transpose(0, 2, 1, 3)


def from_heads(t):
    b, h, s, d = t.shape
    return t.transpose(0, 2, 1, 3).reshape(b, s, h * d)


def moba_attention(q, k, v):
    B, H, S, dh = q.shape
    nb = -(-S // MOBA_BLOCK)
    pad = nb * MOBA_BLOCK - S
    kp = jnp.pad(k, ((0, 0), (0, 0), (0, pad), (0, 0)))
    vp = jnp.pad(v, ((0, 0), (0, 0), (0, pad), (0, 0)))
    kb = kp.reshape(B, H, nb, MOBA_BLOCK, dh)
    vb = vp.reshape(B, H, nb, MOBA_BLOCK, dh)
    n_sel = min(MOBA_TOPK, nb - 1)
    scale = dh ** -0.5
    nqc = S // Q_BLOCK
    pos = jnp.arange(S)
    q_blk = pos // MOBA_BLOCK

    qc = q.reshape(B, H, nqc, Q_BLOCK, dh).transpose(0, 2, 1, 3, 4).reshape(B * nqc, H, Q_BLOCK, dh)
    if n_sel > 0:
        kmean = jnp.mean(kb, axis=3, dtype=jnp.float32)
        bscore = jnp.einsum('bhsd,bhnd->bhsn', q.astype(jnp.float32), kmean)
        past = jnp.arange(nb)[None, :] < q_blk[:, None]
        bscore = jnp.where(past[None, None], bscore, -jnp.inf)
        _, sel = lax.top_k(bscore, n_sel)
        selc = sel.reshape(B, H, nqc, Q_BLOCK, n_sel).transpose(0, 2, 1, 3, 4).reshape(B * nqc, H, Q_BLOCK, n_sel)
    else:
        selc = jnp.zeros((B * nqc, H, Q_BLOCK, 0), jnp.int32)
    hidx = jnp.arange(H)[:, None, None]

    def step(args):
        sid, qb, selb = args
        bi = sid // nqc
        ci = sid % nqc
        kb_b = kb[bi]
        vb_b = vb[bi]
        qpos = ci * Q_BLOCK + jnp.arange(Q_BLOCK)
        own = (ci * Q_BLOCK) // MOBA_BLOCK
        k_own = lax.dynamic_index_in_dim(kb_b, own, axis=1, keepdims=False)
        v_own = lax.dynamic_index_in_dim(vb_b, own, axis=1, keepdims=False)
        kpos_own = own * MOBA_BLOCK + jnp.arange(MOBA_BLOCK)
        s_own = jnp.einsum('hqd,hkd->hqk', qb, k_own, preferred_element_type=jnp.float32) * scale
        s_own = jnp.where((kpos_own[None, :] <= qpos[:, None])[None], s_own, -jnp.inf)
        if n_sel > 0:
            k_sel = kb_b[hidx, selb]
            v_sel = vb_b[hidx, selb]
            s_sel = jnp.einsum('hqd,hqrkd->hqrk', qb, k_sel, preferred_element_type=jnp.float32) * scale
            valid = jnp.arange(n_sel)[None, :] < (qpos // MOBA_BLOCK)[:, None]
            s_sel = jnp.where(valid[None, :, :, None], s_sel, -jnp.inf)
            s = jnp.concatenate([s_sel.reshape(H, Q_BLOCK, n_sel * MOBA_BLOCK), s_own], axis=-1)
            p = jax.nn.softmax(s, axis=-1).astype(v.dtype)
            p_sel = p[..., : n_sel * MOBA_BLOCK].reshape(H, Q_BLOCK, n_sel, MOBA_BLOCK)
            p_own = p[..., n_sel * MOBA_BLOCK:]
            o = (jnp.einsum('hqrk,hqrkd->hqd', p_sel, v_sel)
                 + jnp.einsum('hqk,hkd->hqd', p_own, v_own))
        else:
            p_own = jax.nn.softmax(s_own, axis=-1).astype(v.dtype)
            o = jnp.einsum('hqk,hkd->hqd', p_own, v_own)
        return o.astype(q.dtype)

    out = lax.map(step, (jnp.arange(B * nqc), qc, selc))
    return out.reshape(B, nqc, H, Q_BLOCK, dh).transpose(0, 2, 1, 3, 4).reshape(B, H, S, dh)


def retention(q, k, v):
    B, H, S, dk = q.shape
    dv = v.shape[-1]
    C = R_CHUNK
    n = S // C
    q = q.astype(jnp.float32)
    k = k.astype(jnp.float32)
    v = v.astype(jnp.float32)
    log_g = jnp.log(1.0 - jnp.exp2(-5.0 - jnp.arange(H, dtype=jnp.float32)))
    i = jnp.arange(C, dtype=jnp.float32)
    diff = i[:, None] - i[None, :]
    dmask = jnp.where(diff >= 0, jnp.exp(log_g[:, None, None] * jnp.maximum(diff, 0.0)), 0.0)
    q_dec = jnp.exp(log_g[:, None] * (i + 1.0))
    k_dec = jnp.exp(log_g[:, None] * (C - 1.0 - i))
    chunk_dec = jnp.exp(log_g * C)

    qc = q.reshape(B, H, n, C, dk)
    kc = k.reshape(B, H, n, C, dk)
    vc = v.reshape(B, H, n, C, dv)
    att = jnp.einsum('bhnid,bhnjd->bhnij', qc, kc) * dmask[None, :, None]
    inner = jnp.einsum('bhnij,bhnje->bhnie', att, vc)
    kv = jnp.einsum('bhnjd,bhnje->bhnde', kc * k_dec[None, :, None, :, None], vc)

    def scan_fn(state, kv_n):
        return chunk_dec[None, :, None, None] * state + kv_n, state

    _, prev = lax.scan(scan_fn, jnp.zeros((B, H, dk, dv), jnp.float32), kv.transpose(2, 0, 1, 3, 4))
    prev = prev.transpose(1, 2, 0, 3, 4)
    cross = jnp.einsum('bhnid,bhnde->bhnie', qc * q_dec[None, :, None, :, None], prev)
    return (inner + cross).reshape(B, H, S, dv)


def head_group_norm(y):
    mu = jnp.mean(y, axis=-1, keepdims=True)
    var = jnp.mean(jnp.square(y - mu), axis=-1, keepdims=True)
    return (y - mu) * lax.rsqrt(var + EPS)


def cross_attention(h, m, w_q, w_kv, w_o):
    q = to_heads(h @ w_q, X_HEADS, X_HEAD_DIM)
    kv = m @ w_kv
    k = to_heads(kv[..., :D_MODEL], X_HEADS, X_HEAD_DIM)
    v = to_heads(kv[..., D_MODEL:], X_HEADS, X_HEAD_DIM)
    s = jnp.einsum('bhsd,bhmd->bhsm', q, k, preferred_element_type=jnp.float32) * (X_HEAD_DIM ** -0.5)
    p = jax.nn.softmax(s, axis=-1).astype(v.dtype)
    o = jnp.einsum('bhsm,bhmd->bhsd', p, v)
    return from_heads(o) @ w_o


def setup_inputs(seed: int = 0) -> dict:
    key = jax.random.key(seed)
    ks = jax.random.split(key, 20)

    def w(k, shape, fan_in):
        return jax.random.normal(k, shape, jnp.float32) * (fan_in ** -0.5)

    def gain(k, shape):
        return 1.0 + 0.02 * jax.random.normal(k, shape, jnp.float32)

    return {
        "x": jax.random.normal(ks[0], (BATCH, SEQ, D_MODEL), jnp.float32),
        "mem": jax.random.normal(ks[1], (BATCH, MEM_LEN, D_MODEL), jnp.float32),
        "norm_mix": gain(ks[2], (DEPTH, D_MODEL)),
        "w_in": w(ks[3], (DEPTH, D_MODEL, IN_COLS), D_MODEL),
        "w_branch_a": w(ks[4], (DEPTH, A_WIDTH, D_MODEL), A_WIDTH),
        "w_branch_b": w(ks[5], (DEPTH, R_V_WIDTH, D_MODEL), R_V_WIDTH),
        "w_out": w(ks[6], (DEPTH, D_MODEL, D_MODEL), D_MODEL),
        "norm_cross": gain(ks[7], (DEPTH, D_MODEL)),
        "norm_mem": gain(ks[8], (DEPTH, D_MODEL)),
        "w_xq": w(ks[9], (DEPTH, D_MODEL, D_MODEL), D_MODEL),
        "w_xkv": w(ks[10], (DEPTH, D_MODEL, 2 * D_MODEL), D_MODEL),
        "w_xo": w(ks[11], (DEPTH, D_MODEL, D_MODEL), D_MODEL),
        "norm_ffn": gain(ks[12], (DEPTH, D_MODEL)),
        "w_gate": w(ks[13], (DEPTH, D_MODEL, D_FF), D_MODEL),
        "w_up": w(ks[14], (DEPTH, D_MODEL, D_FF), D_MODEL),
        "w_down": w(ks[15], (DEPTH, D_FF, D_MODEL), D_FF),
        "norm_final": gain(ks[16], (D_MODEL,)),
    }


def reference(x, mem, norm_mix, w_in, w_branch_a, w_branch_b, w_out, norm_cross, norm_mem,
              w_xq, w_xkv, w_xo, norm_ffn, w_gate, w_up, w_down, norm_final):
    B, S, _ = x.shape
    pos = jnp.arange(S)
    split_points = [int(p) for p in np.cumsum(SPLIT_SIZES)[:-1]]
    for l in range(DEPTH):
        h = rms_norm(x, norm_mix[l])
        proj = h @ w_in[l]
        aq, ak, av, rq, rk, rv, rg, ga, gb = jnp.split(proj, split_points, axis=-1)

        aq = rotary(to_heads(aq, A_HEADS, A_HEAD_DIM), pos)
        ak = rotary(to_heads(ak, A_HEADS, A_HEAD_DIM), pos)
        av = to_heads(av, A_HEADS, A_HEAD_DIM)
        y_a = from_heads(moba_attention(aq, ak, av))

        rq = rotary(to_heads(rq, R_HEADS, R_QK_DIM), pos)
        rk = rotary(to_heads(rk, R_HEADS, R_QK_DIM), pos) * (R_QK_DIM ** -0.5)
        rv = to_heads(rv, R_HEADS, R_V_DIM)
        y_r = head_group_norm(retention(rq, rk, rv))
        y_b = from_heads(y_r).astype(x.dtype) * jax.nn.silu(rg)

        merged = (jax.nn.sigmoid(ga) * (y_a @ w_branch_a[l])
                  + jax.nn.sigmoid(gb) * (y_b @ w_branch_b[l]))
        x = x + merged @ w_out[l]

        x = x + cross_attention(rms_norm(x, norm_cross[l]), rms_norm(mem, norm_mem[l]),
                                w_xq[l], w_xkv[l], w_xo[l])

        h = rms_norm(x, norm_ffn[l])
        x = x + (jax.nn.silu(h @ w_gate[l]) * (h @ w_up[l])) @ w_down[l]
    return rms_norm(x, norm_final)
```

```python
import contextlib
import numpy as np
import ml_dtypes
import concourse.bass as bass
import concourse.mybir as mybir
from concourse.bass_utils import run_bass_kernel_spmd

F32 = mybir.dt.float32
BF16 = mybir.dt.bfloat16
AF = mybir.ActivationFunctionType
ALU = mybir.AluOpType
AX = mybir.AxisListType

S = 4096
D = 1024
T = 512
NT = S // T
NST = T // 128
DFF = 2816
NFC = DFF // 128
BIG = 30000.0
EPS = 1e-6
ALLQ = ("sp", "pe", "act", "dve", "pool")

C_AQ, C_AK, C_AV, C_RQ, C_RK, C_RV, C_RG, C_GA, C_GB = 0, 512, 1024, 1536, 2048, 2560, 3584, 4608, 5632


class Op:
    __slots__ = ("eng", "fn", "deps", "sem", "val", "signal", "is_dma", "idx")


class Prog:
    def __init__(self, nc):
        self.nc = nc
        self.q = {e: [] for e in ALLQ}
        self.lastw = {}
        self.readers = {}
        self.nops = 0
        self.last_on = {}

    limit = None
    skip = None

    def add(self, eng, fn, reads=(), writes=(), dma_sem=None):
        if Prog.limit is not None and self.nops >= Prog.limit:
            return None
        if Prog.skip and self.nops in Prog.skip:
            self.nops += 1
            return None
        is_dma = dma_sem is not None
        op = Op()
        op.eng, op.fn, op.is_dma = eng, fn, is_dma
        op.sem = dma_sem if is_dma else eng
        op.val = None
        op.signal = is_dma
        op.deps = []
        op.idx = self.nops
        self.nops += 1
        deps = []
        for k in reads:
            w = self.lastw.get(k)
            if w is not None:
                deps.append(w)
        for k in writes:
            w = self.lastw.get(k)
            if w is not None:
                deps.append(w)
            deps.extend(self.readers.get(k, ()))
        seen = set()
        for d in deps:
            if d is op or id(d) in seen:
                continue
            seen.add(id(d))
            if (not d.is_dma) and (not is_dma) and d.eng == "pe" and eng == "pe":
                continue
            op.deps.append(d)
            d.signal = True
        for k in writes:
            self.lastw[k] = op
            self.readers[k] = []
        for k in reads:
            self.readers.setdefault(k, []).append(op)
        self.q[eng].append(op)
        self.last_on[op.sem] = op
        return op

    def barrier(self):
        lasts = list(self.last_on.values())
        for o in lasts:
            o.signal = True
        for e in ALLQ:
            op = Op()
            op.eng, op.fn, op.is_dma = e, None, False
            op.sem, op.val, op.signal = e, None, False
            op.deps = list(lasts)
            op.idx = self.nops
            self.nops += 1
            self.q[e].append(op)
        self.lastw = {}
        self.readers = {}

    def emit(self):
        nc = self.nc
        cnt = {}
        allops = []
        for e in ALLQ:
            allops.extend(self.q[e])
        allops.sort(key=lambda o: o.idx)
        for op in allops:
            if op.signal and op.fn is not None:
                inc = 16 if op.is_dma else 1
                cnt[op.sem] = cnt.get(op.sem, 0) + inc
                op.val = cnt[op.sem]
        semnames = sorted(cnt.keys())
        sems = {}
        with contextlib.ExitStack() as st:
            for s in semnames:
                sems[s] = st.enter_context(nc.semaphore("s_" + s))
            block = st.enter_context(nc.Block())

            def run(engname, eng):
                waited = {}
                for op in self.q[engname]:
                    need = {}
                    for d in op.deps:
                        if d.val is None:
                            continue
                        if d.val > need.get(d.sem, 0):
                            need[d.sem] = d.val
                    for s, v in need.items():
                        if waited.get(s, 0) >= v:
                            continue
                        eng.wait_ge(sems[s], v)
                        waited[s] = v
                    if op.fn is None:
                        continue
                    ins = op.fn(eng)
                    if op.signal:
                        ins.then_inc(sems[op.sem], 16 if op.is_dma else 1)
                if engname == "sp":
                    for s in semnames:
                        if waited.get(s, 0) < cnt[s]:
                            eng.wait_ge(sems[s], cnt[s])

            block.sync(lambda e: run("sp", e))
            block.tensor(lambda e: run("pe", e))
            block.scalar(lambda e: run("act", e))
            block.vector(lambda e: run("dve", e))
            block.gpsimd(lambda e: run("pool", e))


class Arena:
    def __init__(self, nc, st, nbytes):
        self.nbytes = nbytes
        self.t16 = st.enter_context(nc.sbuf_tensor("arena", [128, nbytes // 2], BF16))
        self.t32 = self.t16.bitcast(F32)
        self.off = 0

    def alloc(self, shape, dt):
        n = int(np.prod(shape))
        nb = n * (4 if dt == F32 else 2)
        o = self.off
        self.off += (nb + 63) // 64 * 64
        assert self.off <= self.nbytes, ("SBUF arena overflow", self.off, self.nbytes)
        v = self.t32[:, o // 4:o // 4 + n] if dt == F32 else self.t16[:, o // 2:o // 2 + n]
        if len(shape) == 2:
            v = v.rearrange("p (a b) -> p a b", b=shape[1])
        elif len(shape) == 3:
            v = v.rearrange("p (a b c) -> p a b c", b=shape[1], c=shape[2])
        return v


def make_consts():
    bf = ml_dtypes.bfloat16
    c = {}
    c["c_ident"] = np.eye(128, dtype=np.float32).astype(bf)
    pa = np.zeros((128, 128), np.float32)
    pb = np.zeros((128, 128), np.float32)
    for m in range(128):
        pa[(m + 32) if (m % 64) < 32 else (m - 32), m] = 1.0
        pb[(m + 64) if m < 64 else (m - 64), m] = 1.0
    c["c_permA"] = pa.astype(bf)
    c["c_permB"] = pb.astype(bf)
    pos = np.arange(S, dtype=np.float32)
    invA = (10000.0 ** (-np.arange(0, 64, 2, dtype=np.float32) / 64)).astype(np.float32)
    invB = (10000.0 ** (-np.arange(0, 128, 2, dtype=np.float32) / 128)).astype(np.float32)
    angA = (pos[None, :] * invA[:, None]).astype(np.float32)
    angB = (pos[None, :] * invB[:, None]).astype(np.float32)
    p = np.arange(128)
    c["c_cosA"] = np.cos(angA)[p % 32].astype(np.float32)
    c["c_sinA"] = (np.sin(angA)[p % 32] * np.where((p % 64) < 32, -1.0, 1.0)[:, None]).astype(np.float32)
    c["c_cosB"] = np.cos(angB)[p % 64].astype(np.float32)
    c["c_sinB"] = (np.sin(angB)[p % 64] * np.where(p < 64, -1.0, 1.0)[:, None]).astype(np.float32)
    c["c_tri"] = (p[:, None] <= p[None, :]).astype(np.float32).astype(bf)
    gam = 1.0 - 2.0 ** (-5.0 - np.arange(4, dtype=np.float64))
    sc = 128.0 ** -0.5
    j = np.arange(128, dtype=np.float64)
    m2 = np.zeros((128, 4, 128), np.float64)
    for h in range(4):
        m2[:, h, :] = (sc * gam[h] ** (-(j + 1.0)))[:, None] * (j[:, None] <= j[None, :])
    c["c_M2"] = m2.astype(np.float32)
    c["c_kdec"] = (sc * gam[None, :] ** (127.0 - j[:, None])).astype(np.float32)
    c["c_epsq"] = (EPS / gam[None, :] ** (2.0 * (j[:, None] + 1.0))).astype(np.float32)
    n = np.arange(16)
    sel = np.zeros((3, 16, 16), np.float32)
    sel[0] = np.where(n[None, :] >= n[:, None], -BIG, 0.0)
    sel[1] = (n[None, :] < n[:, None]).astype(np.float32)
    sel[2] = (n[None, :] == n[:, None]).astype(np.float32)
    c["c_sel"] = sel
    c["c_onehot"] = (np.arange(S)[None, :] // 256 == n[:, None]).astype(np.float32).astype(bf)
    return c, gam


CONST_SPECS = [("c_ident", [128, 128], BF16), ("c_permA", [128, 128], BF16), ("c_permB", [128, 128], BF16),
               ("c_cosA", [128, S], F32), ("c_sinA", [128, S], F32), ("c_cosB", [128, S], F32),
               ("c_sinB", [128, S], F32), ("c_tri", [128, 128], BF16), ("c_M2", [128, 4, 128], F32),
               ("c_kdec", [128, 4], F32), ("c_epsq", [128, 4], F32), ("c_sel", [3, 16, 16], F32),
               ("c_onehot", [16, S], BF16)]

W_SPECS = [("norm_mix", [D]), ("w_in", [D, 6656]), ("w_branch_a", [512, D]), ("w_branch_b", [D, D]),
           ("w_out", [D, D]), ("norm_cross", [D]), ("norm_mem", [D]), ("w_xq", [D, D]), ("w_xkv", [D, 2 * D]),
           ("w_xo", [D, D]), ("norm_ffn", [D]), ("w_gate", [D, DFF]), ("w_up", [D, DFF]), ("w_down", [DFF, D]),
           ("norm_final", [D])]


def build(nt0=NT, nt1=NT, nt2=NT, dbg=False):
    _, gam = make_consts()
    gamC = [float(g ** 128.0) for g in gam]
    nc = bass.Bass("TRN2", target_bir_lowering=False)
    dr = {}
    dr["x"] = nc.dram_tensor("x", [S, D], F32, kind="ExternalInput").ap()
    dr["mem"] = nc.dram_tensor("mem", [256, D], F32, kind="ExternalInput").ap()
    for name, shp in W_SPECS:
        dr[name] = nc.dram_tensor(name, shp, F32, kind="ExternalInput").ap()
    for name, shp, dt in CONST_SPECS:
        dr[name] = nc.dram_tensor(name, shp, dt, kind="ExternalInput").ap()
    out_d = nc.dram_tensor("out", [S, D], F32, kind="ExternalOutput").ap()
    skind = "ExternalOutput" if dbg else "Internal"
    ya_s = nc.dram_tensor("ya_s", [512, S], BF16, kind=skind).ap()
    x1_s = nc.dram_tensor("x1_s", [S, D], F32, kind=skind).ap()

    with contextlib.ExitStack() as st:
        st.enter_context(nc.allow_low_precision(reason="bf16 matmul operands by design (fp32 accumulate)"))
        st.enter_context(nc.allow_non_contiguous_dma(reason="tiny norm-gain transposes / strided weight blocks"))
        AR = Arena(nc, st, 206 * 1024)
        ps_t = [st.enter_context(nc.psum_tensor("ps%d" % i, [128, 512], F32)) for i in range(8)]
        ps = [p_[:, :] for p_ in ps_t]
        ps16 = [p_.bitcast(BF16)[:, :] for p_ in ps_t]
        P = Prog(nc)
        uid = [0]

        def dma(q, out, in_, reads, writes, sem):
            P.add(q, lambda e: e.dma_start(out=out, in_=in_), reads=reads, writes=writes, dma_sem=sem)

        def bk(b):
            return ("ps", b)

        ident = AR.alloc([128], BF16)
        permA = AR.alloc([128], BF16)
        permB = AR.alloc([128], BF16)
        tri = AR.alloc([128], BF16)
        onesb = AR.alloc([128], BF16)
        ones32 = AR.alloc([64], F32)
        M2 = AR.alloc([4, 128], F32)
        kdec = AR.alloc([4], F32)
        epsq = AR.alloc([4], F32)
        seltab = AR.alloc([3, 16, 16], F32)
        eps1 = AR.alloc([1], F32)
        gT = {k: AR.alloc([8], F32) for k in ("norm_mix", "norm_cross", "norm_mem", "norm_ffn")}
        gfin = AR.alloc([D], F32)
        kxT = AR.alloc([8, 256], BF16)
        vx = AR.alloc([2, D], BF16)
        ss = [AR.alloc([1], F32) for _ in range(2)]
        sd = [AR.alloc([1], F32) for _ in range(2)]
        rs = [AR.alloc([1], F32) for _ in range(2)]
        for nm, buf in (("c_ident", ident), ("c_permA", permA), ("c_permB", permB), ("c_tri", tri),
                        ("c_M2", M2), ("c_kdec", kdec), ("c_epsq", epsq)):
            dma("sp", buf, dr[nm], [], ["const"], "const")
        dma("sp", seltab.rearrange("p a b c -> p (a b c)"),
            dr["c_sel"].rearrange("a b c -> (a b c)").partition_broadcast(128), [], ["const"], "const")
        for k in gT:
            dma("sp", gT[k], dr[k].rearrange("(c p) -> p c", p=128), [], ["const"], "const")
        dma("sp", gfin, dr["norm_final"].partition_broadcast(128), [], ["const"], "const")
        P.add("pool", lambda e: e.memset(onesb, 1.0), writes=["const"])
        P.add("pool", lambda e: e.memset(ones32, 1.0), writes=["const"])
        P.add("pool", lambda e: e.memset(eps1, EPS), writes=["const"])
        persist_mark = AR.off

        class Ctx:
            pass

        cx = Ctx()

        def setup_common(nw, xt_full):
            cx.wslots = [AR.alloc([8, 512], BF16) for _ in range(nw)]
            cx.wi = 0
            cx.nw = nw
            cx.xb = [AR.alloc([D], BF16) for _ in range(2)]
            cx.junk = AR.alloc([D], BF16)
            cx.hT = AR.alloc([8, T], BF16)
            cx.xt = AR.alloc([NST, D], F32) if xt_full else None
            cx.nrm = 0

        def load_w(src, kcs, n):
            s = cx.wi % cx.nw
            cx.wi += 1
            dst = cx.wslots[s][:, 0:kcs, 0:n]
            dma("pool", dst, src.rearrange("(kc p) n -> p kc n", p=128), [], [("w", s)], "w%d" % s)
            return dst, ("w", s)

        def norm_T(xsrc, xkey, gkey, st_i, bank, hkeys):
            i = cx.nrm % 2
            cx.nrm += 1
            xb = cx.xb[i]
            junk = cx.junk
            hT = cx.hT
            P.add("act", lambda e: e.activation(out=junk, in_=xsrc, func=AF.Square, accum_out=ss[i]),
                  reads=[xkey], writes=["junk", ("ss", i)])
            P.add("act", lambda e: e.activation(out=sd[i], in_=ss[i], func=AF.Sqrt, scale=1.0 / D, bias=eps1),
                  reads=[("ss", i), "const"], writes=[("sd", i)])
            P.add("dve", lambda e: e.reciprocal(out=rs[i], in_=sd[i]), reads=[("sd", i)], writes=[("rs", i)])
            P.add("dve", lambda e: e.tensor_scalar(out=xb, in0=xsrc, scalar1=rs[i], scalar2=None, op0=ALU.mult),
                  reads=[xkey, ("rs", i)], writes=[("xb", i)])
            pT = ps16[bank]
            for kc in range(8):
                P.add("pe", lambda e, kc=kc: e.transpose(out=pT[:, kc * 128:(kc + 1) * 128],
                                                         in_=xb[:, kc * 128:(kc + 1) * 128], identity=ident),
                      reads=[("xb", i), "const"], writes=[bk(bank)])
            g = gT[gkey]
            P.add("dve", lambda e: e.tensor_tensor(out=hT[:, :, st_i * 128:(st_i + 1) * 128],
                                                   in0=pT.rearrange("p (a b) -> p a b", b=128),
                                                   in1=g.unsqueeze(2).broadcast_to([128, 8, 128]), op=ALU.mult),
                  reads=[bk(bank), "const"], writes=hkeys)

        def fm_chunk(bank, wv, wkey, c, rhs_of_kc, nk, rkeys, ncols=T):
            for kc in range(nk):
                r_ = rhs_of_kc(kc)
                P.add("pe", lambda e, kc=kc, r_=r_: e.matmul(ps[bank][:, 0:ncols], lhsT=wv[:, kc, c * 128:(c + 1) * 128],
                                                             rhs=r_, start=(kc == 0), stop=(kc == nk - 1)),
                      reads=[wkey] + rkeys, writes=[bk(bank)])

        def tm_group(bank, wv, wkey, lhs_of_kc, nk, lkeys, ncols=512):
            for kc in range(nk):
                l_ = lhs_of_kc(kc)
                P.add("pe", lambda e, kc=kc, l_=l_: e.matmul(ps[bank][:, 0:ncols], lhsT=l_, rhs=wv[:, kc, 0:ncols],
                                                             start=(kc == 0), stop=(kc == nk - 1)),
                      reads=[wkey] + lkeys, writes=[bk(bank)])

        class RR:
            def __init__(self, ids):
                self.ids = list(ids)
                self.i = 0

            def next(self):
                b = self.ids[self.i % len(self.ids)]
                self.i += 1
                return b

        HK = [("hT", i) for i in range(NST)]

        def rotary_chunk(G, wv, wkey, c, perm, cosT, sinT, tabkey, Qb, qbkey, t1, t2, tkey, out_fn):
            b1 = G.next()
            fm_chunk(b1, wv, wkey, c, lambda kc: cx.hT[:, kc, :], 8, HK)
            P.add("act", lambda e: e.activation(out=Qb, in_=ps[b1], func=AF.Copy), reads=[bk(b1)], writes=[qbkey])
            P.add("dve", lambda e: e.tensor_tensor(out=t1, in0=ps[b1], in1=cosT, op=ALU.mult),
                  reads=[bk(b1), tabkey, qbkey], writes=[(tkey, 1)])
            b2 = G.next()
            P.add("pe", lambda e: e.matmul(ps[b2], lhsT=perm, rhs=Qb, start=True, stop=True),
                  reads=[qbkey, "const"], writes=[bk(b2)])
            P.add("dve", lambda e: e.tensor_tensor(out=t2, in0=ps[b2], in1=sinT, op=ALU.mult),
                  reads=[bk(b2), tabkey], writes=[(tkey, 2)])
            out_fn()

        mark0 = AR.off
        setup_common(3, False)
        mt = AR.alloc([2, D], F32)
        mT = AR.alloc([8, 256], BF16)
        dma("sp", mt, dr["mem"].rearrange("(a p) d -> p a d", p=128), [], ["mt"], "mt")
        G = RR([0, 1, 2, 3])
        for a in range(2):
            i = cx.nrm % 2
            cx.nrm += 1
            xb = cx.xb[i]
            src = mt[:, a, :]
            P.add("act", lambda e, src=src, i=i, junk=cx.junk: e.activation(out=junk, in_=src, func=AF.Square, accum_out=ss[i]),
                  reads=["mt"], writes=["junk", ("ss", i)])
            P.add("act", lambda e, i=i: e.activation(out=sd[i], in_=ss[i], func=AF.Sqrt, scale=1.0 / D, bias=eps1),
                  reads=[("ss", i), "const"], writes=[("sd", i)])
            P.add("dve", lambda e, i=i: e.reciprocal(out=rs[i], in_=sd[i]), reads=[("sd", i)], writes=[("rs", i)])
            P.add("dve", lambda e, src=src, i=i, xb=xb: e.tensor_scalar(out=xb, in0=src, scalar1=rs[i], scalar2=None,
                                                                      op0=ALU.mult),
                  reads=["mt", ("rs", i)], writes=[("xb", i)])
            b = G.next()
            pT = ps16[b]
            for kc in range(8):
                P.add("pe", lambda e, kc=kc, pT=pT, xb=xb: e.transpose(out=pT[:, kc * 128:(kc + 1) * 128],
                                                                    in_=xb[:, kc * 128:(kc + 1) * 128], identity=ident),
                      reads=[("xb", i), "const"], writes=[bk(b)])
            P.add("dve", lambda e, pT=pT, a=a: e.tensor_tensor(out=mT[:, :, a * 128:(a + 1) * 128],
                                                             in0=pT.rearrange("p (a b) -> p a b", b=128),
                                                             in1=gT["norm_mem"].unsqueeze(2).broadcast_to([128, 8, 128]),
                                                             op=ALU.mult),
                  reads=[bk(b), "const"], writes=["mT"])
        for g in range(2):
            wv, wk = load_w(dr["w_xkv"][:, g * 512:(g + 1) * 512], 8, 512)
            for c in range(4):
                b = G.next()
                fm_chunk(b, wv, wk, c, lambda kc: mT[:, kc, :], 8, ["mT"], ncols=256)
                P.add("act", lambda e, b=b, cc=g * 4 + c: e.activation(out=kxT[:, cc, :], in_=ps[b][:, 0:256], func=AF.Copy),
                      reads=[bk(b)], writes=["kxT"])
        for g in range(2):
            wv, wk = load_w(dr["w_xkv"][:, D + g * 512:D + (g + 1) * 512], 8, 512)
            for a in range(2):
                b = G.next()
                tm_group(b, wv, wk, lambda kc, a=a: mT[:, kc, a * 128:(a + 1) * 128], 8, ["mT"])
                P.add("act", lambda e, b=b, a=a, g=g: e.activation(out=vx[:, a, g * 512:(g + 1) * 512], in_=ps[b], func=AF.Copy),
                      reads=[bk(b)], writes=["vx"])
        P.barrier()
        AR.off = mark0

        def sweep0():
            setup_common(3, False)
            xs = [AR.alloc([D], F32) for _ in range(2)]
            Kc = AR.alloc([8, S], BF16)
            Vc = AR.alloc([32, 8, 65], BF16)
            Qa = AR.alloc([8, T], BF16)
            kmT = AR.alloc([8, 16], BF16)
            kmf = AR.alloc([8, 2], F32)
            cosA = AR.alloc([T], F32)
            sinA = AR.alloc([T], F32)
            Qb = [AR.alloc([T], BF16) for _ in range(2)]
            t1 = AR.alloc([T], F32)
            t2 = AR.alloc([T], F32)
            bsb = AR.alloc([8, 16], F32)
            top8 = AR.alloc([8, 8], F32)
            selb = AR.alloc([8, 16], F32)
            mb = AR.alloc([NST, 8, 16], BF16)
            PT = [AR.alloc([T], BF16) for _ in range(4)]
            rc = AR.alloc([T], F32)
            bcs = AR.alloc([T], F32)
            yaT = [AR.alloc([4, T], BF16) for _ in range(2)]
            for h in range(8):
                dma("sp", Kc[64:80, h, :], dr["c_onehot"], [], ["Kaux"], "const")
            P.add("pool", lambda e: e.memset(Vc[:, :, :, 64:65], 1.0), writes=["Vones"])
            P.add("pool", lambda e: e.memset(kmT[0:64], 0.0), writes=["kmT"])
            G = RR([0, 1, 2, 3])
            OB = RR([4, 5])
            pti = [0]
            for j in range(nt0):
                tok0 = j * T
                dma("sp", cosA, dr["c_cosA"][:, tok0:tok0 + T], [], ["tabA"], "tabA")
                dma("sp", sinA, dr["c_sinA"][:, tok0:tok0 + T], [], ["tabA"], "tabA")
                for s_ in range(NST):
                    xi = (j * NST + s_) % 2
                    dma("sp", xs[xi], dr["x"][tok0 + s_ * 128:tok0 + (s_ + 1) * 128, :], [], [("xs", xi)], "xs%d" % xi)
                    norm_T(xs[xi], ("xs", xi), "norm_mix", s_, G.next(), [HK[s_]])
                for which, col0 in (("q", C_AQ), ("k", C_AK)):
                    wv, wk = load_w(dr["w_in"][:, col0:col0 + 512], 8, 512)
                    for c in range(4):
                        qi = uid[0] % 2
                        uid[0] += 1

                        def comb(c=c, which=which):
                            for half in range(2):
                                h = 2 * c + half
                                if which == "q":
                                    dst = Qa[0:64, h, :]
                                    wr = [("Qa", h)]
                                else:
                                    dst = Kc[0:64, h, tok0:tok0 + T]
                                    wr = [("Kc", j, h)]
                                P.add("dve", lambda e, dst=dst, half=half: e.tensor_tensor(
                                    out=dst, in0=t1[half * 64:(half + 1) * 64, :], in1=t2[half * 64:(half + 1) * 64, :],
                                    op=ALU.add), reads=[("tA", 1), ("tA", 2)], writes=wr)

                        rotary_chunk(G, wv, wk, c, permA, cosA, sinA, "tabA", Qb[qi], ("Qb", qi), t1, t2, "tA", comb)
                wv, wk = load_w(dr["w_in"][:, C_AV:C_AV + 512], 8, 512)
                for s_ in range(NST):
                    b = G.next()
                    tm_group(b, wv, wk, lambda kc, s_=s_: cx.hT[:, kc, s_ * 128:(s_ + 1) * 128], 8, [HK[s_]])
                    kt = j * NST + s_
                    P.add("act", lambda e, b=b, kt=kt: e.activation(out=Vc[:, kt, :, 0:64],
                                                                    in_=ps[b].rearrange("p (h d) -> p h d", d=64),
                                                                    func=AF.Copy),
                          reads=[bk(b), "Vones"], writes=[("Vc", kt)])
                P.add("dve", lambda e, j=j: e.tensor_reduce(
                    out=kmf[0:64], in_=Kc[0:64, :, j * T:(j + 1) * T].rearrange("p h (n k) -> p h n k", k=256),
                    axis=AX.X, op=ALU.add), reads=[("Kc", j, h) for h in range(8)], writes=["kmf"])
                P.add("dve", lambda e, j=j: e.tensor_scalar(out=kmT[0:64, :, 2 * j:2 * j + 2], in0=kmf[0:64],
                                                            scalar1=1.0 / 256, scalar2=None, op0=ALU.mult),
                      reads=["kmf"], writes=["kmT"])
                BSB = 7
                for s_ in range(NST):
                    for h in range(8):
                        P.add("pe", lambda e, s_=s_, h=h: e.matmul(
                            ps[BSB][:, (s_ * 8 + h) * 16:(s_ * 8 + h + 1) * 16],
                            lhsT=Qa[0:64, h, s_ * 128:(s_ + 1) * 128], rhs=kmT[0:64, h, :], start=True, stop=True),
                            reads=[("Qa", h), "kmT"], writes=[bk(BSB)])
                for s_ in range(NST):
                    blk = (j * NST + s_) // 2
                    P.add("dve", lambda e, s_=s_, blk=blk: e.tensor_tensor(
                        out=bsb, in0=ps[BSB][:, s_ * 128:(s_ + 1) * 128].rearrange("p (h n) -> p h n", n=16),
                        in1=seltab[:, 0, blk, :].unsqueeze(1).broadcast_to([128, 8, 16]), op=ALU.add),
                        reads=[bk(BSB), "const"], writes=["bsb"])
                    for h in range(8):
                        P.add("dve", lambda e, h=h: e.max(out=top8[:, h, :], in_=bsb[:, h, :]),
                              reads=["bsb"], writes=["top8"])
                    P.add("dve", lambda e: e.tensor_tensor(out=selb, in0=bsb, in1=top8[:, :, 2:3].broadcast_to([128, 8, 16]),
                                                           op=ALU.is_ge), reads=["bsb", "top8"], writes=["selb"])
                    P.add("dve", lambda e, blk=blk: e.tensor_tensor(
                        out=selb, in0=selb, in1=seltab[:, 1, blk, :].unsqueeze(1).broadcast_to([128, 8, 16]), op=ALU.mult),
                        reads=["selb", "const"], writes=["selb"])
                    P.add("dve", lambda e, blk=blk: e.tensor_tensor(
                        out=selb, in0=selb, in1=seltab[:, 2, blk, :].unsqueeze(1).broadcast_to([128, 8, 16]), op=ALU.add),
                        reads=["selb", "const"], writes=["selb"])
                    P.add("dve", lambda e, s_=s_: e.tensor_scalar(out=mb[:, s_], in0=selb, scalar1=BIG, scalar2=-BIG,
                                                                  op0=ALU.mult, op1=ALU.add),
                          reads=["selb"], writes=["mb"])
                for h in range(8):
                    b = G.next()
                    for s_ in range(NST):
                        P.add("pe", lambda e, b=b, h=h, s_=s_: e.transpose(out=ps16[b][0:16, s_ * 128:(s_ + 1) * 128],
                                                                           in_=mb[:, s_, h, :], identity=ident),
                              reads=["mb", "const"], writes=[bk(b)])
                    P.add("act", lambda e, b=b, h=h: e.activation(out=Qa[64:80, h, :], in_=ps16[b][0:16, 0:T], func=AF.Copy),
                          reads=[bk(b)], writes=[("Qa", h)])
                yb_ = yaT[j % 2]
                nkt = (j + 1) * NST
                tasks = [(h, kt) for h in range(8) for kt in range(nkt)]
                LAG = 2
                obs = {}
                info = {}
                deferred = []
                BC = 6

                def emit_S(i, j=j, nkt=nkt):
                    h, kt = tasks[i]
                    if kt == 0:
                        obs[h] = OB.next()
                    r = kt - j * NST
                    c0 = 0 if r <= 0 else r * 128
                    sb_ = G.next()
                    P.add("pe", lambda e, sb_=sb_, kt=kt, c0=c0, h=h: e.matmul(
                        ps[sb_][:, c0:T], lhsT=Kc[0:80, h, kt * 128:(kt + 1) * 128], rhs=Qa[0:80, h, c0:T],
                        start=True, stop=True),
                        reads=[("Kc", kt // NST, h), "Kaux", ("Qa", h)], writes=[bk(sb_)])
                    pt = PT[pti[0] % 4]
                    ptk = ("PT", pti[0] % 4)
                    pti[0] += 1
                    P.add("act", lambda e, sb_=sb_, pt=pt, c0=c0: e.activation(out=pt[:, c0:T], in_=ps[sb_][:, c0:T],
                                                                                func=AF.Exp, scale=0.125),
                          reads=[bk(sb_)], writes=[ptk])
                    if r >= 0:
                        P.add("dve", lambda e, pt=pt, c0=c0: e.tensor_tensor(out=pt[:, c0:c0 + 128], in0=pt[:, c0:c0 + 128],
                                                                             in1=tri, op=ALU.mult),
                              reads=[ptk, "const"], writes=[ptk])
                    info[i] = (pt, ptk, c0)

                def norm_tail(h, ob, yb_=yb_, j=j):
                    P.add("pe", lambda e: e.matmul(ps[BC][0:64, :], lhsT=ones32[64:65, :], rhs=rc[64:65, :], start=True, stop=True),
                          reads=["rc", "const"], writes=[bk(BC)])
                    P.add("act", lambda e: e.activation(out=bcs[0:64, :], in_=ps[BC][0:64, :], func=AF.Copy),
                          reads=[bk(BC)], writes=["bcs"])
                    po = (h % 2) * 64
                    P.add("dve", lambda e, ob=ob, po=po, h=h, yb_=yb_: e.tensor_tensor(
                        out=yb_[po:po + 64, h // 2, :], in0=ps[ob][0:64, :], in1=bcs[0:64, :], op=ALU.mult),
                        reads=[bk(ob), "bcs"], writes=[("yaT", j % 2)])

                def emit_PV(i, nkt=nkt):
                    h, kt = tasks[i]
                    pt, ptk, c0 = info.pop(i)
                    ob = obs[h]
                    P.add("pe", lambda e, ob=ob, kt=kt, h=h, pt=pt, c0=c0, nkt=nkt: e.matmul(
                        ps[ob][0:65, c0:T], lhsT=Vc[:, kt, h, :], rhs=pt[:, c0:T],
                        start=(kt == 0), stop=(kt == nkt - 1), skip_group_check=True),
                        reads=[("Vc", kt), "Vones", ptk], writes=[bk(ob)])
                    if kt == nkt - 1:
                        P.add("dve", lambda e, ob=ob: e.reciprocal(out=rc[64:65, :], in_=ps[ob][64:65, :]),
                              reads=[bk(ob)], writes=["rc"])
                        deferred.append((i + LAG + 2, h, ob))

                ntask = len(tasks)
                for i in range(ntask + LAG + 3):
                    if i < ntask:
                        emit_S(i)
                    if 0 <= i - LAG < ntask:
                        emit_PV(i - LAG)
                    while deferred and deferred[0][0] <= i:
                        _, h_, ob_ = deferred.pop(0)
                        norm_tail(h_, ob_)
                assert not deferred and not info
                dma("sp", ya_s[:, tok0:tok0 + T].rearrange("(c p) t -> p c t", p=128), yb_,
                    [("yaT", j % 2)], [("yas", j)], "yaT%d" % (j % 2))
            P.barrier()
            AR.off = mark0

        if nt0 > 0:
            sweep0()

        def sweep1():
            setup_common(4, True)
            xt = cx.xt
            cosB = AR.alloc([T], F32)
            sinB = AR.alloc([T], F32)
            Qb = [AR.alloc([T], BF16) for _ in range(2)]
            t1 = AR.alloc([T], F32)
            t2 = AR.alloc([T], F32)
            qT = AR.alloc([4, T], BF16)
            kT = AR.alloc([4, T], BF16)
            ktok = AR.alloc([NST, 4, 128], BF16)
            vr = AR.alloc([NST, D], BF16)
            sil = AR.alloc([NST, D], BF16)
            Sst = AR.alloc([4, 256], F32)
            Sbf = AR.alloc([4, 256], BF16)
            attT = [AR.alloc([128], BF16) for _ in range(4)]
            st6 = AR.alloc([4, 6], F32)
            mv4 = AR.alloc([4, 2], F32)
            ve4 = AR.alloc([4], F32)
            rstd4 = AR.alloc([4], F32)
            ytmp = [AR.alloc([256], F32) for _ in range(2)]
            sgaA = AR.alloc([8, T], BF16)
            sgbA = AR.alloc([8, T], BF16)
            ybt = [AR.alloc([D], BF16) for _ in range(2)]
            ybT = AR.alloc([8, T], BF16)
            yaT1 = AR.alloc([4, T], BF16)
            mtmp = [AR.alloc([T], F32) for _ in range(2)]
            mrg = AR.alloc([8, T], BF16)
            P.add("pool", lambda e: e.memset(Sst, 0.0), writes=["S"])
            P.add("pool", lambda e: e.memset(Sbf, 0.0), writes=["Sbf"])
            G = RR([0, 1, 2, 3, 4, 5, 6, 7])
            ai = [0]
            for j in range(nt1):
                tok0 = j * T
                dma("sp", cosB, dr["c_cosB"][:, tok0:tok0 + T], [], ["tabB"], "tabB")
                dma("sp", sinB, dr["c_sinB"][:, tok0:tok0 + T], [], ["tabB"], "tabB")
                dma("sp", xt, dr["x"][tok0:tok0 + T, :].rearrange("(a p) d -> p a d", p=128), [],
                    [("xt", s_) for s_ in range(NST)], "xt")
                dma("sp", yaT1, ya_s[:, tok0:tok0 + T].rearrange("(c p) t -> p c t", p=128), [("yas", j)], ["yaT1"], "yaL")
                for s_ in range(NST):
                    norm_T(xt[:, s_, :], ("xt", s_), "norm_mix", s_, G.next(), [HK[s_]])
                for which, col0, dstT in (("q", C_RQ, qT), ("k", C_RK, kT)):
                    wv, wk = load_w(dr["w_in"][:, col0:col0 + 512], 8, 512)
                    for c in range(4):
                        qi = uid[0] % 2
                        uid[0] += 1

                        def comb(c=c, dstT=dstT, which=which):
                            P.add("dve", lambda e: e.tensor_tensor(out=dstT[:, c, :], in0=t1, in1=t2, op=ALU.add),
                                  reads=[("tB", 1), ("tB", 2)], writes=[(which + "T", c)])

                        rotary_chunk(G, wv, wk, c, permB, cosB, sinB, "tabB", Qb[qi], ("Qb", qi), t1, t2, "tB", comb)
                for g in range(2):
                    wv, wk = load_w(dr["w_in"][:, C_RV + g * 512:C_RV + (g + 1) * 512], 8, 512)
                    for s_ in range(NST):
                        b = G.next()
                        tm_group(b, wv, wk, lambda kc, s_=s_: cx.hT[:, kc, s_ * 128:(s_ + 1) * 128], 8, [HK[s_]])
                        P.add("act", lambda e, b=b, s_=s_, g=g: e.activation(out=vr[:, s_, g * 512:(g + 1) * 512], in_=ps[b],
                                                                             func=AF.Copy),
                              reads=[bk(b)], writes=[("vr", s_)])
                for g in range(2):
                    wv, wk = load_w(dr["w_in"][:, C_RG + g * 512:C_RG + (g + 1) * 512], 8, 512)
                    for s_ in range(NST):
                        b = G.next()
                        tm_group(b, wv, wk, lambda kc, s_=s_: cx.hT[:, kc, s_ * 128:(s_ + 1) * 128], 8, [HK[s_]])
                        P.add("act", lambda e, b=b, s_=s_, g=g: e.activation(out=sil[:, s_, g * 512:(g + 1) * 512], in_=ps[b],
                                                                             func=AF.Silu),
                              reads=[bk(b)], writes=[("sil", s_)])
                def gate_group(which, g):
                    col0 = (C_GA if which == "a" else C_GB) + g * 512
                    wv_, wk_ = load_w(dr["w_in"][:, col0:col0 + 512], 8, 512)
                    dstA = sgaA if which == "a" else sgbA
                    for c in range(4):
                        m = g * 4 + c
                        b = G.next()
                        fm_chunk(b, wv_, wk_, c, lambda kc: cx.hT[:, kc, :], 8, HK)
                        P.add("act", lambda e, b=b, m=m, dstA=dstA: e.activation(out=dstA[:, m, :], in_=ps[b], func=AF.Sigmoid),
                              reads=[bk(b)], writes=[("sg" + which, m)])

                gate_plan = [("a", 0), ("a", 1), ("b", 0), ("b", 1)]
                for s_ in range(NST):
                    cs = slice(s_ * 128, (s_ + 1) * 128)
                    b = G.next()
                    for h in range(4):
                        P.add("pe", lambda e, b=b, h=h, cs=cs: e.transpose(out=ps16[b][:, h * 128:(h + 1) * 128],
                                                                           in_=kT[:, h, cs], identity=ident),
                              reads=[("kT", h), "const"], writes=[bk(b)])
                    P.add("dve", lambda e, b=b, s_=s_: e.tensor_tensor(
                        out=ktok[:, s_], in0=ps16[b][:, 0:512].rearrange("p (h d) -> p h d", d=128),
                        in1=kdec.unsqueeze(2).broadcast_to([128, 4, 128]), op=ALU.mult),
                        reads=[bk(b), "const"], writes=[("ktok", s_)])
                    yb_ = ybt[s_ % 2]
                    ybk = ("ybt", s_ % 2)
                    bas = []
                    for h in range(4):
                        ba = G.next()
                        bas.append(ba)
                        P.add("pe", lambda e, ba=ba, h=h, cs=cs: e.matmul(ps[ba][:, 0:128], lhsT=kT[:, h, cs], rhs=qT[:, h, cs],
                                                                          start=True, stop=True),
                              reads=[("kT", h), ("qT", h)], writes=[bk(ba)])
                    ats = []
                    for h in range(4):
                        at = attT[h]
                        atk = ("attT", h)
                        ats.append((at, atk))
                        P.add("dve", lambda e, ba=bas[h], at=at, h=h: e.tensor_tensor(out=at, in0=ps[ba][:, 0:128], in1=M2[:, h, :],
                                                                                      op=ALU.mult),
                              reads=[bk(bas[h]), "const"], writes=[atk])
                    bys = []
                    for h in range(4):
                        hs = slice(h * 256, (h + 1) * 256)
                        at, atk = ats[h]
                        by = G.next()
                        bys.append(by)
                        P.add("pe", lambda e, by=by, at=at, s_=s_, hs=hs: e.matmul(ps[by][:, 0:256], lhsT=at, rhs=vr[:, s_, hs],
                                                                                   start=True, stop=False),
                              reads=[atk, ("vr", s_)], writes=[bk(by)])
                        P.add("pe", lambda e, by=by, h=h, cs=cs: e.matmul(ps[by][:, 0:256], lhsT=qT[:, h, cs], rhs=Sbf[:, h, :],
                                                                          start=False, stop=True),
                              reads=[("qT", h), ("Sbf", h)], writes=[bk(by)])
                        P.add("pe", lambda e, by=by, h=h, s_=s_, hs=hs: e.matmul(ps[by][:, 256:512], lhsT=ktok[:, s_, h, :],
                                                                                 rhs=vr[:, s_, hs], start=True, stop=True),
                              reads=[("ktok", s_), ("vr", s_)], writes=[bk(by)])
                    for h in range(4):
                        by = bys[h]
                        P.add("dve", lambda e, by=by, h=h: e.scalar_tensor_tensor(
                            out=Sst[:, h, :], in0=Sst[:, h, :], scalar=gamC[h], in1=ps[by][:, 256:512], op0=ALU.mult, op1=ALU.add),
                            reads=[bk(by), ("S", h)], writes=[("S", h)])
                        P.add("act", lambda e, h=h: e.activation(out=Sbf[:, h, :], in_=Sst[:, h, :], func=AF.Copy),
                              reads=[("S", h)], writes=[("Sbf", h)])
                    gate_group(*gate_plan[s_])
                    for h in range(4):
                        by = bys[h]
                        P.add("dve", lambda e, by=by, h=h: e.bn_stats(out=st6[:, h, :], in_=ps[by][:, 0:256]),
                              reads=[bk(by)], writes=[("st6", h)])
                        P.add("dve", lambda e, h=h: e.bn_aggr(out=mv4[:, h, :], in_=st6[:, h, :]), reads=[("st6", h)], writes=["mv4"])
                    P.add("dve", lambda e: e.tensor_tensor(out=ve4, in0=mv4[:, :, 1], in1=epsq, op=ALU.add),
                          reads=["mv4", "const"], writes=["ve4"])
                    P.add("act", lambda e: e.activation(out=rstd4, in_=ve4, func=AF.Sqrt), reads=["ve4"], writes=["rstd4"])
                    P.add("dve", lambda e: e.reciprocal(out=rstd4, in_=rstd4), reads=["rstd4"], writes=["rstd4"])
                    for h in range(4):
                        by = bys[h]
                        hs = slice(h * 256, (h + 1) * 256)
                        yt = ytmp[h % 2]
                        ytk = ("ytmp", h % 2)
                        P.add("dve", lambda e, by=by, s_=s_, hs=hs, h=h, yt=yt: e.scalar_tensor_tensor(
                            out=yt, in0=ps[by][:, 0:256], scalar=mv4[:, h, 0:1], in1=sil[:, s_, hs], op0=ALU.subtract, op1=ALU.mult),
                            reads=[bk(by), "mv4", ("sil", s_)], writes=[ytk])
                        P.add("dve", lambda e, yb_=yb_, hs=hs, h=h, yt=yt: e.tensor_scalar(out=yb_[:, hs], in0=yt, scalar1=rstd4[:, h:h + 1],
                                                                                         scalar2=None, op0=ALU.mult),
                              reads=[ytk, "rstd4"], writes=[ybk])
                    b = G.next()
                    for kc in range(8):
                        P.add("pe", lambda e, b=b, kc=kc, yb_=yb_: e.transpose(out=ps16[b][:, kc * 128:(kc + 1) * 128],
                                                                               in_=yb_[:, kc * 128:(kc + 1) * 128], identity=ident),
                              reads=[ybk, "const"], writes=[bk(b)])
                    P.add("act", lambda e, b=b, cs=cs: e.activation(out=ybT[:, :, cs], in_=ps16[b].rearrange("p (a b) -> p a b", b=128),
                                                                    func=AF.Copy),
                          reads=[bk(b)], writes=[("ybT", s_)])
                YBK = [("ybT", s_) for s_ in range(NST)]
                for g in range(2):
                    wA, kA = load_w(dr["w_branch_a"][:, g * 512:(g + 1) * 512], 4, 512)
                    wB, kB = load_w(dr["w_branch_b"][:, g * 512:(g + 1) * 512], 8, 512)
                    for c in range(4):
                        m = g * 4 + c
                        i2 = m % 2
                        b = G.next()
                        fm_chunk(b, wA, kA, c, lambda kc: yaT1[:, kc, :], 4, ["yaT1"])
                        P.add("dve", lambda e, b=b, i2=i2, m=m: e.tensor_tensor(out=mtmp[i2], in0=ps[b], in1=sgaA[:, m, :], op=ALU.mult),
                              reads=[bk(b), ("sga", m)], writes=[("mtmp", i2)])
                        b = G.next()
                        fm_chunk(b, wB, kB, c, lambda kc: ybT[:, kc, :], 8, YBK)
                        P.add("dve", lambda e, b=b, m=m: e.tensor_tensor(out=sgbA[:, m, :], in0=ps[b], in1=sgbA[:, m, :], op=ALU.mult),
                              reads=[bk(b), ("sgb", m)], writes=[("sgb", m)])
                        P.add("dve", lambda e, m=m, i2=i2: e.tensor_tensor(out=mrg[:, m, :], in0=mtmp[i2], in1=sgbA[:, m, :], op=ALU.add),
                              reads=[("mtmp", i2), ("sgb", m)], writes=[("mrg", m)])
                MK = [("mrg", mm) for mm in range(8)]
                for g in range(2):
                    wv, wk = load_w(dr["w_out"][:, g * 512:(g + 1) * 512], 8, 512)
                    for s_ in range(NST):
                        b = G.next()
                        tm_group(b, wv, wk, lambda kc, s_=s_: mrg[:, kc, s_ * 128:(s_ + 1) * 128], 8, MK)
                        P.add("dve", lambda e, b=b, s_=s_, g=g: e.tensor_tensor(out=xt[:, s_, g * 512:(g + 1) * 512],
                                                                                in0=ps[b], in1=xt[:, s_, g * 512:(g + 1) * 512], op=ALU.add),
                              reads=[bk(b), ("xt", s_)], writes=[("xt", s_)])
                dma("sp", x1_s[tok0:tok0 + T, :].rearrange("(a p) d -> p a d", p=128), xt,
                    [("xt", s_) for s_ in range(NST)], [("x1s", j)], "xto")
            P.barrier()
            AR.off = mark0

        if nt1 > 0:
            sweep1()

        def sweep2():
            setup_common(4, True)
            xt = cx.xt
            qxT = AR.alloc([8, T], BF16)
            PTx = [AR.alloc([T], BF16) for _ in range(4)]
            rcs = [AR.alloc([T], F32) for _ in range(2)]
            oT = AR.alloc([8, T], BF16)
            sgt = [AR.alloc([T], BF16) for _ in range(3)]
            aT = AR.alloc([NFC, T], BF16)
            ot = [AR.alloc([D], F32) for _ in range(2)]
            G = RR([0, 1, 2, 3])
            pi = [0]
            gi = [0]
            for j in range(nt2):
                tok0 = j * T
                src = x1_s if nt1 > 0 else dr["x"]
                dma("sp", xt, src[tok0:tok0 + T, :].rearrange("(a p) d -> p a d", p=128), [("x1s", j)],
                    [("xt", s_) for s_ in range(NST)], "xt")
                for s_ in range(NST):
                    norm_T(xt[:, s_, :], ("xt", s_), "norm_cross", s_, G.next(), [HK[s_]])
                for g in range(2):
                    wv, wk = load_w(dr["w_xq"][:, g * 512:(g + 1) * 512], 8, 512)
                    for c in range(4):
                        b = G.next()
                        m = g * 4 + c
                        fm_chunk(b, wv, wk, c, lambda kc: cx.hT[:, kc, :], 8, HK)
                        P.add("act", lambda e, b=b, m=m: e.activation(out=qxT[:, m, :], in_=ps[b], func=AF.Copy),
                              reads=[bk(b)], writes=[("qxT", m)])
                for h in range(4):
                    pts = []
                    for mc in range(2):
                        b = G.next()
                        for dc in range(2):
                            P.add("pe", lambda e, b=b, h=h, mc=mc, dc=dc: e.matmul(
                                ps[b], lhsT=kxT[:, 2 * h + dc, mc * 128:(mc + 1) * 128], rhs=qxT[:, 2 * h + dc, :],
                                start=(dc == 0), stop=(dc == 1)),
                                reads=["kxT", ("qxT", 2 * h + dc)], writes=[bk(b)])
                        pt = PTx[pi[0] % 4]
                        ptk = ("PTx", pi[0] % 4)
                        pi[0] += 1
                        P.add("act", lambda e, b=b, pt=pt: e.activation(out=pt, in_=ps[b], func=AF.Exp, scale=1.0 / 16),
                              reads=[bk(b)], writes=[ptk])
                        pts.append((pt, ptk))
                    b = G.next()
                    for mc in range(2):
                        P.add("pe", lambda e, b=b, mc=mc, pt=pts[mc][0]: e.matmul(ps[b], lhsT=onesb, rhs=pt, start=(mc == 0), stop=(mc == 1)),
                              reads=[pts[mc][1], "const"], writes=[bk(b)])
                    rcv = rcs[h % 2]
                    P.add("dve", lambda e, b=b, rcv=rcv: e.reciprocal(out=rcv, in_=ps[b]), reads=[bk(b)], writes=[("rcs", h % 2)])
                    for ec in range(2):
                        b = G.next()
                        for mc in range(2):
                            P.add("pe", lambda e, b=b, h=h, ec=ec, mc=mc, pt=pts[mc][0]: e.matmul(
                                ps[b], lhsT=vx[:, mc, h * 256 + ec * 128:h * 256 + (ec + 1) * 128], rhs=pt,
                                start=(mc == 0), stop=(mc == 1)),
                                reads=[pts[mc][1], "vx"], writes=[bk(b)])
                        mo = 2 * h + ec
                        P.add("dve", lambda e, b=b, rcv=rcv, mo=mo: e.tensor_tensor(out=oT[:, mo, :], in0=ps[b], in1=rcv, op=ALU.mult),
                              reads=[bk(b), ("rcs", h % 2)], writes=[("oT", mo)])
                OK_ = [("oT", mm) for mm in range(8)]
                for g in range(2):
                    wv, wk = load_w(dr["w_xo"][:, g * 512:(g + 1) * 512], 8, 512)
                    for s_ in range(NST):
                        b = G.next()
                        tm_group(b, wv, wk, lambda kc, s_=s_: oT[:, kc, s_ * 128:(s_ + 1) * 128], 8, OK_)
                        P.add("dve", lambda e, b=b, s_=s_, g=g: e.tensor_tensor(out=xt[:, s_, g * 512:(g + 1) * 512],
                                                                                in0=ps[b], in1=xt[:, s_, g * 512:(g + 1) * 512], op=ALU.add),
                              reads=[bk(b), ("xt", s_)], writes=[("xt", s_)])
                for s_ in range(NST):
                    norm_T(xt[:, s_, :], ("xt", s_), "norm_ffn", s_, G.next(), [HK[s_]])
                for fg in range(6):
                    ncol = 512 if fg < 5 else 256
                    wg, kg = load_w(dr["w_gate"][:, fg * 512:fg * 512 + ncol], 8, ncol)
                    wu, ku = load_w(dr["w_up"][:, fg * 512:fg * 512 + ncol], 8, ncol)
                    for c in range(ncol // 128):
                        fc = fg * 4 + c
                        bg = G.next()
                        fm_chunk(bg, wg, kg, c, lambda kc: cx.hT[:, kc, :], 8, HK)
                        sg_ = sgt[gi[0] % 3]
                        sgk = ("sgt", gi[0] % 3)
                        gi[0] += 1
                        P.add("act", lambda e, bg=bg, sg_=sg_: e.activation(out=sg_, in_=ps[bg], func=AF.Silu),
                              reads=[bk(bg)], writes=[sgk])
                        bu = G.next()
                        fm_chunk(bu, wu, ku, c, lambda kc: cx.hT[:, kc, :], 8, HK)
                        P.add("dve", lambda e, bu=bu, sg_=sg_, fc=fc: e.tensor_tensor(out=aT[:, fc, :], in0=ps[bu], in1=sg_, op=ALU.mult),
                              reads=[bk(bu), sgk], writes=[("aT", fc)])
                pieces = [(0, 8), (8, 8), (16, 6)]
                for g in range(2):
                    for (f0, nf) in pieces:
                        wv, wk = load_w(dr["w_down"][f0 * 128:(f0 + nf) * 128, g * 512:(g + 1) * 512], nf, 512)
                        for s_ in range(NST):
                            for fl in range(nf):
                                fc = f0 + fl
                                P.add("pe", lambda e, s_=s_, fl=fl, fc=fc, wv=wv: e.matmul(
                                    ps[4 + s_], lhsT=aT[:, fc, s_ * 128:(s_ + 1) * 128], rhs=wv[:, fl, :],
                                    start=(fc == 0), stop=(fc == NFC - 1), skip_group_check=True),
                                    reads=[wk, ("aT", fc)], writes=[bk(4 + s_)])
                    for s_ in range(NST):
                        P.add("dve", lambda e, s_=s_, g=g: e.tensor_tensor(out=xt[:, s_, g * 512:(g + 1) * 512], in0=ps[4 + s_],
                                                                           in1=xt[:, s_, g * 512:(g + 1) * 512], op=ALU.add),
                              reads=[bk(4 + s_), ("xt", s_)], writes=[("xt", s_)])
                for s_ in range(NST):
                    i = cx.nrm % 2
                    cx.nrm += 1
                    o_ = ot[s_ % 2]
                    ok_ = ("ot", s_ % 2)
                    src = xt[:, s_, :]
                    P.add("act", lambda e, src=src, i=i, junk=cx.junk: e.activation(out=junk, in_=src, func=AF.Square, accum_out=ss[i]),
                          reads=[("xt", s_)], writes=["junk", ("ss", i)])
                    P.add("act", lambda e, i=i: e.activation(out=sd[i], in_=ss[i], func=AF.Sqrt, scale=1.0 / D, bias=eps1),
                          reads=[("ss", i), "const"], writes=[("sd", i)])
                    P.add("dve", lambda e, i=i: e.reciprocal(out=rs[i], in_=sd[i]), reads=[("sd", i)], writes=[("rs", i)])
                    P.add("dve", lambda e, src=src, i=i, o_=o_: e.scalar_tensor_tensor(out=o_, in0=src, scalar=rs[i], in1=gfin,
                                                                                     op0=ALU.mult, op1=ALU.mult),
                          reads=[("xt", s_), ("rs", i), "const"], writes=[ok_])
                    dma("sp", out_d[tok0 + s_ * 128:tok0 + (s_ + 1) * 128, :], o_, [ok_], [], "ot%d" % (s_ % 2))
        if nt2 > 0:
            sweep2()
        P.emit()
        nc._n_ops = P.nops
    return nc


_NC_CACHE = {}


def kernel(**inputs):
    consts, _ = make_consts()
    if "nc" not in _NC_CACHE:
        _NC_CACHE["nc"] = build()
    nc = _NC_CACHE["nc"]
    x = np.ascontiguousarray(np.asarray(inputs["x"], dtype=np.float32))
    mem = np.ascontiguousarray(np.asarray(inputs["mem"], dtype=np.float32))
    shared = {}
    for name, shp in W_SPECS:
        shared[name] = np.ascontiguousarray(np.asarray(inputs[name], dtype=np.float32).reshape(shp))
    shared.update(consts)
    in_maps = []
    for b in range(8):
        m = dict(shared)
        m["x"] = x[b]
        m["mem"] = mem[b]
        in_maps.append(m)
    res = run_bass_kernel_spmd(nc, in_maps, core_ids=list(range(8)))
    return np.stack([np.asarray(r["out"], dtype=np.float32) for r in res.results], axis=0)
```

```python
import contextlib
import numpy as np
import ml_dtypes
import concourse.bass as bass
import concourse.mybir as mybir
from concourse.bass_utils import run_bass_kernel_spmd

F32 = mybir.dt.float32
BF16 = mybir.dt.bfloat16
AF = mybir.ActivationFunctionType
ALU = mybir.AluOpType
AX = mybir.AxisListType

S = 4096
D = 1024
T = 512
NT = S // T
NST = T // 128
DFF = 2816
NFC = DFF // 128
BIG = 30000.0
EPS = 1e-6
ALLQ = ("sp", "pe", "act", "dve", "pool")

C_AQ, C_AK, C_AV, C_RQ, C_RK, C_RV, C_RG, C_GA, C_GB = 0, 512, 1024, 1536, 2048, 2560, 3584, 4608, 5632


class Op:
    __slots__ = ("eng", "fn", "deps", "sem", "val", "signal", "is_dma", "idx")


class Prog:
    def __init__(self, nc):
        self.nc = nc
        self.q = {e: [] for e in ALLQ}
        self.lastw = {}
        self.readers = {}
        self.nops = 0
        self.last_on = {}

    limit = None
    skip = None

    def add(self, eng, fn, reads=(), writes=(), dma_sem=None):
        if Prog.limit is not None and self.nops >= Prog.limit:
            return None
        if Prog.skip and self.nops in Prog.skip:
            self.nops += 1
            return None
        is_dma = dma_sem is not None
        op = Op()
        op.eng, op.fn, op.is_dma = eng, fn, is_dma
        op.sem = dma_sem if is_dma else eng
        op.val = None
        op.signal = is_dma
        op.deps = []
        op.idx = self.nops
        self.nops += 1
        deps = []
        for k in reads:
            w = self.lastw.get(k)
            if w is not None:
                deps.append(w)
        for k in writes:
            w = self.lastw.get(k)
            if w is not None:
                deps.append(w)
            deps.extend(self.readers.get(k, ()))
        seen = set()
        for d in deps:
            if d is op or id(d) in seen:
                continue
            seen.add(id(d))
            if (not d.is_dma) and (not is_dma) and d.eng == "pe" and eng == "pe":
                continue
            op.deps.append(d)
            d.signal = True
        for k in writes:
            self.lastw[k] = op
            self.readers[k] = []
        for k in reads:
            self.readers.setdefault(k, []).append(op)
        self.q[eng].append(op)
        self.last_on[op.sem] = op
        return op

    def barrier(self):
        lasts = list(self.last_on.values())
        for o in lasts:
            o.signal = True
        for e in ALLQ:
            op = Op()
            op.eng, op.fn, op.is_dma = e, None, False
            op.sem, op.val, op.signal = e, None, False
            op.deps = list(lasts)
            op.idx = self.nops
            self.nops += 1
            self.q[e].append(op)
        self.lastw = {}
        self.readers = {}

    def emit(self):
        nc = self.nc
        cnt = {}
        allops = []
        for e in ALLQ:
            allops.extend(self.q[e])
        allops.sort(key=lambda o: o.idx)
        for op in allops:
            if op.signal and op.fn is not None:
                inc = 16 if op.is_dma else 1
                cnt[op.sem] = cnt.get(op.sem, 0) + inc
                op.val = cnt[op.sem]
        semnames = sorted(cnt.keys())
        sems = {}
        with contextlib.ExitStack() as st:
            for s in semnames:
                sems[s] = st.enter_context(nc.semaphore("s_" + s))
            block = st.enter_context(nc.Block())

            def run(engname, eng):
                waited = {}
                for op in self.q[engname]:
                    need = {}
                    for d in op.deps:
                        if d.val is None:
                            continue
                        if d.val > need.get(d.sem, 0):
                            need[d.sem] = d.val
                    for s, v in need.items():
                        if waited.get(s, 0) >= v:
                            continue
                        eng.wait_ge(sems[s], v)
                        waited[s] = v
                    if op.fn is None:
                        continue
                    ins = op.fn(eng)
                    if op.signal:
                        ins.then_inc(sems[op.sem], 16 if op.is_dma else 1)
                if engname == "sp":
                    for s in semnames:
                        if waited.get(s, 0) < cnt[s]:
                            eng.wait_ge(sems[s], cnt[s])

            block.sync(lambda e: run("sp", e))
            block.tensor(lambda e: run("pe", e))
            block.scalar(lambda e: run("act", e))
            block.vector(lambda e: run("dve", e))
            block.gpsimd(lambda e: run("pool", e))


class Arena:
    def __init__(self, nc, st, nbytes):
        self.nbytes = nbytes
        self.t16 = st.enter_context(nc.sbuf_tensor("arena", [128, nbytes // 2], BF16))
        self.t32 = self.t16.bitcast(F32)
        self.off = 0

    def alloc(self, shape, dt):
        n = int(np.prod(shape))
        nb = n * (4 if dt == F32 else 2)
        o = self.off
        self.off += (nb + 63) // 64 * 64
        assert self.off <= self.nbytes, ("SBUF arena overflow", self.off, self.nbytes)
        v = self.t32[:, o // 4:o // 4 + n] if dt == F32 else self.t16[:, o // 2:o // 2 + n]
        if len(shape) == 2:
            v = v.rearrange("p (a b) -> p a b", b=shape[1])
        elif len(shape) == 3:
            v = v.rearrange("p (a b c) -> p a b c", b=shape[1], c=shape[2])
        return v


def make_consts():
    bf = ml_dtypes.bfloat16
    c = {}
    c["c_ident"] = np.eye(128, dtype=np.float32).astype(bf)
    pa = np.zeros((128, 128), np.float32)
    pb = np.zeros((128, 128), np.float32)
    for m in range(128):
        pa[(m + 32) if (m % 64) < 32 else (m - 32), m] = 1.0
        pb[(m + 64) if m < 64 else (m - 64), m] = 1.0
    c["c_permA"] = pa.astype(bf)
    c["c_permB"] = pb.astype(bf)
    pos = np.arange(S, dtype=np.float32)
    invA = (10000.0 ** (-np.arange(0, 64, 2, dtype=np.float32) / 64)).astype(np.float32)
    invB = (10000.0 ** (-np.arange(0, 128, 2, dtype=np.float32) / 128)).astype(np.float32)
    angA = (pos[None, :] * invA[:, None]).astype(np.float32)
    angB = (pos[None, :] * invB[:, None]).astype(np.float32)
    p = np.arange(128)
    c["c_cosA"] = np.cos(angA)[p % 32].astype(np.float32)
    c["c_sinA"] = (np.sin(angA)[p % 32] * np.where((p % 64) < 32, -1.0, 1.0)[:, None]).astype(np.float32)
    c["c_cosB"] = np.cos(angB)[p % 64].astype(np.float32)
    c["c_sinB"] = (np.sin(angB)[p % 64] * np.where(p < 64, -1.0, 1.0)[:, None]).astype(np.float32)
    c["c_tri"] = (p[:, None] <= p[None, :]).astype(np.float32).astype(bf)
    gam = 1.0 - 2.0 ** (-5.0 - np.arange(4, dtype=np.float64))
    sc = 128.0 ** -0.5
    j = np.arange(128, dtype=np.float64)
    m2 = np.zeros((128, 4, 128), np.float64)
    for h in range(4):
        m2[:, h, :] = (sc * gam[h] ** (-(j + 1.0)))[:, None] * (j[:, None] <= j[None, :])
    c["c_M2"] = m2.astype(np.float32)
    c["c_kdec"] = (sc * gam[None, :] ** (127.0 - j[:, None])).astype(np.float32)
    c["c_epsq"] = (EPS / gam[None, :] ** (2.0 * (j[:, None] + 1.0))).astype(np.float32)
    n = np.arange(16)
    sel = np.zeros((3, 16, 16), np.float32)
    sel[0] = np.where(n[None, :] >= n[:, None], -BIG, 0.0)
    sel[1] = (n[None, :] < n[:, None]).astype(np.float32)
    sel[2] = (n[None, :] == n[:, None]).astype(np.float32)
    c["c_sel"] = sel
    c["c_onehot"] = (np.arange(S)[None, :] // 256 == n[:, None]).astype(np.float32).astype(bf)
    return c, gam


CONST_SPECS = [("c_ident", [128, 128], BF16), ("c_permA", [128, 128], BF16), ("c_permB", [128, 128], BF16),
               ("c_cosA", [128, S], F32), ("c_sinA", [128, S], F32), ("c_cosB", [128, S], F32),
               ("c_sinB", [128, S], F32), ("c_tri", [128, 128], BF16), ("c_M2", [128, 4, 128], F32),
               ("c_kdec", [128, 4], F32), ("c_epsq", [128, 4], F32), ("c_sel", [3, 16, 16], F32),
               ("c_onehot", [16, S], BF16)]

W_SPECS = [("norm_mix", [D]), ("w_in", [D, 6656]), ("w_branch_a", [512, D]), ("w_branch_b", [D, D]),
           ("w_out", [D, D]), ("norm_cross", [D]), ("norm_mem", [D]), ("w_xq", [D, D]), ("w_xkv", [D, 2 * D]),
           ("w_xo", [D, D]), ("norm_ffn", [D]), ("w_gate", [D, DFF]), ("w_up", [D, DFF]), ("w_down", [DFF, D]),
           ("norm_final", [D])]


def build(nt0=NT, nt1=NT, nt2=NT, dbg=False):
    _, gam = make_consts()
    gamC = [float(g ** 128.0) for g in gam]
    nc = bass.Bass("TRN2", target_bir_lowering=False)
    dr = {}
    dr["x"] = nc.dram_tensor("x", [S, D], F32, kind="ExternalInput").ap()
    dr["mem"] = nc.dram_tensor("mem", [256, D], F32, kind="ExternalInput").ap()
    for name, shp in W_SPECS:
        dr[name] = nc.dram_tensor(name, shp, F32, kind="ExternalInput").ap()
    for name, shp, dt in CONST_SPECS:
        dr[name] = nc.dram_tensor(name, shp, dt, kind="ExternalInput").ap()
    out_d = nc.dram_tensor("out", [S, D], F32, kind="ExternalOutput").ap()
    skind = "ExternalOutput" if dbg else "Internal"
    ya_s = nc.dram_tensor("ya_s", [512, S], BF16, kind=skind).ap()
    x1_s = nc.dram_tensor("x1_s", [S, D], F32, kind=skind).ap()

    with contextlib.ExitStack() as st:
        st.enter_context(nc.allow_low_precision(reason="bf16 matmul operands by design (fp32 accumulate)"))
        st.enter_context(nc.allow_non_contiguous_dma(reason="tiny norm-gain transposes / strided weight blocks"))
        AR = Arena(nc, st, 206 * 1024)
        ps_t = [st.enter_context(nc.psum_tensor("ps%d" % i, [128, 512], F32)) for i in range(8)]
        ps = [p_[:, :] for p_ in ps_t]
        ps16 = [p_.bitcast(BF16)[:, :] for p_ in ps_t]
        P = Prog(nc)
        uid = [0]

        def dma(q, out, in_, reads, writes, sem):
            P.add(q, lambda e: e.dma_start(out=out, in_=in_), reads=reads, writes=writes, dma_sem=sem)

        def bk(b):
            return ("ps", b)

        ident = AR.alloc([128], BF16)
        permA = AR.alloc([128], BF16)
        permB = AR.alloc([128], BF16)
        tri = AR.alloc([128], BF16)
        onesb = AR.alloc([128], BF16)
        ones32 = AR.alloc([64], F32)
        M2 = AR.alloc([4, 128], F32)
        kdec = AR.alloc([4], F32)
        epsq = AR.alloc([4], F32)
        seltab = AR.alloc([3, 16, 16], F32)
        eps1 = AR.alloc([1], F32)
        gT = {k: AR.alloc([8], F32) for k in ("norm_mix", "norm_cross", "norm_mem", "norm_ffn")}
        gfin = AR.alloc([D], F32)
        kxT = AR.alloc([8, 256], BF16)
        vx = AR.alloc([2, D], BF16)
        ss = [AR.alloc([1], F32) for _ in range(2)]
        sd = [AR.alloc([1], F32) for _ in range(2)]
        rs = [AR.alloc([1], F32) for _ in range(2)]
        for nm, buf in (("c_ident", ident), ("c_permA", permA), ("c_permB", permB), ("c_tri", tri),
                        ("c_M2", M2), ("c_kdec", kdec), ("c_epsq", epsq)):
            dma("sp", buf, dr[nm], [], ["const"], "const")
        dma("sp", seltab.rearrange("p a b c -> p (a b c)"),
            dr["c_sel"].rearrange("a b c -> (a b c)").partition_broadcast(128), [], ["const"], "const")
        for k in gT:
            dma("sp", gT[k], dr[k].rearrange("(c p) -> p c", p=128), [], ["const"], "const")
        dma("sp", gfin, dr["norm_final"].partition_broadcast(128), [], ["const"], "const")
        P.add("pool", lambda e: e.memset(onesb, 1.0), writes=["const"])
        P.add("pool", lambda e: e.memset(ones32, 1.0), writes=["const"])
        P.add("pool", lambda e: e.memset(eps1, EPS), writes=["const"])
        persist_mark = AR.off

        class Ctx:
            pass

        cx = Ctx()

        def setup_common(nw, xt_full):
            cx.wslots = [AR.alloc([8, 512], BF16) for _ in range(nw)]
            cx.wi = 0
            cx.nw = nw
            cx.xb = [AR.alloc([D], BF16) for _ in range(2)]
            cx.junk = AR.alloc([D], BF16)
            cx.hT = AR.alloc([8, T], BF16)
            cx.xts = [AR.alloc([NST, D], F32) for _ in range(2)] if xt_full else None
            cx.xt = cx.xts[0] if xt_full else None
            cx.nrm = 0

        def load_w(src, kcs, n):
            s = cx.wi % cx.nw
            cx.wi += 1
            dst = cx.wslots[s][:, 0:kcs, 0:n]
            dma("pool", dst, src.rearrange("(kc p) n -> p kc n", p=128), [], [("w", s)], "w%d" % s)
            return dst, ("w", s)

        def norm_T(xsrc, xkey, gkey, st_i, bank, hkeys):
            i = cx.nrm % 2
            cx.nrm += 1
            xb = cx.xb[i]
            junk = cx.junk
            hT = cx.hT
            P.add("act", lambda e: e.activation(out=junk, in_=xsrc, func=AF.Square, accum_out=ss[i]),
                  reads=[xkey], writes=["junk", ("ss", i)])
            P.add("act", lambda e: e.activation(out=sd[i], in_=ss[i], func=AF.Sqrt, scale=1.0 / D, bias=eps1),
                  reads=[("ss", i), "const"], writes=[("sd", i)])
            P.add("dve", lambda e: e.reciprocal(out=rs[i], in_=sd[i]), reads=[("sd", i)], writes=[("rs", i)])
            P.add("dve", lambda e: e.tensor_scalar(out=xb, in0=xsrc, scalar1=rs[i], scalar2=None, op0=ALU.mult),
                  reads=[xkey, ("rs", i)], writes=[("xb", i)])
            pT = ps16[bank]
            for kc in range(8):
                P.add("pe", lambda e, kc=kc: e.transpose(out=pT[:, kc * 128:(kc + 1) * 128],
                                                         in_=xb[:, kc * 128:(kc + 1) * 128], identity=ident),
                      reads=[("xb", i), "const"], writes=[bk(bank)])
            g = gT[gkey]
            P.add("dve", lambda e: e.tensor_tensor(out=hT[:, :, st_i * 128:(st_i + 1) * 128],
                                                   in0=pT.rearrange("p (a b) -> p a b", b=128),
                                                   in1=g.unsqueeze(2).broadcast_to([128, 8, 128]), op=ALU.mult),
                  reads=[bk(bank), "const"], writes=hkeys)

        def fm_chunk(bank, wv, wkey, c, rhs_of_kc, nk, rkeys, ncols=T):
            for kc in range(nk):
                r_ = rhs_of_kc(kc)
                P.add("pe", lambda e, kc=kc, r_=r_: e.matmul(ps[bank][:, 0:ncols], lhsT=wv[:, kc, c * 128:(c + 1) * 128],
                                                             rhs=r_, start=(kc == 0), stop=(kc == nk - 1)),
                      reads=[wkey] + rkeys, writes=[bk(bank)])

        def tm_group(bank, wv, wkey, lhs_of_kc, nk, lkeys, ncols=512):
            for kc in range(nk):
                l_ = lhs_of_kc(kc)
                P.add("pe", lambda e, kc=kc, l_=l_: e.matmul(ps[bank][:, 0:ncols], lhsT=l_, rhs=wv[:, kc, 0:ncols],
                                                             start=(kc == 0), stop=(kc == nk - 1)),
                      reads=[wkey] + lkeys, writes=[bk(bank)])

        class RR:
            def __init__(self, ids):
                self.ids = list(ids)
                self.i = 0

            def next(self):
                b = self.ids[self.i % len(self.ids)]
                self.i += 1
                return b

        HK = [("hT", i) for i in range(NST)]

        def rotary_chunk(G, wv, wkey, c, perm, cosT, sinT, tabkey, Qb, qbkey, t1, t2, tkey, out_fn):
            b1 = G.next()
            fm_chunk(b1, wv, wkey, c, lambda kc: cx.hT[:, kc, :], 8, HK)
            P.add("act", lambda e: e.activation(out=Qb, in_=ps[b1], func=AF.Copy), reads=[bk(b1)], writes=[qbkey])
            P.add("dve", lambda e: e.tensor_tensor(out=t1, in0=ps[b1], in1=cosT, op=ALU.mult),
                  reads=[bk(b1), tabkey, qbkey], writes=[(tkey, 1)])
            b2 = G.next()
            P.add("pe", lambda e: e.matmul(ps[b2], lhsT=perm, rhs=Qb, start=True, stop=True),
                  reads=[qbkey, "const"], writes=[bk(b2)])
            P.add("dve", lambda e: e.tensor_tensor(out=t2, in0=ps[b2], in1=sinT, op=ALU.mult),
                  reads=[bk(b2), tabkey], writes=[(tkey, 2)])
            out_fn()

        mark0 = AR.off
        setup_common(3, False)
        mt = AR.alloc([2, D], F32)
        mT = AR.alloc([8, 256], BF16)
        dma("sp", mt, dr["mem"].rearrange("(a p) d -> p a d", p=128), [], ["mt"], "mt")
        G = RR([0, 1, 2, 3])
        for a in range(2):
            i = cx.nrm % 2
            cx.nrm += 1
            xb = cx.xb[i]
            src = mt[:, a, :]
            P.add("act", lambda e, src=src, i=i, junk=cx.junk: e.activation(out=junk, in_=src, func=AF.Square, accum_out=ss[i]),
                  reads=["mt"], writes=["junk", ("ss", i)])
            P.add("act", lambda e, i=i: e.activation(out=sd[i], in_=ss[i], func=AF.Sqrt, scale=1.0 / D, bias=eps1),
                  reads=[("ss", i), "const"], writes=[("sd", i)])
            P.add("dve", lambda e, i=i: e.reciprocal(out=rs[i], in_=sd[i]), reads=[("sd", i)], writes=[("rs", i)])
            P.add("dve", lambda e, src=src, i=i, xb=xb: e.tensor_scalar(out=xb, in0=src, scalar1=rs[i], scalar2=None,
                                                                      op0=ALU.mult),
                  reads=["mt", ("rs", i)], writes=[("xb", i)])
            b = G.next()
            pT = ps16[b]
            for kc in range(8):
                P.add("pe", lambda e, kc=kc, pT=pT, xb=xb: e.transpose(out=pT[:, kc * 128:(kc + 1) * 128],
                                                                    in_=xb[:, kc * 128:(kc + 1) * 128], identity=ident),
                      reads=[("xb", i), "const"], writes=[bk(b)])
            P.add("dve", lambda e, pT=pT, a=a: e.tensor_tensor(out=mT[:, :, a * 128:(a + 1) * 128],
                                                             in0=pT.rearrange("p (a b) -> p a b", b=128),
                                                             in1=gT["norm_mem"].unsqueeze(2).broadcast_to([128, 8, 128]),
                                                             op=ALU.mult),
                  reads=[bk(b), "const"], writes=["mT"])
        for g in range(2):
            wv, wk = load_w(dr["w_xkv"][:, g * 512:(g + 1) * 512], 8, 512)
            for c in range(4):
                b = G.next()
                fm_chunk(b, wv, wk, c, lambda kc: mT[:, kc, :], 8, ["mT"], ncols=256)
                P.add("act", lambda e, b=b, cc=g * 4 + c: e.activation(out=kxT[:, cc, :], in_=ps[b][:, 0:256], func=AF.Copy),
                      reads=[bk(b)], writes=["kxT"])
        for g in range(2):
            wv, wk = load_w(dr["w_xkv"][:, D + g * 512:D + (g + 1) * 512], 8, 512)
            for a in range(2):
                b = G.next()
                tm_group(b, wv, wk, lambda kc, a=a: mT[:, kc, a * 128:(a + 1) * 128], 8, ["mT"])
                P.add("act", lambda e, b=b, a=a, g=g: e.activation(out=vx[:, a, g * 512:(g + 1) * 512], in_=ps[b], func=AF.Copy),
                      reads=[bk(b)], writes=["vx"])
        P.barrier()
        AR.off = mark0

        def sweep0():
            setup_common(3, False)
            xs = [AR.alloc([D], F32) for _ in range(2)]
            Kc = AR.alloc([8, S], BF16)
            Vc = AR.alloc([32, 8, 65], BF16)
            Qa = AR.alloc([8, T], BF16)
            kmT = AR.alloc([8, 16], BF16)
            kmf = AR.alloc([8, 2], F32)
            cosA = AR.alloc([T], F32)
            sinA = AR.alloc([T], F32)
            Qb = [AR.alloc([T], BF16) for _ in range(2)]
            t1 = AR.alloc([T], F32)
            t2 = AR.alloc([T], F32)
            bsb = AR.alloc([8, 16], F32)
            top8 = AR.alloc([8, 8], F32)
            selb = AR.alloc([8, 16], F32)
            mb = AR.alloc([NST, 8, 16], BF16)
            PT = [AR.alloc([T], BF16) for _ in range(4)]
            rc = AR.alloc([T], F32)
            bcs = AR.alloc([T], F32)
            yaT = [AR.alloc([4, T], BF16) for _ in range(2)]
            for h in range(8):
                dma("sp", Kc[64:80, h, :], dr["c_onehot"], [], ["Kaux"], "const")
            P.add("pool", lambda e: e.memset(Vc[:, :, :, 64:65], 1.0), writes=["Vones"])
            P.add("pool", lambda e: e.memset(kmT[0:64], 0.0), writes=["kmT"])
            G = RR([0, 1, 2, 3])
            OB = RR([4, 5])
            pti = [0]
            for j in range(nt0):
                tok0 = j * T
                dma("sp", cosA, dr["c_cosA"][:, tok0:tok0 + T], [], ["tabA"], "tabA")
                dma("sp", sinA, dr["c_sinA"][:, tok0:tok0 + T], [], ["tabA"], "tabA")
                for s_ in range(NST):
                    xi = (j * NST + s_) % 2
                    dma("sp", xs[xi], dr["x"][tok0 + s_ * 128:tok0 + (s_ + 1) * 128, :], [], [("xs", xi)], "xs%d" % xi)
                    norm_T(xs[xi], ("xs", xi), "norm_mix", s_, G.next(), [HK[s_]])
                for which, col0 in (("q", C_AQ), ("k", C_AK)):
                    wv, wk = load_w(dr["w_in"][:, col0:col0 + 512], 8, 512)
                    for c in range(4):
                        qi = uid[0] % 2
                        uid[0] += 1

                        def comb(c=c, which=which):
                            for half in range(2):
                                h = 2 * c + half
                                if which == "q":
                                    dst = Qa[0:64, h, :]
                                    wr = [("Qa", h)]
                                else:
                                    dst = Kc[0:64, h, tok0:tok0 + T]
                                    wr = [("Kc", j, h)]
                                P.add("dve", lambda e, dst=dst, half=half: e.tensor_tensor(
                                    out=dst, in0=t1[half * 64:(half + 1) * 64, :], in1=t2[half * 64:(half + 1) * 64, :],
                                    op=ALU.add), reads=[("tA", 1), ("tA", 2)], writes=wr)

                        rotary_chunk(G, wv, wk, c, permA, cosA, sinA, "tabA", Qb[qi], ("Qb", qi), t1, t2, "tA", comb)
                wv, wk = load_w(dr["w_in"][:, C_AV:C_AV + 512], 8, 512)
                for s_ in range(NST):
                    b = G.next()
                    tm_group(b, wv, wk, lambda kc, s_=s_: cx.hT[:, kc, s_ * 128:(s_ + 1) * 128], 8, [HK[s_]])
                    kt = j * NST + s_
                    P.add("act", lambda e, b=b, kt=kt: e.activation(out=Vc[:, kt, :, 0:64],
                                                                    in_=ps[b].rearrange("p (h d) -> p h d", d=64),
                                                                    func=AF.Copy),
                          reads=[bk(b), "Vones"], writes=[("Vc", kt)])
                P.add("dve", lambda e, j=j: e.tensor_reduce(
                    out=kmf[0:64], in_=Kc[0:64, :, j * T:(j + 1) * T].rearrange("p h (n k) -> p h n k", k=256),
                    axis=AX.X, op=ALU.add), reads=[("Kc", j, h) for h in range(8)], writes=["kmf"])
                P.add("dve", lambda e, j=j: e.tensor_scalar(out=kmT[0:64, :, 2 * j:2 * j + 2], in0=kmf[0:64],
                                                            scalar1=1.0 / 256, scalar2=None, op0=ALU.mult),
                      reads=["kmf"], writes=["kmT"])
                BSB = 7
                for s_ in range(NST):
                    for h in range(8):
                        P.add("pe", lambda e, s_=s_, h=h: e.matmul(
                            ps[BSB][:, (s_ * 8 + h) * 16:(s_ * 8 + h + 1) * 16],
                            lhsT=Qa[0:64, h, s_ * 128:(s_ + 1) * 128], rhs=kmT[0:64, h, :], start=True, stop=True),
                            reads=[("Qa", h), "kmT"], writes=[bk(BSB)])
                for s_ in range(NST):
                    blk = (j * NST + s_) // 2
                    P.add("dve", lambda e, s_=s_, blk=blk: e.tensor_tensor(
                        out=bsb, in0=ps[BSB][:, s_ * 128:(s_ + 1) * 128].rearrange("p (h n) -> p h n", n=16),
                        in1=seltab[:, 0, blk, :].unsqueeze(1).broadcast_to([128, 8, 16]), op=ALU.add),
                        reads=[bk(BSB), "const"], writes=["bsb"])
                    for h in range(8):
                        P.add("dve", lambda e, h=h: e.max(out=top8[:, h, :], in_=bsb[:, h, :]),
                              reads=["bsb"], writes=["top8"])
                    P.add("dve", lambda e: e.tensor_tensor(out=selb, in0=bsb, in1=top8[:, :, 2:3].broadcast_to([128, 8, 16]),
                                                           op=ALU.is_ge), reads=["bsb", "top8"], writes=["selb"])
                    P.add("dve", lambda e, blk=blk: e.tensor_tensor(
                        out=selb, in0=selb, in1=seltab[:, 1, blk, :].unsqueeze(1).broadcast_to([128, 8, 16]), op=ALU.mult),
                        reads=["selb", "const"], writes=["selb"])
                    P.add("dve", lambda e, blk=blk: e.tensor_tensor(
                        out=selb, in0=selb, in1=seltab[:, 2, blk, :].unsqueeze(1).broadcast_to([128, 8, 16]), op=ALU.add),
                        reads=["selb", "const"], writes=["selb"])
                    P.add("dve", lambda e, s_=s_: e.tensor_scalar(out=mb[:, s_], in0=selb, scalar1=BIG, scalar2=-BIG,
                                                                  op0=ALU.mult, op1=ALU.add),
                          reads=["selb"], writes=["mb"])
                for h in range(8):
                    b = G.next()
                    for s_ in range(NST):
                        P.add("pe", lambda e, b=b, h=h, s_=s_: e.transpose(out=ps16[b][0:16, s_ * 128:(s_ + 1) * 128],
                                                                           in_=mb[:, s_, h, :], identity=ident),
                              reads=["mb", "const"], writes=[bk(b)])
                    P.add("act", lambda e, b=b, h=h: e.activation(out=Qa[64:80, h, :], in_=ps16[b][0:16, 0:T], func=AF.Copy),
                          reads=[bk(b)], writes=[("Qa", h)])
                yb_ = yaT[j % 2]
                nkt = (j + 1) * NST
                tasks = [(h, kt) for h in range(8) for kt in range(nkt)]
                LAG = 2
                obs = {}
                info = {}
                deferred = []
                BC = 6

                def emit_S(i, j=j, nkt=nkt):
                    h, kt = tasks[i]
                    if kt == 0:
                        obs[h] = OB.next()
                    r = kt - j * NST
                    c0 = 0 if r <= 0 else r * 128
                    sb_ = G.next()
                    P.add("pe", lambda e, sb_=sb_, kt=kt, c0=c0, h=h: e.matmul(
                        ps[sb_][:, c0:T], lhsT=Kc[0:80, h, kt * 128:(kt + 1) * 128], rhs=Qa[0:80, h, c0:T],
                        start=True, stop=True),
                        reads=[("Kc", kt // NST, h), "Kaux", ("Qa", h)], writes=[bk(sb_)])
                    pt = PT[pti[0] % 4]
                    ptk = ("PT", pti[0] % 4)
                    pti[0] += 1
                    P.add("act", lambda e, sb_=sb_, pt=pt, c0=c0: e.activation(out=pt[:, c0:T], in_=ps[sb_][:, c0:T],
                                                                                func=AF.Exp, scale=0.125),
                          reads=[bk(sb_)], writes=[ptk])
                    if r >= 0:
                        P.add("dve", lambda e, pt=pt, c0=c0: e.tensor_tensor(out=pt[:, c0:c0 + 128], in0=pt[:, c0:c0 + 128],
                                                                             in1=tri, op=ALU.mult),
                              reads=[ptk, "const"], writes=[ptk])
                    info[i] = (pt, ptk, c0)

                def norm_tail(h, ob, yb_=yb_, j=j):
                    P.add("pe", lambda e: e.matmul(ps[BC][0:64, :], lhsT=ones32[64:65, :], rhs=rc[64:65, :], start=True, stop=True),
                          reads=["rc", "const"], writes=[bk(BC)])
                    P.add("act", lambda e: e.activation(out=bcs[0:64, :], in_=ps[BC][0:64, :], func=AF.Copy),
                          reads=[bk(BC)], writes=["bcs"])
                    po = (h % 2) * 64
                    P.add("dve", lambda e, ob=ob, po=po, h=h, yb_=yb_: e.tensor_tensor(
                        out=yb_[po:po + 64, h // 2, :], in0=ps[ob][0:64, :], in1=bcs[0:64, :], op=ALU.mult),
                        reads=[bk(ob), "bcs"], writes=[("yaT", j % 2)])

                def emit_PV(i, nkt=nkt):
                    h, kt = tasks[i]
                    pt, ptk, c0 = info.pop(i)
                    ob = obs[h]
                    P.add("pe", lambda e, ob=ob, kt=kt, h=h, pt=pt, c0=c0, nkt=nkt: e.matmul(
                        ps[ob][0:65, c0:T], lhsT=Vc[:, kt, h, :], rhs=pt[:, c0:T],
                        start=(kt == 0), stop=(kt == nkt - 1), skip_group_check=True),
                        reads=[("Vc", kt), "Vones", ptk], writes=[bk(ob)])
                    if kt == nkt - 1:
                        P.add("dve", lambda e, ob=ob: e.reciprocal(out=rc[64:65, :], in_=ps[ob][64:65, :]),
                              reads=[bk(ob)], writes=["rc"])
                        deferred.append((i + LAG + 2, h, ob))

                ntask = len(tasks)
                for i in range(ntask + LAG + 3):
                    if i < ntask:
                        emit_S(i)
                    if 0 <= i - LAG < ntask:
                        emit_PV(i - LAG)
                    while deferred and deferred[0][0] <= i:
                        _, h_, ob_ = deferred.pop(0)
                        norm_tail(h_, ob_)
                assert not deferred and not info
                dma("sp", ya_s[:, tok0:tok0 + T].rearrange("(c p) t -> p c t", p=128), yb_,
                    [("yaT", j % 2)], [("yas", j)], "yaT%d" % (j % 2))
            P.barrier()
            AR.off = mark0

        if nt0 > 0:
            sweep0()

        def sweep1():
            setup_common(4, True)
            cosB = AR.alloc([T], F32)
            sinB = AR.alloc([T], F32)
            Qb = [AR.alloc([T], BF16) for _ in range(2)]
            t1 = AR.alloc([T], F32)
            t2 = AR.alloc([T], F32)
            qT = AR.alloc([4, T], BF16)
            kT = AR.alloc([4, T], BF16)
            ktok = AR.alloc([NST, 4, 128], BF16)
            vr = AR.alloc([NST, D], BF16)
            sil = AR.alloc([NST, D], BF16)
            Sst = AR.alloc([4, 256], F32)
            Sbf = AR.alloc([4, 256], BF16)
            attT = [AR.alloc([128], BF16) for _ in range(4)]
            st6 = AR.alloc([4, 6], F32)
            mv4 = AR.alloc([4, 2], F32)
            ve4 = AR.alloc([4], F32)
            rstd4 = AR.alloc([4], F32)
            ytmp = [AR.alloc([256], F32) for _ in range(2)]
            sgaA = AR.alloc([8, T], BF16)
            sgbA = AR.alloc([8, T], BF16)
            ybt = [AR.alloc([D], BF16) for _ in range(2)]
            ybT = AR.alloc([8, T], BF16)
            yaT1 = AR.alloc([4, T], BF16)
            mtmp = [AR.alloc([T], F32) for _ in range(2)]
            mrg = AR.alloc([8, T], BF16)
            P.add("pool", lambda e: e.memset(Sst, 0.0), writes=["S"])
            P.add("pool", lambda e: e.memset(Sbf, 0.0), writes=["Sbf"])
            G = RR([0, 1, 2, 3, 4, 5, 6, 7])
            ai = [0]
            for j in range(nt1):
                tok0 = j * T
                dma("sp", cosB, dr["c_cosB"][:, tok0:tok0 + T], [], ["tabB"], "tabB")
                dma("sp", sinB, dr["c_sinB"][:, tok0:tok0 + T], [], ["tabB"], "tabB")
                def xload1(jn):
                    bi = jn % 2
                    dma("sp", cx.xts[bi], dr["x"][jn * T:(jn + 1) * T, :].rearrange("(a p) d -> p a d", p=128), [],
                        [("xt", bi, s_) for s_ in range(NST)], "xt%d" % bi)

                if j == 0:
                    xload1(0)
                if j + 1 < nt1:
                    xload1(j + 1)
                xt = cx.xts[j % 2]
                XK = [("xt", j % 2, s_) for s_ in range(NST)]
                dma("sp", yaT1, ya_s[:, tok0:tok0 + T].rearrange("(c p) t -> p c t", p=128), [("yas", j)], ["yaT1"], "yaL")
                for s_ in range(NST):
                    norm_T(xt[:, s_, :], XK[s_], "norm_mix", s_, G.next(), [HK[s_]])
                for which, col0, dstT in (("q", C_RQ, qT), ("k", C_RK, kT)):
                    wv, wk = load_w(dr["w_in"][:, col0:col0 + 512], 8, 512)
                    for c in range(4):
                        qi = uid[0] % 2
                        uid[0] += 1

                        def comb(c=c, dstT=dstT, which=which):
                            P.add("dve", lambda e: e.tensor_tensor(out=dstT[:, c, :], in0=t1, in1=t2, op=ALU.add),
                                  reads=[("tB", 1), ("tB", 2)], writes=[(which + "T", c)])

                        rotary_chunk(G, wv, wk, c, permB, cosB, sinB, "tabB", Qb[qi], ("Qb", qi), t1, t2, "tB", comb)
                for g in range(2):
                    wv, wk = load_w(dr["w_in"][:, C_RV + g * 512:C_RV + (g + 1) * 512], 8, 512)
                    for s_ in range(NST):
                        b = G.next()
                        tm_group(b, wv, wk, lambda kc, s_=s_: cx.hT[:, kc, s_ * 128:(s_ + 1) * 128], 8, [HK[s_]])
                        P.add("act", lambda e, b=b, s_=s_, g=g: e.activation(out=vr[:, s_, g * 512:(g + 1) * 512], in_=ps[b],
                                                                             func=AF.Copy),
                              reads=[bk(b)], writes=[("vr", s_)])
                for g in range(2):
                    wv, wk = load_w(dr["w_in"][:, C_RG + g * 512:C_RG + (g + 1) * 512], 8, 512)
                    for s_ in range(NST):
                        b = G.next()
                        tm_group(b, wv, wk, lambda kc, s_=s_: cx.hT[:, kc, s_ * 128:(s_ + 1) * 128], 8, [HK[s_]])
                        P.add("act", lambda e, b=b, s_=s_, g=g: e.activation(out=sil[:, s_, g * 512:(g + 1) * 512], in_=ps[b],
                                                                             func=AF.Silu),
                              reads=[bk(b)], writes=[("sil", s_)])
                def gate_group(which, g):
                    col0 = (C_GA if which == "a" else C_GB) + g * 512
                    wv_, wk_ = load_w(dr["w_in"][:, col0:col0 + 512], 8, 512)
                    dstA = sgaA if which == "a" else sgbA
                    for c in range(4):
                        m = g * 4 + c
                        b = G.next()
                        fm_chunk(b, wv_, wk_, c, lambda kc: cx.hT[:, kc, :], 8, HK)
                        P.add("act", lambda e, b=b, m=m, dstA=dstA: e.activation(out=dstA[:, m, :], in_=ps[b], func=AF.Sigmoid),
                              reads=[bk(b)], writes=[("sg" + which, m)])

                gate_plan = [("a", 0), ("a", 1), ("b", 0), ("b", 1)]
                for s_ in range(NST):
                    cs = slice(s_ * 128, (s_ + 1) * 128)
                    b = G.next()
                    for h in range(4):
                        P.add("pe", lambda e, b=b, h=h, cs=cs: e.transpose(out=ps16[b][:, h * 128:(h + 1) * 128],
                                                                           in_=kT[:, h, cs], identity=ident),
                              reads=[("kT", h), "const"], writes=[bk(b)])
                    P.add("dve", lambda e, b=b, s_=s_: e.tensor_tensor(
                        out=ktok[:, s_], in0=ps16[b][:, 0:512].rearrange("p (h d) -> p h d", d=128),
                        in1=kdec.unsqueeze(2).broadcast_to([128, 4, 128]), op=ALU.mult),
                        reads=[bk(b), "const"], writes=[("ktok", s_)])
                    yb_ = ybt[s_ % 2]
                    ybk = ("ybt", s_ % 2)
                    bas = []
                    for h in range(4):
                        ba = G.next()
                        bas.append(ba)
                        P.add("pe", lambda e, ba=ba, h=h, cs=cs: e.matmul(ps[ba][:, 0:128], lhsT=kT[:, h, cs], rhs=qT[:, h, cs],
                                                                          start=True, stop=True),
                              reads=[("kT", h), ("qT", h)], writes=[bk(ba)])
                    ats = []
                    for h in range(4):
                        at = attT[h]
                        atk = ("attT", h)
                        ats.append((at, atk))
                        P.add("dve", lambda e, ba=bas[h], at=at, h=h: e.tensor_tensor(out=at, in0=ps[ba][:, 0:128], in1=M2[:, h, :],
                                                                                      op=ALU.mult),
                              reads=[bk(bas[h]), "const"], writes=[atk])
                    bys = []
                    for h in range(4):
                        hs = slice(h * 256, (h + 1) * 256)
                        at, atk = ats[h]
                        by = G.next()
                        bys.append(by)
                        P.add("pe", lambda e, by=by, at=at, s_=s_, hs=hs: e.matmul(ps[by][:, 0:256], lhsT=at, rhs=vr[:, s_, hs],
                                                                                   start=True, stop=False),
                              reads=[atk, ("vr", s_)], writes=[bk(by)])
                        P.add("pe", lambda e, by=by, h=h, cs=cs: e.matmul(ps[by][:, 0:256], lhsT=qT[:, h, cs], rhs=Sbf[:, h, :],
                                                                          start=False, stop=True),
                              reads=[("qT", h), ("Sbf", h)], writes=[bk(by)])
                        P.add("pe", lambda e, by=by, h=h, s_=s_, hs=hs: e.matmul(ps[by][:, 256:512], lhsT=ktok[:, s_, h, :],
                                                                                 rhs=vr[:, s_, hs], start=True, stop=True),
                              reads=[("ktok", s_), ("vr", s_)], writes=[bk(by)])
                    for h in range(4):
                        by = bys[h]
                        P.add("dve", lambda e, by=by, h=h: e.scalar_tensor_tensor(
                            out=Sst[:, h, :], in0=Sst[:, h, :], scalar=gamC[h], in1=ps[by][:, 256:512], op0=ALU.mult, op1=ALU.add),
                            reads=[bk(by), ("S", h)], writes=[("S", h)])
                        P.add("act", lambda e, h=h: e.activation(out=Sbf[:, h, :], in_=Sst[:, h, :], func=AF.Copy),
                              reads=[("S", h)], writes=[("Sbf", h)])
                    gate_group(*gate_plan[s_])
                    for h in range(4):
                        by = bys[h]
                        P.add("dve", lambda e, by=by, h=h: e.bn_stats(out=st6[:, h, :], in_=ps[by][:, 0:256]),
                              reads=[bk(by)], writes=[("st6", h)])
                        P.add("dve", lambda e, h=h: e.bn_aggr(out=mv4[:, h, :], in_=st6[:, h, :]), reads=[("st6", h)], writes=["mv4"])
                    P.add("dve", lambda e: e.tensor_tensor(out=ve4, in0=mv4[:, :, 1], in1=epsq, op=ALU.add),
                          reads=["mv4", "const"], writes=["ve4"])
                    P.add("act", lambda e: e.activation(out=rstd4, in_=ve4, func=AF.Sqrt), reads=["ve4"], writes=["rstd4"])
                    P.add("dve", lambda e: e.reciprocal(out=rstd4, in_=rstd4), reads=["rstd4"], writes=["rstd4"])
                    for h in range(4):
                        by = bys[h]
                        hs = slice(h * 256, (h + 1) * 256)
                        yt = ytmp[h % 2]
                        ytk = ("ytmp", h % 2)
                        P.add("dve", lambda e, by=by, s_=s_, hs=hs, h=h, yt=yt: e.scalar_tensor_tensor(
                            out=yt, in0=ps[by][:, 0:256], scalar=mv4[:, h, 0:1], in1=sil[:, s_, hs], op0=ALU.subtract, op1=ALU.mult),
                            reads=[bk(by), "mv4", ("sil", s_)], writes=[ytk])
                        P.add("dve", lambda e, yb_=yb_, hs=hs, h=h, yt=yt: e.tensor_scalar(out=yb_[:, hs], in0=yt, scalar1=rstd4[:, h:h + 1],
                                                                                         scalar2=None, op0=ALU.mult),
                              reads=[ytk, "rstd4"], writes=[ybk])
                    b = G.next()
                    for kc in range(8):
                        P.add("pe", lambda e, b=b, kc=kc, yb_=yb_: e.transpose(out=ps16[b][:, kc * 128:(kc + 1) * 128],
                                                                               in_=yb_[:, kc * 128:(kc + 1) * 128], identity=ident),
                              reads=[ybk, "const"], writes=[bk(b)])
                    P.add("act", lambda e, b=b, cs=cs: e.activation(out=ybT[:, :, cs], in_=ps16[b].rearrange("p (a b) -> p a b", b=128),
                                                                    func=AF.Copy),
                          reads=[bk(b)], writes=[("ybT", s_)])
                YBK = [("ybT", s_) for s_ in range(NST)]
                for g in range(2):
                    wA, kA = load_w(dr["w_branch_a"][:, g * 512:(g + 1) * 512], 4, 512)
                    wB, kB = load_w(dr["w_branch_b"][:, g * 512:(g + 1) * 512], 8, 512)
                    for c in range(4):
                        m = g * 4 + c
                        i2 = m % 2
                        b = G.next()
                        fm_chunk(b, wA, kA, c, lambda kc: yaT1[:, kc, :], 4, ["yaT1"])
                        P.add("dve", lambda e, b=b, i2=i2, m=m: e.tensor_tensor(out=mtmp[i2], in0=ps[b], in1=sgaA[:, m, :], op=ALU.mult),
                              reads=[bk(b), ("sga", m)], writes=[("mtmp", i2)])
                        b = G.next()
                        fm_chunk(b, wB, kB, c, lambda kc: ybT[:, kc, :], 8, YBK)
                        P.add("dve", lambda e, b=b, m=m: e.tensor_tensor(out=sgbA[:, m, :], in0=ps[b], in1=sgbA[:, m, :], op=ALU.mult),
                              reads=[bk(b), ("sgb", m)], writes=[("sgb", m)])
                        P.add("dve", lambda e, m=m, i2=i2: e.tensor_tensor(out=mrg[:, m, :], in0=mtmp[i2], in1=sgbA[:, m, :], op=ALU.add),
                              reads=[("mtmp", i2), ("sgb", m)], writes=[("mrg", m)])
                MK = [("mrg", mm) for mm in range(8)]
                for g in range(2):
                    wv, wk = load_w(dr["w_out"][:, g * 512:(g + 1) * 512], 8, 512)
                    for s_ in range(NST):
                        b = G.next()
                        tm_group(b, wv, wk, lambda kc, s_=s_: mrg[:, kc, s_ * 128:(s_ + 1) * 128], 8, MK)
                        P.add("dve", lambda e, b=b, s_=s_, g=g, xt=xt: e.tensor_tensor(out=xt[:, s_, g * 512:(g + 1) * 512],
                                                                                       in0=ps[b], in1=xt[:, s_, g * 512:(g + 1) * 512], op=ALU.add),
                              reads=[bk(b), XK[s_]], writes=[XK[s_]])
                dma("sp", x1_s[tok0:tok0 + T, :].rearrange("(a p) d -> p a d", p=128), xt,
                    list(XK), [("x1s", j)], "xto%d" % (j % 2))
            P.barrier()
            AR.off = mark0

        if nt1 > 0:
            sweep1()

        def sweep2():
            setup_common(4, True)
            qxT = AR.alloc([8, T], BF16)
            PTx = [AR.alloc([T], BF16) for _ in range(4)]
            rcs = [AR.alloc([T], F32) for _ in range(2)]
            oT = AR.alloc([8, T], BF16)
            sgt = [AR.alloc([T], BF16) for _ in range(3)]
            aT = AR.alloc([NFC, T], BF16)
            ot = [AR.alloc([D], F32) for _ in range(2)]
            G = RR([0, 1, 2, 3])
            pi = [0]
            gi = [0]
            for j in range(nt2):
                tok0 = j * T
                src = x1_s if nt1 > 0 else dr["x"]

                def xload2(jn):
                    bi = jn % 2
                    dma("sp", cx.xts[bi], src[jn * T:(jn + 1) * T, :].rearrange("(a p) d -> p a d", p=128), [("x1s", jn)],
                        [("xt", bi, s_) for s_ in range(NST)], "xt%d" % bi)

                if j == 0:
                    xload2(0)
                if j + 1 < nt2:
                    xload2(j + 1)
                xt = cx.xts[j % 2]
                XK = [("xt", j % 2, s_) for s_ in range(NST)]
                for s_ in range(NST):
                    norm_T(xt[:, s_, :], XK[s_], "norm_cross", s_, G.next(), [HK[s_]])
                for g in range(2):
                    wv, wk = load_w(dr["w_xq"][:, g * 512:(g + 1) * 512], 8, 512)
                    for c in range(4):
                        b = G.next()
                        m = g * 4 + c
                        fm_chunk(b, wv, wk, c, lambda kc: cx.hT[:, kc, :], 8, HK)
                        P.add("act", lambda e, b=b, m=m: e.activation(out=qxT[:, m, :], in_=ps[b], func=AF.Copy),
                              reads=[bk(b)], writes=[("qxT", m)])
                for h in range(4):
                    pts = []
                    for mc in range(2):
                        b = G.next()
                        for dc in range(2):
                            P.add("pe", lambda e, b=b, h=h, mc=mc, dc=dc: e.matmul(
                                ps[b], lhsT=kxT[:, 2 * h + dc, mc * 128:(mc + 1) * 128], rhs=qxT[:, 2 * h + dc, :],
                                start=(dc == 0), stop=(dc == 1)),
                                reads=["kxT", ("qxT", 2 * h + dc)], writes=[bk(b)])
                        pt = PTx[pi[0] % 4]
                        ptk = ("PTx", pi[0] % 4)
                        pi[0] += 1
                        P.add("act", lambda e, b=b, pt=pt: e.activation(out=pt, in_=ps[b], func=AF.Exp, scale=1.0 / 16),
                              reads=[bk(b)], writes=[ptk])
                        pts.append((pt, ptk))
                    b = G.next()
                    for mc in range(2):
                        P.add("pe", lambda e, b=b, mc=mc, pt=pts[mc][0]: e.matmul(ps[b], lhsT=onesb, rhs=pt, start=(mc == 0), stop=(mc == 1)),
                              reads=[pts[mc][1], "const"], writes=[bk(b)])
                    rcv = rcs[h % 2]
                    P.add("dve", lambda e, b=b, rcv=rcv: e.reciprocal(out=rcv, in_=ps[b]), reads=[bk(b)], writes=[("rcs", h % 2)])
                    for ec in range(2):
                        b = G.next()
                        for mc in range(2):
                            P.add("pe", lambda e, b=b, h=h, ec=ec, mc=mc, pt=pts[mc][0]: e.matmul(
                                ps[b], lhsT=vx[:, mc, h * 256 + ec * 128:h * 256 + (ec + 1) * 128], rhs=pt,
                                start=(mc == 0), stop=(mc == 1)),
                                reads=[pts[mc][1], "vx"], writes=[bk(b)])
                        mo = 2 * h + ec
                        P.add("dve", lambda e, b=b, rcv=rcv, mo=mo: e.tensor_tensor(out=oT[:, mo, :], in0=ps[b], in1=rcv, op=ALU.mult),
                              reads=[bk(b), ("rcs", h % 2)], writes=[("oT", mo)])
                OK_ = [("oT", mm) for mm in range(8)]
                for g in range(2):
                    wv, wk = load_w(dr["w_xo"][:, g * 512:(g + 1) * 512], 8, 512)
                    for s_ in range(NST):
                        b = G.next()
                        tm_group(b, wv, wk, lambda kc, s_=s_: oT[:, kc, s_ * 128:(s_ + 1) * 128], 8, OK_)
                        P.add("dve", lambda e, b=b, s_=s_, g=g, xt=xt: e.tensor_tensor(out=xt[:, s_, g * 512:(g + 1) * 512],
                                                                                       in0=ps[b], in1=xt[:, s_, g * 512:(g + 1) * 512], op=ALU.add),
                              reads=[bk(b), XK[s_]], writes=[XK[s_]])
                for s_ in range(NST):
                    norm_T(xt[:, s_, :], XK[s_], "norm_ffn", s_, G.next(), [HK[s_]])
                for fg in range(6):
                    ncol = 512 if fg < 5 else 256
                    wg, kg = load_w(dr["w_gate"][:, fg * 512:fg * 512 + ncol], 8, ncol)
                    wu, ku = load_w(dr["w_up"][:, fg * 512:fg * 512 + ncol], 8, ncol)
                    for c in range(ncol // 128):
                        fc = fg * 4 + c
                        bg = G.next()
                        fm_chunk(bg, wg, kg, c, lambda kc: cx.hT[:, kc, :], 8, HK)
                        sg_ = sgt[gi[0] % 3]
                        sgk = ("sgt", gi[0] % 3)
                        gi[0] += 1
                        P.add("act", lambda e, bg=bg, sg_=sg_: e.activation(out=sg_, in_=ps[bg], func=AF.Silu),
                              reads=[bk(bg)], writes=[sgk])
                        bu = G.next()
                        fm_chunk(bu, wu, ku, c, lambda kc: cx.hT[:, kc, :], 8, HK)
                        P.add("dve", lambda e, bu=bu, sg_=sg_, fc=fc: e.tensor_tensor(out=aT[:, fc, :], in0=ps[bu], in1=sg_, op=ALU.mult),
                              reads=[bk(bu), sgk], writes=[("aT", fc)])
                pieces = [(0, 8), (8, 8), (16, 6)]
                for g in range(2):
                    for (f0, nf) in pieces:
                        wv, wk = load_w(dr["w_down"][f0 * 128:(f0 + nf) * 128, g * 512:(g + 1) * 512], nf, 512)
                        for s_ in range(NST):
                            for fl in range(nf):
                                fc = f0 + fl
                                P.add("pe", lambda e, s_=s_, fl=fl, fc=fc, wv=wv: e.matmul(
                                    ps[4 + s_], lhsT=aT[:, fc, s_ * 128:(s_ + 1) * 128], rhs=wv[:, fl, :],
                                    start=(fc == 0), stop=(fc == NFC - 1), skip_group_check=True),
                                    reads=[wk, ("aT", fc)], writes=[bk(4 + s_)])
                    for s_ in range(NST):
                        P.add("dve", lambda e, s_=s_, g=g, xt=xt: e.tensor_tensor(out=xt[:, s_, g * 512:(g + 1) * 512], in0=ps[4 + s_],
                                                                                  in1=xt[:, s_, g * 512:(g + 1) * 512], op=ALU.add),
                              reads=[bk(4 + s_), XK[s_]], writes=[XK[s_]])
                for s_ in range(NST):
                    i = cx.nrm % 2
                    cx.nrm += 1
                    o_ = ot[s_ % 2]
                    ok_ = ("ot", s_ % 2)
                    src = xt[:, s_, :]
                    P.add("act", lambda e, src=src, i=i, junk=cx.junk: e.activation(out=junk, in_=src, func=AF.Square, accum_out=ss[i]),
                          reads=[XK[s_]], writes=["junk", ("ss", i)])
                    P.add("act", lambda e, i=i: e.activation(out=sd[i], in_=ss[i], func=AF.Sqrt, scale=1.0 / D, bias=eps1),
                          reads=[("ss", i), "const"], writes=[("sd", i)])
                    P.add("dve", lambda e, i=i: e.reciprocal(out=rs[i], in_=sd[i]), reads=[("sd", i)], writes=[("rs", i)])
                    P.add("dve", lambda e, src=src, i=i, o_=o_: e.scalar_tensor_tensor(out=o_, in0=src, scalar=rs[i], in1=gfin,
                                                                                     op0=ALU.mult, op1=ALU.mult),
                          reads=[XK[s_], ("rs", i), "const"], writes=[ok_])
                    dma("sp", out_d[tok0 + s_ * 128:tok0 + (s_ + 1) * 128, :], o_, [ok_], [], "ot%d" % (s_ % 2))
        if nt2 > 0:
            sweep2()
        P.emit()
        nc._n_ops = P.nops
    return nc


_NC_CACHE = {}


def kernel(**inputs):
    consts, _ = make_consts()
    if "nc" not in _NC_CACHE:
        _NC_CACHE["nc"] = build()
    nc = _NC_CACHE["nc"]
    x = np.ascontiguousarray(np.asarray(inputs["x"], dtype=np.float32))
    mem = np.ascontiguousarray(np.asarray(inputs["mem"], dtype=np.float32))
    shared = {}
    for name, shp in W_SPECS:
        shared[name] = np.ascontiguousarray(np.asarray(inputs[name], dtype=np.float32).reshape(shp))
    shared.update(consts)
    in_maps = []
    for b in range(8):
        m = dict(shared)
        m["x"] = x[b]
        m["mem"] = mem[b]
        in_maps.append(m)
    res = run_bass_kernel_spmd(nc, in_maps, core_ids=list(range(8)))
    return np.stack([np.asarray(r["out"], dtype=np.float32) for r in res.results], axis=0)
```

```python
import contextlib
import numpy as np
import ml_dtypes
import concourse.bass as bass
import concourse.mybir as mybir
from concourse.bass_utils import run_bass_kernel_spmd

F32 = mybir.dt.float32
BF16 = mybir.dt.bfloat16
AF = mybir.ActivationFunctionType
ALU = mybir.AluOpType
AX = mybir.AxisListType

S = 4096
D = 1024
T = 512
NT = S // T
NST = T // 128
DFF = 2816
NFC = DFF // 128
BIG = 30000.0
EPS = 1e-6
ALLQ = ("sp", "pe", "act", "dve", "pool")

C_AQ, C_AK, C_AV, C_RQ, C_RK, C_RV, C_RG, C_GA, C_GB = 0, 512, 1024, 1536, 2048, 2560, 3584, 4608, 5632


class Op:
    __slots__ = ("eng", "fn", "deps", "sem", "val", "signal", "is_dma", "idx")


class Prog:
    def __init__(self, nc):
        self.nc = nc
        self.q = {e: [] for e in ALLQ}
        self.lastw = {}
        self.readers = {}
        self.nops = 0
        self.last_on = {}

    limit = None
    skip = None

    def add(self, eng, fn, reads=(), writes=(), dma_sem=None):
        if Prog.limit is not None and self.nops >= Prog.limit:
            return None
        if Prog.skip and self.nops in Prog.skip:
            self.nops += 1
            return None
        is_dma = dma_sem is not None
        op = Op()
        op.eng, op.fn, op.is_dma = eng, fn, is_dma
        op.sem = dma_sem if is_dma else eng
        op.val = None
        op.signal = is_dma
        op.deps = []
        op.idx = self.nops
        self.nops += 1
        deps = []
        for k in reads:
            w = self.lastw.get(k)
            if w is not None:
                deps.append(w)
        for k in writes:
            w = self.lastw.get(k)
            if w is not None:
                deps.append(w)
            deps.extend(self.readers.get(k, ()))
        seen = set()
        for d in deps:
            if d is op or id(d) in seen:
                continue
            seen.add(id(d))
            if (not d.is_dma) and (not is_dma) and d.eng == "pe" and eng == "pe":
                continue
            op.deps.append(d)
            d.signal = True
        for k in writes:
            self.lastw[k] = op
            self.readers[k] = []
        for k in reads:
            self.readers.setdefault(k, []).append(op)
        self.q[eng].append(op)
        self.last_on[op.sem] = op
        return op

    def barrier(self):
        lasts = list(self.last_on.values())
        for o in lasts:
            o.signal = True
        for e in ALLQ:
            op = Op()
            op.eng, op.fn, op.is_dma = e, None, False
            op.sem, op.val, op.signal = e, None, False
            op.deps = list(lasts)
            op.idx = self.nops
            self.nops += 1
            self.q[e].append(op)
        self.lastw = {}
        self.readers = {}

    def emit(self):
        nc = self.nc
        cnt = {}
        allops = []
        for e in ALLQ:
            allops.extend(self.q[e])
        allops.sort(key=lambda o: o.idx)
        for op in allops:
            if op.signal and op.fn is not None:
                inc = 16 if op.is_dma else 1
                cnt[op.sem] = cnt.get(op.sem, 0) + inc
                op.val = cnt[op.sem]
        semnames = sorted(cnt.keys())
        sems = {}
        with contextlib.ExitStack() as st:
            for s in semnames:
                sems[s] = st.enter_context(nc.semaphore("s_" + s))
            block = st.enter_context(nc.Block())

            def run(engname, eng):
                waited = {}
                for op in self.q[engname]:
                    need = {}
                    for d in op.deps:
                        if d.val is None:
                            continue
                        if d.val > need.get(d.sem, 0):
                            need[d.sem] = d.val
                    for s, v in need.items():
                        if waited.get(s, 0) >= v:
                            continue
                        eng.wait_ge(sems[s], v)
                        waited[s] = v
                    if op.fn is None:
                        continue
                    ins = op.fn(eng)
                    if op.signal:
                        ins.then_inc(sems[op.sem], 16 if op.is_dma else 1)
                if engname == "sp":
                    for s in semnames:
                        if waited.get(s, 0) < cnt[s]:
                            eng.wait_ge(sems[s], cnt[s])

            block.sync(lambda e: run("sp", e))
            block.tensor(lambda e: run("pe", e))
            block.scalar(lambda e: run("act", e))
            block.vector(lambda e: run("dve", e))
            block.gpsimd(lambda e: run("pool", e))


class Arena:
    def __init__(self, nc, st, nbytes):
        self.nbytes = nbytes
        self.t16 = st.enter_context(nc.sbuf_tensor("arena", [128, nbytes // 2], BF16))
        self.t32 = self.t16.bitcast(F32)
        self.off = 0

    def alloc(self, shape, dt):
        n = int(np.prod(shape))
        nb = n * (4 if dt == F32 else 2)
        o = self.off
        self.off += (nb + 63) // 64 * 64
        assert self.off <= self.nbytes, ("SBUF arena overflow", self.off, self.nbytes)
        v = self.t32[:, o // 4:o // 4 + n] if dt == F32 else self.t16[:, o // 2:o // 2 + n]
        if len(shape) == 2:
            v = v.rearrange("p (a b) -> p a b", b=shape[1])
        elif len(shape) == 3:
            v = v.rearrange("p (a b c) -> p a b c", b=shape[1], c=shape[2])
        return v


def make_consts():
    bf = ml_dtypes.bfloat16
    c = {}
    c["c_ident"] = np.eye(128, dtype=np.float32).astype(bf)
    pa = np.zeros((128, 128), np.float32)
    pb = np.zeros((128, 128), np.float32)
    for m in range(128):
        pa[(m + 32) if (m % 64) < 32 else (m - 32), m] = 1.0
        pb[(m + 64) if m < 64 else (m - 64), m] = 1.0
    c["c_permA"] = pa.astype(bf)
    c["c_permB"] = pb.astype(bf)
    pos = np.arange(S, dtype=np.float32)
    invA = (10000.0 ** (-np.arange(0, 64, 2, dtype=np.float32) / 64)).astype(np.float32)
    invB = (10000.0 ** (-np.arange(0, 128, 2, dtype=np.float32) / 128)).astype(np.float32)
    angA = (pos[None, :] * invA[:, None]).astype(np.float32)
    angB = (pos[None, :] * invB[:, None]).astype(np.float32)
    p = np.arange(128)
    c["c_cosA"] = np.cos(angA)[p % 32].astype(np.float32)
    c["c_sinA"] = (np.sin(angA)[p % 32] * np.where((p % 64) < 32, -1.0, 1.0)[:, None]).astype(np.float32)
    c["c_cosB"] = np.cos(angB)[p % 64].astype(np.float32)
    c["c_sinB"] = (np.sin(angB)[p % 64] * np.where(p < 64, -1.0, 1.0)[:, None]).astype(np.float32)
    c["c_tri"] = (p[:, None] <= p[None, :]).astype(np.float32).astype(bf)
    gam = 1.0 - 2.0 ** (-5.0 - np.arange(4, dtype=np.float64))
    sc = 128.0 ** -0.5
    j = np.arange(128, dtype=np.float64)
    m2 = np.zeros((128, 4, 128), np.float64)
    for h in range(4):
        m2[:, h, :] = (sc * gam[h] ** (-(j + 1.0)))[:, None] * (j[:, None] <= j[None, :])
    c["c_M2"] = m2.astype(np.float32)
    c["c_kdec"] = (sc * gam[None, :] ** (127.0 - j[:, None])).astype(np.float32)
    c["c_epsq"] = (EPS / gam[None, :] ** (2.0 * (j[:, None] + 1.0))).astype(np.float32)
    n = np.arange(16)
    sel = np.zeros((3, 16, 16), np.float32)
    sel[0] = np.where(n[None, :] >= n[:, None], -BIG, 0.0)
    sel[1] = (n[None, :] < n[:, None]).astype(np.float32)
    sel[2] = (n[None, :] == n[:, None]).astype(np.float32)
    c["c_sel"] = sel
    c["c_onehot"] = (np.arange(S)[None, :] // 256 == n[:, None]).astype(np.float32).astype(bf)
    return c, gam


CONST_SPECS = [("c_ident", [128, 128], BF16), ("c_permA", [128, 128], BF16), ("c_permB", [128, 128], BF16),
               ("c_cosA", [128, S], F32), ("c_sinA", [128, S], F32), ("c_cosB", [128, S], F32),
               ("c_sinB", [128, S], F32), ("c_tri", [128, 128], BF16), ("c_M2", [128, 4, 128], F32),
               ("c_kdec", [128, 4], F32), ("c_epsq", [128, 4], F32), ("c_sel", [3, 16, 16], F32),
               ("c_onehot", [16, S], BF16)]

W_SPECS = [("norm_mix", [D]), ("w_in", [D, 6656]), ("w_branch_a", [512, D]), ("w_branch_b", [D, D]),
           ("w_out", [D, D]), ("norm_cross", [D]), ("norm_mem", [D]), ("w_xq", [D, D]), ("w_xkv", [D, 2 * D]),
           ("w_xo", [D, D]), ("norm_ffn", [D]), ("w_gate", [D, DFF]), ("w_up", [D, DFF]), ("w_down", [DFF, D]),
           ("norm_final", [D])]


def build(nt0=NT, nt1=NT, nt2=NT, dbg=False):
    _, gam = make_consts()
    gamC = [float(g ** 128.0) for g in gam]
    nc = bass.Bass("TRN2", target_bir_lowering=False)
    dr = {}
    dr["x"] = nc.dram_tensor("x", [S, D], F32, kind="ExternalInput").ap()
    dr["mem"] = nc.dram_tensor("mem", [256, D], F32, kind="ExternalInput").ap()
    for name, shp in W_SPECS:
        dr[name] = nc.dram_tensor(name, shp, F32, kind="ExternalInput").ap()
    for name, shp, dt in CONST_SPECS:
        dr[name] = nc.dram_tensor(name, shp, dt, kind="ExternalInput").ap()
    out_d = nc.dram_tensor("out", [S, D], F32, kind="ExternalOutput").ap()
    skind = "ExternalOutput" if dbg else "Internal"
    ya_s = nc.dram_tensor("ya_s", [512, S], BF16, kind=skind).ap()
    x1_s = nc.dram_tensor("x1_s", [S, D], F32, kind=skind).ap()

    with contextlib.ExitStack() as st:
        st.enter_context(nc.allow_low_precision(reason="bf16 matmul operands by design (fp32 accumulate)"))
        st.enter_context(nc.allow_non_contiguous_dma(reason="tiny norm-gain transposes / strided weight blocks"))
        AR = Arena(nc, st, 206 * 1024)
        ps_t = [st.enter_context(nc.psum_tensor("ps%d" % i, [128, 512], F32)) for i in range(8)]
        ps = [p_[:, :] for p_ in ps_t]
        ps16 = [p_.bitcast(BF16)[:, :] for p_ in ps_t]
        P = Prog(nc)
        uid = [0]

        def dma(q, out, in_, reads, writes, sem):
            P.add(q, lambda e: e.dma_start(out=out, in_=in_), reads=reads, writes=writes, dma_sem=sem)

        def bk(b):
            return ("ps", b)

        ident = AR.alloc([128], BF16)
        permA = AR.alloc([128], BF16)
        permB = AR.alloc([128], BF16)
        tri = AR.alloc([128], BF16)
        onesb = AR.alloc([128], BF16)
        ones32 = AR.alloc([64], F32)
        M2 = AR.alloc([4, 128], F32)
        kdec = AR.alloc([4], F32)
        epsq = AR.alloc([4], F32)
        seltab = AR.alloc([3, 16, 16], F32)
        eps1 = AR.alloc([1], F32)
        gT = {k: AR.alloc([8], F32) for k in ("norm_mix", "norm_cross", "norm_mem", "norm_ffn")}
        gfin = AR.alloc([D], F32)
        kxT = AR.alloc([8, 256], BF16)
        vx = AR.alloc([2, D], BF16)
        ss = [AR.alloc([1], F32) for _ in range(2)]
        sd = [AR.alloc([1], F32) for _ in range(2)]
        rs = [AR.alloc([1], F32) for _ in range(2)]
        for nm, buf in (("c_ident", ident), ("c_permA", permA), ("c_permB", permB), ("c_tri", tri),
                        ("c_M2", M2), ("c_kdec", kdec), ("c_epsq", epsq)):
            dma("sp", buf, dr[nm], [], ["const"], "const")
        dma("sp", seltab.rearrange("p a b c -> p (a b c)"),
            dr["c_sel"].rearrange("a b c -> (a b c)").partition_broadcast(128), [], ["const"], "const")
        for k in gT:
            dma("sp", gT[k], dr[k].rearrange("(c p) -> p c", p=128), [], ["const"], "const")
        dma("sp", gfin, dr["norm_final"].partition_broadcast(128), [], ["const"], "const")
        P.add("pool", lambda e: e.memset(onesb, 1.0), writes=["const"])
        P.add("pool", lambda e: e.memset(ones32, 1.0), writes=["const"])
        P.add("pool", lambda e: e.memset(eps1, EPS), writes=["const"])
        persist_mark = AR.off

        class Ctx:
            pass

        cx = Ctx()

        def setup_common(nw, xt_full):
            cx.wslots = [AR.alloc([8, 512], BF16) for _ in range(nw)]
            cx.wi = 0
            cx.nw = nw
            cx.xb = [AR.alloc([D], BF16) for _ in range(2)]
            cx.junk = AR.alloc([D], BF16)
            cx.hT = AR.alloc([8, T], BF16)
            cx.xts = [AR.alloc([NST, D], F32) for _ in range(2)] if xt_full else None
            cx.xt = cx.xts[0] if xt_full else None
            cx.nrm = 0

        def load_w(src, kcs, n):
            s = cx.wi % cx.nw
            cx.wi += 1
            dst = cx.wslots[s][:, 0:kcs, 0:n]
            dma("pool", dst, src.rearrange("(kc p) n -> p kc n", p=128), [], [("w", s)], "w%d" % s)
            return dst, ("w", s)

        def norm_T(xsrc, xkey, gkey, st_i, bank, hkeys):
            i = cx.nrm % 2
            cx.nrm += 1
            xb = cx.xb[i]
            junk = cx.junk
            hT = cx.hT
            P.add("act", lambda e: e.activation(out=junk, in_=xsrc, func=AF.Square, accum_out=ss[i]),
                  reads=[xkey], writes=["junk", ("ss", i)])
            P.add("act", lambda e: e.activation(out=sd[i], in_=ss[i], func=AF.Sqrt, scale=1.0 / D, bias=eps1),
                  reads=[("ss", i), "const"], writes=[("sd", i)])
            P.add("dve", lambda e: e.reciprocal(out=rs[i], in_=sd[i]), reads=[("sd", i)], writes=[("rs", i)])
            P.add("dve", lambda e: e.tensor_scalar(out=xb, in0=xsrc, scalar1=rs[i], scalar2=None, op0=ALU.mult),
                  reads=[xkey, ("rs", i)], writes=[("xb", i)])
            pT = ps16[bank]
            for kc in range(8):
                P.add("pe", lambda e, kc=kc: e.transpose(out=pT[:, kc * 128:(kc + 1) * 128],
                                                         in_=xb[:, kc * 128:(kc + 1) * 128], identity=ident),
                      reads=[("xb", i), "const"], writes=[bk(bank)])
            g = gT[gkey]
            P.add("dve", lambda e: e.tensor_tensor(out=hT[:, :, st_i * 128:(st_i + 1) * 128],
                                                   in0=pT.rearrange("p (a b) -> p a b", b=128),
                                                   in1=g.unsqueeze(2).broadcast_to([128, 8, 128]), op=ALU.mult),
                  reads=[bk(bank), "const"], writes=hkeys)

        def fm_chunk(bank, wv, wkey, c, rhs_of_kc, nk, rkeys, ncols=T):
            for kc in range(nk):
                r_ = rhs_of_kc(kc)
                P.add("pe", lambda e, kc=kc, r_=r_: e.matmul(ps[bank][:, 0:ncols], lhsT=wv[:, kc, c * 128:(c + 1) * 128],
                                                             rhs=r_, start=(kc == 0), stop=(kc == nk - 1)),
                      reads=[wkey] + rkeys, writes=[bk(bank)])

        def tm_group(bank, wv, wkey, lhs_of_kc, nk, lkeys, ncols=512):
            for kc in range(nk):
                l_ = lhs_of_kc(kc)
                P.add("pe", lambda e, kc=kc, l_=l_: e.matmul(ps[bank][:, 0:ncols], lhsT=l_, rhs=wv[:, kc, 0:ncols],
                                                             start=(kc == 0), stop=(kc == nk - 1)),
                      reads=[wkey] + lkeys, writes=[bk(bank)])

        class RR:
            def __init__(self, ids):
                self.ids = list(ids)
                self.i = 0

            def next(self):
                b = self.ids[self.i % len(self.ids)]
                self.i += 1
                return b

        HK = [("hT", i) for i in range(NST)]

        def rotary_chunk(G, wv, wkey, c, perm, cosT, sinT, tabkey, Qb, qbkey, t1, t2, tkey, out_fn):
            b1 = G.next()
            fm_chunk(b1, wv, wkey, c, lambda kc: cx.hT[:, kc, :], 8, HK)
            P.add("act", lambda e: e.activation(out=Qb, in_=ps[b1], func=AF.Copy), reads=[bk(b1)], writes=[qbkey])
            P.add("dve", lambda e: e.tensor_tensor(out=t1, in0=ps[b1], in1=cosT, op=ALU.mult),
                  reads=[bk(b1), tabkey, qbkey], writes=[(tkey, 1)])
            b2 = G.next()
            P.add("pe", lambda e: e.matmul(ps[b2], lhsT=perm, rhs=Qb, start=True, stop=True),
                  reads=[qbkey, "const"], writes=[bk(b2)])
            P.add("dve", lambda e: e.tensor_tensor(out=t2, in0=ps[b2], in1=sinT, op=ALU.mult),
                  reads=[bk(b2), tabkey], writes=[(tkey, 2)])
            out_fn()

        mark0 = AR.off
        setup_common(3, False)
        mt = AR.alloc([2, D], F32)
        mT = AR.alloc([8, 256], BF16)
        dma("sp", mt, dr["mem"].rearrange("(a p) d -> p a d", p=128), [], ["mt"], "mt")
        G = RR([0, 1, 2, 3])
        for a in range(2):
            i = cx.nrm % 2
            cx.nrm += 1
            xb = cx.xb[i]
            src = mt[:, a, :]
            P.add("act", lambda e, src=src, i=i, junk=cx.junk: e.activation(out=junk, in_=src, func=AF.Square, accum_out=ss[i]),
                  reads=["mt"], writes=["junk", ("ss", i)])
            P.add("act", lambda e, i=i: e.activation(out=sd[i], in_=ss[i], func=AF.Sqrt, scale=1.0 / D, bias=eps1),
                  reads=[("ss", i), "const"], writes=[("sd", i)])
            P.add("dve", lambda e, i=i: e.reciprocal(out=rs[i], in_=sd[i]), reads=[("sd", i)], writes=[("rs", i)])
            P.add("dve", lambda e, src=src, i=i, xb=xb: e.tensor_scalar(out=xb, in0=src, scalar1=rs[i], scalar2=None,
                                                                      op0=ALU.mult),
                  reads=["mt", ("rs", i)], writes=[("xb", i)])
            b = G.next()
            pT = ps16[b]
            for kc in range(8):
                P.add("pe", lambda e, kc=kc, pT=pT, xb=xb: e.transpose(out=pT[:, kc * 128:(kc + 1) * 128],
                                                                    in_=xb[:, kc * 128:(kc + 1) * 128], identity=ident),
                      reads=[("xb", i), "const"], writes=[bk(b)])
            P.add("dve", lambda e, pT=pT, a=a: e.tensor_tensor(out=mT[:, :, a * 128:(a + 1) * 128],
                                                             in0=pT.rearrange("p (a b) -> p a b", b=128),
                                                             in1=gT["norm_mem"].unsqueeze(2).broadcast_to([128, 8, 128]),
                                                             op=ALU.mult),
                  reads=[bk(b), "const"], writes=["mT"])
        for g in range(2):
            wv, wk = load_w(dr["w_xkv"][:, g * 512:(g + 1) * 512], 8, 512)
            for c in range(4):
                b = G.next()
                fm_chunk(b, wv, wk, c, lambda kc: mT[:, kc, :], 8, ["mT"], ncols=256)
                P.add("act", lambda e, b=b, cc=g * 4 + c: e.activation(out=kxT[:, cc, :], in_=ps[b][:, 0:256], func=AF.Copy),
                      reads=[bk(b)], writes=["kxT"])
        for g in range(2):
            wv, wk = load_w(dr["w_xkv"][:, D + g * 512:D + (g + 1) * 512], 8, 512)
            for a in range(2):
                b = G.next()
                tm_group(b, wv, wk, lambda kc, a=a: mT[:, kc, a * 128:(a + 1) * 128], 8, ["mT"])
                P.add("act", lambda e, b=b, a=a, g=g: e.activation(out=vx[:, a, g * 512:(g + 1) * 512], in_=ps[b], func=AF.Copy),
                      reads=[bk(b)], writes=["vx"])
        P.barrier()
        AR.off = mark0

        def sweep0():
            setup_common(3, False)
            xs = [AR.alloc([D], F32) for _ in range(2)]
            Kc = AR.alloc([8, S], BF16)
            Vc = AR.alloc([32, 8, 65], BF16)
            Qa = AR.alloc([8, T], BF16)
            kmT = AR.alloc([8, 16], BF16)
            kmf = AR.alloc([8, 2], F32)
            cosA = AR.alloc([T], F32)
            sinA = AR.alloc([T], F32)
            Qb = [AR.alloc([T], BF16) for _ in range(2)]
            t1 = AR.alloc([T], F32)
            t2 = AR.alloc([T], F32)
            bsb = AR.alloc([8, 16], F32)
            top8 = AR.alloc([8, 8], F32)
            selb = AR.alloc([8, 16], F32)
            mb = AR.alloc([NST, 8, 16], BF16)
            PT = [AR.alloc([T], BF16) for _ in range(4)]
            rc = AR.alloc([T], F32)
            bcs = AR.alloc([T], F32)
            yaT = [AR.alloc([4, T], BF16) for _ in range(2)]
            for h in range(8):
                dma("sp", Kc[64:80, h, :], dr["c_onehot"], [], ["Kaux"], "const")
            P.add("pool", lambda e: e.memset(Vc[:, :, :, 64:65], 1.0), writes=["Vones"])
            P.add("pool", lambda e: e.memset(kmT[0:64], 0.0), writes=["kmT"])
            G = RR([0, 1, 2, 3])
            OB = RR([4, 5])
            pti = [0]
            for j in range(nt0):
                tok0 = j * T
                dma("sp", cosA, dr["c_cosA"][:, tok0:tok0 + T], [], ["tabA"], "tabA")
                dma("sp", sinA, dr["c_sinA"][:, tok0:tok0 + T], [], ["tabA"], "tabA")
                for s_ in range(NST):
                    xi = (j * NST + s_) % 2
                    dma("sp", xs[xi], dr["x"][tok0 + s_ * 128:tok0 + (s_ + 1) * 128, :], [], [("xs", xi)], "xs%d" % xi)
                    norm_T(xs[xi], ("xs", xi), "norm_mix", s_, G.next(), [HK[s_]])
                for which, col0 in (("q", C_AQ), ("k", C_AK)):
                    wv, wk = load_w(dr["w_in"][:, col0:col0 + 512], 8, 512)
                    for c in range(4):
                        qi = uid[0] % 2
                        uid[0] += 1

                        def comb(c=c, which=which):
                            for half in range(2):
                                h = 2 * c + half
                                if which == "q":
                                    dst = Qa[0:64, h, :]
                                    wr = [("Qa", h)]
                                else:
                                    dst = Kc[0:64, h, tok0:tok0 + T]
                                    wr = [("Kc", j, h)]
                                P.add("dve", lambda e, dst=dst, half=half: e.tensor_tensor(
                                    out=dst, in0=t1[half * 64:(half + 1) * 64, :], in1=t2[half * 64:(half + 1) * 64, :],
                                    op=ALU.add), reads=[("tA", 1), ("tA", 2)], writes=wr)

                        rotary_chunk(G, wv, wk, c, permA, cosA, sinA, "tabA", Qb[qi], ("Qb", qi), t1, t2, "tA", comb)
                wv, wk = load_w(dr["w_in"][:, C_AV:C_AV + 512], 8, 512)
                for s_ in range(NST):
                    b = G.next()
                    tm_group(b, wv, wk, lambda kc, s_=s_: cx.hT[:, kc, s_ * 128:(s_ + 1) * 128], 8, [HK[s_]])
                    kt = j * NST + s_
                    P.add("act", lambda e, b=b, kt=kt: e.activation(out=Vc[:, kt, :, 0:64],
                                                                    in_=ps[b].rearrange("p (h d) -> p h d", d=64),
                                                                    func=AF.Copy),
                          reads=[bk(b), "Vones"], writes=[("Vc", kt)])
                P.add("dve", lambda e, j=j: e.tensor_reduce(
                    out=kmf[0:64], in_=Kc[0:64, :, j * T:(j + 1) * T].rearrange("p h (n k) -> p h n k", k=256),
                    axis=AX.X, op=ALU.add), reads=[("Kc", j, h) for h in range(8)], writes=["kmf"])
                P.add("dve", lambda e, j=j: e.tensor_scalar(out=kmT[0:64, :, 2 * j:2 * j + 2], in0=kmf[0:64],
                                                            scalar1=1.0 / 256, scalar2=None, op0=ALU.mult),
                      reads=["kmf"], writes=["kmT"])
                BSB = 7
                for s_ in range(NST):
                    for h in range(8):
                        P.add("pe", lambda e, s_=s_, h=h: e.matmul(
                            ps[BSB][:, (s_ * 8 + h) * 16:(s_ * 8 + h + 1) * 16],
                            lhsT=Qa[0:64, h, s_ * 128:(s_ + 1) * 128], rhs=kmT[0:64, h, :], start=True, stop=True),
                            reads=[("Qa", h), "kmT"], writes=[bk(BSB)])
                for s_ in range(NST):
                    blk = (j * NST + s_) // 2
                    P.add("dve", lambda e, s_=s_, blk=blk: e.tensor_tensor(
                        out=bsb, in0=ps[BSB][:, s_ * 128:(s_ + 1) * 128].rearrange("p (h n) -> p h n", n=16),
                        in1=seltab[:, 0, blk, :].unsqueeze(1).broadcast_to([128, 8, 16]), op=ALU.add),
                        reads=[bk(BSB), "const"], writes=["bsb"])
                    for h in range(8):
                        P.add("dve", lambda e, h=h: e.max(out=top8[:, h, :], in_=bsb[:, h, :]),
                              reads=["bsb"], writes=["top8"])
                    P.add("dve", lambda e: e.tensor_tensor(out=selb, in0=bsb, in1=top8[:, :, 2:3].broadcast_to([128, 8, 16]),
                                                           op=ALU.is_ge), reads=["bsb", "top8"], writes=["selb"])
                    P.add("dve", lambda e, blk=blk: e.tensor_tensor(
                        out=selb, in0=selb, in1=seltab[:, 1, blk, :].unsqueeze(1).broadcast_to([128, 8, 16]), op=ALU.mult),
                        reads=["selb", "const"], writes=["selb"])
                    P.add("dve", lambda e, blk=blk: e.tensor_tensor(
                        out=selb, in0=selb, in1=seltab[:, 2, blk, :].unsqueeze(1).broadcast_to([128, 8, 16]), op=ALU.add),
                        reads=["selb", "const"], writes=["selb"])
                    P.add("dve", lambda e, s_=s_: e.tensor_scalar(out=mb[:, s_], in0=selb, scalar1=BIG, scalar2=-BIG,
                                                                  op0=ALU.mult, op1=ALU.add),
                          reads=["selb"], writes=["mb"])
                for h in range(8):
                    b = G.next()
                    for s_ in range(NST):
                        P.add("pe", lambda e, b=b, h=h, s_=s_: e.transpose(out=ps16[b][0:16, s_ * 128:(s_ + 1) * 128],
                                                                           in_=mb[:, s_, h, :], identity=ident),
                              reads=["mb", "const"], writes=[bk(b)])
                    P.add("act", lambda e, b=b, h=h: e.activation(out=Qa[64:80, h, :], in_=ps16[b][0:16, 0:T], func=AF.Copy),
                          reads=[bk(b)], writes=[("Qa", h)])
                yb_ = yaT[j % 2]
                nkt = (j + 1) * NST
                tasks = [(h, kt) for h in range(8) for kt in range(nkt)]
                LAG = 2
                obs = {}
                info = {}
                deferred = []
                BC = 6

                def emit_S(i, j=j, nkt=nkt):
                    h, kt = tasks[i]
                    if kt == 0:
                        obs[h] = OB.next()
                    r = kt - j * NST
                    c0 = 0 if r <= 0 else r * 128
                    sb_ = G.next()
                    P.add("pe", lambda e, sb_=sb_, kt=kt, c0=c0, h=h: e.matmul(
                        ps[sb_][:, c0:T], lhsT=Kc[0:80, h, kt * 128:(kt + 1) * 128], rhs=Qa[0:80, h, c0:T],
                        start=True, stop=True),
                        reads=[("Kc", kt // NST, h), "Kaux", ("Qa", h)], writes=[bk(sb_)])
                    pt = PT[pti[0] % 4]
                    ptk = ("PT", pti[0] % 4)
                    pti[0] += 1
                    P.add("act", lambda e, sb_=sb_, pt=pt, c0=c0: e.activation(out=pt[:, c0:T], in_=ps[sb_][:, c0:T],
                                                                                func=AF.Exp, scale=0.125),
                          reads=[bk(sb_)], writes=[ptk])
                    if r >= 0:
                        P.add("dve", lambda e, pt=pt, c0=c0: e.tensor_tensor(out=pt[:, c0:c0 + 128], in0=pt[:, c0:c0 + 128],
                                                                             in1=tri, op=ALU.mult),
                              reads=[ptk, "const"], writes=[ptk])
                    info[i] = (pt, ptk, c0)

                def norm_tail(h, ob, yb_=yb_, j=j):
                    P.add("pe", lambda e: e.matmul(ps[BC][0:64, :], lhsT=ones32[64:65, :], rhs=rc[64:65, :], start=True, stop=True),
                          reads=["rc", "const"], writes=[bk(BC)])
                    P.add("act", lambda e: e.activation(out=bcs[0:64, :], in_=ps[BC][0:64, :], func=AF.Copy),
                          reads=[bk(BC)], writes=["bcs"])
                    po = (h % 2) * 64
                    P.add("dve", lambda e, ob=ob, po=po, h=h, yb_=yb_: e.tensor_tensor(
                        out=yb_[po:po + 64, h // 2, :], in0=ps[ob][0:64, :], in1=bcs[0:64, :], op=ALU.mult),
                        reads=[bk(ob), "bcs"], writes=[("yaT", j % 2)])

                def emit_PV(i, nkt=nkt):
                    h, kt = tasks[i]
                    pt, ptk, c0 = info.pop(i)
                    ob = obs[h]
                    P.add("pe", lambda e, ob=ob, kt=kt, h=h, pt=pt, c0=c0, nkt=nkt: e.matmul(
                        ps[ob][0:65, c0:T], lhsT=Vc[:, kt, h, :], rhs=pt[:, c0:T],
                        start=(kt == 0), stop=(kt == nkt - 1), skip_group_check=True),
                        reads=[("Vc", kt), "Vones", ptk], writes=[bk(ob)])
                    if kt == nkt - 1:
                        P.add("dve", lambda e, ob=ob: e.reciprocal(out=rc[64:65, :], in_=ps[ob][64:65, :]),
                              reads=[bk(ob)], writes=["rc"])
                        deferred.append((i + LAG + 2, h, ob))

                ntask = len(tasks)
                for i in range(ntask + LAG + 3):
                    if i < ntask:
                        emit_S(i)
                    if 0 <= i - LAG < ntask:
                        emit_PV(i - LAG)
                    while deferred and deferred[0][0] <= i:
                        _, h_, ob_ = deferred.pop(0)
                        norm_tail(h_, ob_)
                assert not deferred and not info
                dma("sp", ya_s[:, tok0:tok0 + T].rearrange("(c p) t -> p c t", p=128), yb_,
                    [("yaT", j % 2)], [("yas", j)], "yaT%d" % (j % 2))
            P.barrier()
            AR.off = mark0

        if nt0 > 0:
            sweep0()

        def sweep1():
            setup_common(6, True)
            cosB = AR.alloc([T], F32)
            sinB = AR.alloc([T], F32)
            Qb = [AR.alloc([T], BF16) for _ in range(2)]
            t1 = AR.alloc([T], F32)
            t2 = AR.alloc([T], F32)
            qT = AR.alloc([4, T], BF16)
            kT = AR.alloc([4, T], BF16)
            ktok = AR.alloc([NST, 4, 128], BF16)
            vr = AR.alloc([NST, D], BF16)
            sil = AR.alloc([NST, D], BF16)
            Sst = AR.alloc([4, 256], F32)
            Sbf = AR.alloc([4, 256], BF16)
            attT = [AR.alloc([128], BF16) for _ in range(4)]
            st6 = AR.alloc([4, 6], F32)
            mv4 = AR.alloc([4, 2], F32)
            ve4 = AR.alloc([4], F32)
            rstd4 = AR.alloc([4], F32)
            ytmp = [AR.alloc([256], F32) for _ in range(2)]
            sgaA = AR.alloc([8, T], BF16)
            sgbA = AR.alloc([8, T], BF16)
            ybt = [AR.alloc([D], BF16) for _ in range(2)]
            ybT = AR.alloc([8, T], BF16)
            yaT1 = AR.alloc([4, T], BF16)
            mtmp = [AR.alloc([T], F32) for _ in range(2)]
            mrg = AR.alloc([8, T], BF16)
            P.add("pool", lambda e: e.memset(Sst, 0.0), writes=["S"])
            P.add("pool", lambda e: e.memset(Sbf, 0.0), writes=["Sbf"])
            G = RR([0, 1, 2, 3, 4, 5, 6, 7])
            ai = [0]
            for j in range(nt1):
                tok0 = j * T
                dma("sp", cosB, dr["c_cosB"][:, tok0:tok0 + T], [], ["tabB"], "tabB")
                dma("sp", sinB, dr["c_sinB"][:, tok0:tok0 + T], [], ["tabB"], "tabB")
                def xload1(jn):
                    bi = jn % 2
                    dma("sp", cx.xts[bi], dr["x"][jn * T:(jn + 1) * T, :].rearrange("(a p) d -> p a d", p=128), [],
                        [("xt", bi, s_) for s_ in range(NST)], "xt%d" % bi)

                if j == 0:
                    xload1(0)
                if j + 1 < nt1:
                    xload1(j + 1)
                xt = cx.xts[j % 2]
                XK = [("xt", j % 2, s_) for s_ in range(NST)]
                dma("sp", yaT1, ya_s[:, tok0:tok0 + T].rearrange("(c p) t -> p c t", p=128), [("yas", j)], ["yaT1"], "yaL")
                for s_ in range(NST):
                    norm_T(xt[:, s_, :], XK[s_], "norm_mix", s_, G.next(), [HK[s_]])
                for which, col0, dstT in (("q", C_RQ, qT), ("k", C_RK, kT)):
                    wv, wk = load_w(dr["w_in"][:, col0:col0 + 512], 8, 512)
                    for c in range(4):
                        qi = uid[0] % 2
                        uid[0] += 1

                        def comb(c=c, dstT=dstT, which=which):
                            P.add("dve", lambda e: e.tensor_tensor(out=dstT[:, c, :], in0=t1, in1=t2, op=ALU.add),
                                  reads=[("tB", 1), ("tB", 2)], writes=[(which + "T", c)])

                        rotary_chunk(G, wv, wk, c, permB, cosB, sinB, "tabB", Qb[qi], ("Qb", qi), t1, t2, "tB", comb)
                for g in range(2):
                    wv, wk = load_w(dr["w_in"][:, C_RV + g * 512:C_RV + (g + 1) * 512], 8, 512)
                    for s_ in range(NST):
                        b = G.next()
                        tm_group(b, wv, wk, lambda kc, s_=s_: cx.hT[:, kc, s_ * 128:(s_ + 1) * 128], 8, [HK[s_]])
                        P.add("act", lambda e, b=b, s_=s_, g=g: e.activation(out=vr[:, s_, g * 512:(g + 1) * 512], in_=ps[b],
                                                                             func=AF.Copy),
                              reads=[bk(b)], writes=[("vr", s_)])
                for g in range(2):
                    wv, wk = load_w(dr["w_in"][:, C_RG + g * 512:C_RG + (g + 1) * 512], 8, 512)
                    for s_ in range(NST):
                        b = G.next()
                        tm_group(b, wv, wk, lambda kc, s_=s_: cx.hT[:, kc, s_ * 128:(s_ + 1) * 128], 8, [HK[s_]])
                        P.add("act", lambda e, b=b, s_=s_, g=g: e.activation(out=sil[:, s_, g * 512:(g + 1) * 512], in_=ps[b],
                                                                             func=AF.Silu),
                              reads=[bk(b)], writes=[("sil", s_)])
                def gate_group(which, g):
                    col0 = (C_GA if which == "a" else C_GB) + g * 512
                    wv_, wk_ = load_w(dr["w_in"][:, col0:col0 + 512], 8, 512)
                    dstA = sgaA if which == "a" else sgbA
                    for c in range(4):
                        m = g * 4 + c
                        b = G.next()
                        fm_chunk(b, wv_, wk_, c, lambda kc: cx.hT[:, kc, :], 8, HK)
                        P.add("act", lambda e, b=b, m=m, dstA=dstA: e.activation(out=dstA[:, m, :], in_=ps[b], func=AF.Sigmoid),
                              reads=[bk(b)], writes=[("sg" + which, m)])

                gate_plan = [("a", 0), ("a", 1), ("b", 0), ("b", 1)]
                for s_ in range(NST):
                    cs = slice(s_ * 128, (s_ + 1) * 128)
                    b = G.next()
                    for h in range(4):
                        P.add("pe", lambda e, b=b, h=h, cs=cs: e.transpose(out=ps16[b][:, h * 128:(h + 1) * 128],
                                                                           in_=kT[:, h, cs], identity=ident),
                              reads=[("kT", h), "const"], writes=[bk(b)])
                    P.add("dve", lambda e, b=b, s_=s_: e.tensor_tensor(
                        out=ktok[:, s_], in0=ps16[b][:, 0:512].rearrange("p (h d) -> p h d", d=128),
                        in1=kdec.unsqueeze(2).broadcast_to([128, 4, 128]), op=ALU.mult),
                        reads=[bk(b), "const"], writes=[("ktok", s_)])
                    yb_ = ybt[s_ % 2]
                    ybk = ("ybt", s_ % 2)
                    bas = []
                    for h in range(4):
                        ba = G.next()
                        bas.append(ba)
                        P.add("pe", lambda e, ba=ba, h=h, cs=cs: e.matmul(ps[ba][:, 0:128], lhsT=kT[:, h, cs], rhs=qT[:, h, cs],
                                                                          start=True, stop=True),
                              reads=[("kT", h), ("qT", h)], writes=[bk(ba)])
                    ats = []
                    for h in range(4):
                        at = attT[h]
                        atk = ("attT", h)
                        ats.append((at, atk))
                        P.add("dve", lambda e, ba=bas[h], at=at, h=h: e.tensor_tensor(out=at, in0=ps[ba][:, 0:128], in1=M2[:, h, :],
                                                                                      op=ALU.mult),
                              reads=[bk(bas[h]), "const"], writes=[atk])
                    bys = []
                    for h in range(4):
                        hs = slice(h * 256, (h + 1) * 256)
                        at, atk = ats[h]
                        by = G.next()
                        bys.append(by)
                        P.add("pe", lambda e, by=by, at=at, s_=s_, hs=hs: e.matmul(ps[by][:, 0:256], lhsT=at, rhs=vr[:, s_, hs],
                                                                                   start=True, stop=False),
                              reads=[atk, ("vr", s_)], writes=[bk(by)])
                        P.add("pe", lambda e, by=by, h=h, cs=cs: e.matmul(ps[by][:, 0:256], lhsT=qT[:, h, cs], rhs=Sbf[:, h, :],
                                                                          start=False, stop=True),
                              reads=[("qT", h), ("Sbf", h)], writes=[bk(by)])
                        P.add("pe", lambda e, by=by, h=h, s_=s_, hs=hs: e.matmul(ps[by][:, 256:512], lhsT=ktok[:, s_, h, :],
                                                                                 rhs=vr[:, s_, hs], start=True, stop=True),
                              reads=[("ktok", s_), ("vr", s_)], writes=[bk(by)])
                    for h in range(4):
                        by = bys[h]
                        P.add("dve", lambda e, by=by, h=h: e.scalar_tensor_tensor(
                            out=Sst[:, h, :], in0=Sst[:, h, :], scalar=gamC[h], in1=ps[by][:, 256:512], op0=ALU.mult, op1=ALU.add),
                            reads=[bk(by), ("S", h)], writes=[("S", h)])
                        P.add("act", lambda e, h=h: e.activation(out=Sbf[:, h, :], in_=Sst[:, h, :], func=AF.Copy),
                              reads=[("S", h)], writes=[("Sbf", h)])
                    gate_group(*gate_plan[s_])
                    for h in range(4):
                        by = bys[h]
                        P.add("dve", lambda e, by=by, h=h: e.bn_stats(out=st6[:, h, :], in_=ps[by][:, 0:256]),
                              reads=[bk(by)], writes=[("st6", h)])
                        P.add("dve", lambda e, h=h: e.bn_aggr(out=mv4[:, h, :], in_=st6[:, h, :]), reads=[("st6", h)], writes=["mv4"])
                    P.add("dve", lambda e: e.tensor_tensor(out=ve4, in0=mv4[:, :, 1], in1=epsq, op=ALU.add),
                          reads=["mv4", "const"], writes=["ve4"])
                    P.add("act", lambda e: e.activation(out=rstd4, in_=ve4, func=AF.Sqrt), reads=["ve4"], writes=["rstd4"])
                    P.add("dve", lambda e: e.reciprocal(out=rstd4, in_=rstd4), reads=["rstd4"], writes=["rstd4"])
                    for h in range(4):
                        by = bys[h]
                        hs = slice(h * 256, (h + 1) * 256)
                        yt = ytmp[h % 2]
                        ytk = ("ytmp", h % 2)
                        P.add("dve", lambda e, by=by, s_=s_, hs=hs, h=h, yt=yt: e.scalar_tensor_tensor(
                            out=yt, in0=ps[by][:, 0:256], scalar=mv4[:, h, 0:1], in1=sil[:, s_, hs], op0=ALU.subtract, op1=ALU.mult),
                            reads=[bk(by), "mv4", ("sil", s_)], writes=[ytk])
                        P.add("dve", lambda e, yb_=yb_, hs=hs, h=h, yt=yt: e.tensor_scalar(out=yb_[:, hs], in0=yt, scalar1=rstd4[:, h:h + 1],
                                                                                         scalar2=None, op0=ALU.mult),
                              reads=[ytk, "rstd4"], writes=[ybk])
                    b = G.next()
                    for kc in range(8):
                        P.add("pe", lambda e, b=b, kc=kc, yb_=yb_: e.transpose(out=ps16[b][:, kc * 128:(kc + 1) * 128],
                                                                               in_=yb_[:, kc * 128:(kc + 1) * 128], identity=ident),
                              reads=[ybk, "const"], writes=[bk(b)])
                    P.add("act", lambda e, b=b, cs=cs: e.activation(out=ybT[:, :, cs], in_=ps16[b].rearrange("p (a b) -> p a b", b=128),
                                                                    func=AF.Copy),
                          reads=[bk(b)], writes=[("ybT", s_)])
                YBK = [("ybT", s_) for s_ in range(NST)]
                for g in range(2):
                    wA, kA = load_w(dr["w_branch_a"][:, g * 512:(g + 1) * 512], 4, 512)
                    wB, kB = load_w(dr["w_branch_b"][:, g * 512:(g + 1) * 512], 8, 512)
                    for c in range(4):
                        m = g * 4 + c
                        i2 = m % 2
                        b = G.next()
                        fm_chunk(b, wA, kA, c, lambda kc: yaT1[:, kc, :], 4, ["yaT1"])
                        P.add("dve", lambda e, b=b, i2=i2, m=m: e.tensor_tensor(out=mtmp[i2], in0=ps[b], in1=sgaA[:, m, :], op=ALU.mult),
                              reads=[bk(b), ("sga", m)], writes=[("mtmp", i2)])
                        b = G.next()
                        fm_chunk(b, wB, kB, c, lambda kc: ybT[:, kc, :], 8, YBK)
                        P.add("dve", lambda e, b=b, m=m: e.tensor_tensor(out=sgbA[:, m, :], in0=ps[b], in1=sgbA[:, m, :], op=ALU.mult),
                              reads=[bk(b), ("sgb", m)], writes=[("sgb", m)])
                        P.add("dve", lambda e, m=m, i2=i2: e.tensor_tensor(out=mrg[:, m, :], in0=mtmp[i2], in1=sgbA[:, m, :], op=ALU.add),
                              reads=[("mtmp", i2), ("sgb", m)], writes=[("mrg", m)])
                MK = [("mrg", mm) for mm in range(8)]
                for g in range(2):
                    wv, wk = load_w(dr["w_out"][:, g * 512:(g + 1) * 512], 8, 512)
                    for s_ in range(NST):
                        b = G.next()
                        tm_group(b, wv, wk, lambda kc, s_=s_: mrg[:, kc, s_ * 128:(s_ + 1) * 128], 8, MK)
                        P.add("dve", lambda e, b=b, s_=s_, g=g, xt=xt: e.tensor_tensor(out=xt[:, s_, g * 512:(g + 1) * 512],
                                                                                       in0=ps[b], in1=xt[:, s_, g * 512:(g + 1) * 512], op=ALU.add),
                              reads=[bk(b), XK[s_]], writes=[XK[s_]])
                dma("sp", x1_s[tok0:tok0 + T, :].rearrange("(a p) d -> p a d", p=128), xt,
                    list(XK), [("x1s", j)], "xto%d" % (j % 2))
            P.barrier()
            AR.off = mark0

        if nt1 > 0:
            sweep1()

        def sweep2():
            setup_common(6, True)
            qxT = AR.alloc([8, T], BF16)
            PTx = [AR.alloc([T], BF16) for _ in range(4)]
            rcs = [AR.alloc([T], F32) for _ in range(2)]
            oT = AR.alloc([8, T], BF16)
            sgt = [AR.alloc([T], BF16) for _ in range(3)]
            aT = AR.alloc([NFC, T], BF16)
            ot = [AR.alloc([D], F32) for _ in range(2)]
            G = RR([0, 1, 2, 3])
            pi = [0]
            gi = [0]
            for j in range(nt2):
                tok0 = j * T
                src = x1_s if nt1 > 0 else dr["x"]

                def xload2(jn):
                    bi = jn % 2
                    dma("sp", cx.xts[bi], src[jn * T:(jn + 1) * T, :].rearrange("(a p) d -> p a d", p=128), [("x1s", jn)],
                        [("xt", bi, s_) for s_ in range(NST)], "xt%d" % bi)

                if j == 0:
                    xload2(0)
                if j + 1 < nt2:
                    xload2(j + 1)
                xt = cx.xts[j % 2]
                XK = [("xt", j % 2, s_) for s_ in range(NST)]
                for s_ in range(NST):
                    norm_T(xt[:, s_, :], XK[s_], "norm_cross", s_, G.next(), [HK[s_]])
                for g in range(2):
                    wv, wk = load_w(dr["w_xq"][:, g * 512:(g + 1) * 512], 8, 512)
                    for c in range(4):
                        b = G.next()
                        m = g * 4 + c
                        fm_chunk(b, wv, wk, c, lambda kc: cx.hT[:, kc, :], 8, HK)
                        P.add("act", lambda e, b=b, m=m: e.activation(out=qxT[:, m, :], in_=ps[b], func=AF.Copy),
                              reads=[bk(b)], writes=[("qxT", m)])
                for h in range(4):
                    pts = []
                    for mc in range(2):
                        b = G.next()
                        for dc in range(2):
                            P.add("pe", lambda e, b=b, h=h, mc=mc, dc=dc: e.matmul(
                                ps[b], lhsT=kxT[:, 2 * h + dc, mc * 128:(mc + 1) * 128], rhs=qxT[:, 2 * h + dc, :],
                                start=(dc == 0), stop=(dc == 1)),
                                reads=["kxT", ("qxT", 2 * h + dc)], writes=[bk(b)])
                        pt = PTx[pi[0] % 4]
                        ptk = ("PTx", pi[0] % 4)
                        pi[0] += 1
                        P.add("act", lambda e, b=b, pt=pt: e.activation(out=pt, in_=ps[b], func=AF.Exp, scale=1.0 / 16),
                              reads=[bk(b)], writes=[ptk])
                        pts.append((pt, ptk))
                    b = G.next()
                    for mc in range(2):
                        P.add("pe", lambda e, b=b, mc=mc, pt=pts[mc][0]: e.matmul(ps[b], lhsT=onesb, rhs=pt, start=(mc == 0), stop=(mc == 1)),
                              reads=[pts[mc][1], "const"], writes=[bk(b)])
                    rcv = rcs[h % 2]
                    P.add("dve", lambda e, b=b, rcv=rcv: e.reciprocal(out=rcv, in_=ps[b]), reads=[bk(b)], writes=[("rcs", h % 2)])
                    for ec in range(2):
                        b = G.next()
                        for mc in range(2):
                            P.add("pe", lambda e, b=b, h=h, ec=ec, mc=mc, pt=pts[mc][0]: e.matmul(
                                ps[b], lhsT=vx[:, mc, h * 256 + ec * 128:h * 256 + (ec + 1) * 128], rhs=pt,
                                start=(mc == 0), stop=(mc == 1)),
                                reads=[pts[mc][1], "vx"], writes=[bk(b)])
                        mo = 2 * h + ec
                        P.add("dve", lambda e, b=b, rcv=rcv, mo=mo: e.tensor_tensor(out=oT[:, mo, :], in0=ps[b], in1=rcv, op=ALU.mult),
                              reads=[bk(b), ("rcs", h % 2)], writes=[("oT", mo)])
                OK_ = [("oT", mm) for mm in range(8)]
                for g in range(2):
                    wv, wk = load_w(dr["w_xo"][:, g * 512:(g + 1) * 512], 8, 512)
                    for s_ in range(NST):
                        b = G.next()
                        tm_group(b, wv, wk, lambda kc, s_=s_: oT[:, kc, s_ * 128:(s_ + 1) * 128], 8, OK_)
                        P.add("dve", lambda e, b=b, s_=s_, g=g, xt=xt: e.tensor_tensor(out=xt[:, s_, g * 512:(g + 1) * 512],
                                                                                       in0=ps[b], in1=xt[:, s_, g * 512:(g + 1) * 512], op=ALU.add),
                              reads=[bk(b), XK[s_]], writes=[XK[s_]])
                for s_ in range(NST):
                    norm_T(xt[:, s_, :], XK[s_], "norm_ffn", s_, G.next(), [HK[s_]])
                for fg in range(6):
                    ncol = 512 if fg < 5 else 256
                    wg, kg = load_w(dr["w_gate"][:, fg * 512:fg * 512 + ncol], 8, ncol)
                    wu, ku = load_w(dr["w_up"][:, fg * 512:fg * 512 + ncol], 8, ncol)
                    for c in range(ncol // 128):
                        fc = fg * 4 + c
                        bg = G.next()
                        fm_chunk(bg, wg, kg, c, lambda kc: cx.hT[:, kc, :], 8, HK)
                        sg_ = sgt[gi[0] % 3]
                        sgk = ("sgt", gi[0] % 3)
                        gi[0] += 1
                        P.add("act", lambda e, bg=bg, sg_=sg_: e.activation(out=sg_, in_=ps[bg], func=AF.Silu),
                              reads=[bk(bg)], writes=[sgk])
                        bu = G.next()
                        fm_chunk(bu, wu, ku, c, lambda kc: cx.hT[:, kc, :], 8, HK)
                        P.add("dve", lambda e, bu=bu, sg_=sg_, fc=fc: e.tensor_tensor(out=aT[:, fc, :], in0=ps[bu], in1=sg_, op=ALU.mult),
                              reads=[bk(bu), sgk], writes=[("aT", fc)])
                pieces = [(0, 8), (8, 8), (16, 6)]
                for g in range(2):
                    for (f0, nf) in pieces:
                        wv, wk = load_w(dr["w_down"][f0 * 128:(f0 + nf) * 128, g * 512:(g + 1) * 512], nf, 512)
                        for s_ in range(NST):
                            for fl in range(nf):
                                fc = f0 + fl
                                P.add("pe", lambda e, s_=s_, fl=fl, fc=fc, wv=wv: e.matmul(
                                    ps[4 + s_], lhsT=aT[:, fc, s_ * 128:(s_ + 1) * 128], rhs=wv[:, fl, :],
                                    start=(fc == 0), stop=(fc == NFC - 1), skip_group_check=True),
                                    reads=[wk, ("aT", fc)], writes=[bk(4 + s_)])
                    for s_ in range(NST):
                        P.add("dve", lambda e, s_=s_, g=g, xt=xt: e.tensor_tensor(out=xt[:, s_, g * 512:(g + 1) * 512], in0=ps[4 + s_],
                                                                                  in1=xt[:, s_, g * 512:(g + 1) * 512], op=ALU.add),
                              reads=[bk(4 + s_), XK[s_]], writes=[XK[s_]])
                for s_ in range(NST):
                    i = cx.nrm % 2
                    cx.nrm += 1
                    o_ = ot[s_ % 2]
                    ok_ = ("ot", s_ % 2)
                    src = xt[:, s_, :]
                    P.add("act", lambda e, src=src, i=i, junk=cx.junk: e.activation(out=junk, in_=src, func=AF.Square, accum_out=ss[i]),
                          reads=[XK[s_]], writes=["junk", ("ss", i)])
                    P.add("act", lambda e, i=i: e.activation(out=sd[i], in_=ss[i], func=AF.Sqrt, scale=1.0 / D, bias=eps1),
                          reads=[("ss", i), "const"], writes=[("sd", i)])
                    P.add("dve", lambda e, i=i: e.reciprocal(out=rs[i], in_=sd[i]), reads=[("sd", i)], writes=[("rs", i)])
                    P.add("dve", lambda e, src=src, i=i, o_=o_: e.scalar_tensor_tensor(out=o_, in0=src, scalar=rs[i], in1=gfin,
                                                                                     op0=ALU.mult, op1=ALU.mult),
                          reads=[XK[s_], ("rs", i), "const"], writes=[ok_])
                    dma("sp", out_d[tok0 + s_ * 128:tok0 + (s_ + 1) * 128, :], o_, [ok_], [], "ot%d" % (s_ % 2))
        if nt2 > 0:
            sweep2()
        P.emit()
        nc._n_ops = P.nops
    return nc


_NC_CACHE = {}


def kernel(**inputs):
    consts, _ = make_consts()
    if "nc" not in _NC_CACHE:
        _NC_CACHE["nc"] = build()
    nc = _NC_CACHE["nc"]
    x = np.ascontiguousarray(np.asarray(inputs["x"], dtype=np.float32))
    mem = np.ascontiguousarray(np.asarray(inputs["mem"], dtype=np.float32))
    shared = {}
    for name, shp in W_SPECS:
        shared[name] = np.ascontiguousarray(np.asarray(inputs[name], dtype=np.float32).reshape(shp))
    shared.update(consts)
    in_maps = []
    for b in range(8):
        m = dict(shared)
        m["x"] = x[b]
        m["mem"] = mem[b]
        in_maps.append(m)
    res = run_bass_kernel_spmd(nc, in_maps, core_ids=list(range(8)))
    return np.stack([np.asarray(r["out"], dtype=np.float32) for r in res.results], axis=0)
```

```python
import contextlib
import numpy as np
import ml_dtypes
import concourse.bass as bass
import concourse.mybir as mybir
from concourse.bass_utils import run_bass_kernel_spmd

F32 = mybir.dt.float32
BF16 = mybir.dt.bfloat16
AF = mybir.ActivationFunctionType
ALU = mybir.AluOpType
AX = mybir.AxisListType

S = 4096
D = 1024
T = 512
NT = S // T
NST = T // 128
DFF = 2816
NFC = DFF // 128
BIG = 30000.0
EPS = 1e-6
ALLQ = ("sp", "pe", "act", "dve", "pool")

C_AQ, C_AK, C_AV, C_RQ, C_RK, C_RV, C_RG, C_GA, C_GB = 0, 512, 1024, 1536, 2048, 2560, 3584, 4608, 5632


class Op:
    __slots__ = ("eng", "fn", "deps", "sem", "val", "signal", "is_dma", "idx")


class Prog:
    def __init__(self, nc):
        self.nc = nc
        self.q = {e: [] for e in ALLQ}
        self.lastw = {}
        self.readers = {}
        self.nops = 0
        self.last_on = {}

    limit = None
    skip = None

    def add(self, eng, fn, reads=(), writes=(), dma_sem=None):
        if Prog.limit is not None and self.nops >= Prog.limit:
            return None
        if Prog.skip and self.nops in Prog.skip:
            self.nops += 1
            return None
        is_dma = dma_sem is not None
        op = Op()
        op.eng, op.fn, op.is_dma = eng, fn, is_dma
        op.sem = dma_sem if is_dma else eng
        op.val = None
        op.signal = is_dma
        op.deps = []
        op.idx = self.nops
        self.nops += 1
        deps = []
        for k in reads:
            w = self.lastw.get(k)
            if w is not None:
                deps.append(w)
        for k in writes:
            w = self.lastw.get(k)
            if w is not None:
                deps.append(w)
            deps.extend(self.readers.get(k, ()))
        seen = set()
        for d in deps:
            if d is op or id(d) in seen:
                continue
            seen.add(id(d))
            if (not d.is_dma) and (not is_dma) and d.eng == "pe" and eng == "pe":
                continue
            op.deps.append(d)
            d.signal = True
        for k in writes:
            self.lastw[k] = op
            self.readers[k] = []
        for k in reads:
            self.readers.setdefault(k, []).append(op)
        self.q[eng].append(op)
        self.last_on[op.sem] = op
        return op

    def barrier(self):
        lasts = list(self.last_on.values())
        for o in lasts:
            o.signal = True
        for e in ALLQ:
            op = Op()
            op.eng, op.fn, op.is_dma = e, None, False
            op.sem, op.val, op.signal = e, None, False
            op.deps = list(lasts)
            op.idx = self.nops
            self.nops += 1
            self.q[e].append(op)
        self.lastw = {}
        self.readers = {}

    def emit(self):
        nc = self.nc
        cnt = {}
        allops = []
        for e in ALLQ:
            allops.extend(self.q[e])
        allops.sort(key=lambda o: o.idx)
        for op in allops:
            if op.signal and op.fn is not None:
                inc = 16 if op.is_dma else 1
                cnt[op.sem] = cnt.get(op.sem, 0) + inc
                op.val = cnt[op.sem]
        semnames = sorted(cnt.keys())
        sems = {}
        with contextlib.ExitStack() as st:
            for s in semnames:
                sems[s] = st.enter_context(nc.semaphore("s_" + s))
            block = st.enter_context(nc.Block())

            def run(engname, eng):
                waited = {}
                for op in self.q[engname]:
                    need = {}
                    for d in op.deps:
                        if d.val is None:
                            continue
                        if d.val > need.get(d.sem, 0):
                            need[d.sem] = d.val
                    for s, v in need.items():
                        if waited.get(s, 0) >= v:
                            continue
                        eng.wait_ge(sems[s], v)
                        waited[s] = v
                    if op.fn is None:
                        continue
                    ins = op.fn(eng)
                    if op.signal:
                        ins.then_inc(sems[op.sem], 16 if op.is_dma else 1)
                if engname == "sp":
                    for s in semnames:
                        if waited.get(s, 0) < cnt[s]:
                            eng.wait_ge(sems[s], cnt[s])

            block.sync(lambda e: run("sp", e))
            block.tensor(lambda e: run("pe", e))
            block.scalar(lambda e: run("act", e))
            block.vector(lambda e: run("dve", e))
            block.gpsimd(lambda e: run("pool", e))


class Arena:
    def __init__(self, nc, st, nbytes):
        self.nbytes = nbytes
        self.t16 = st.enter_context(nc.sbuf_tensor("arena", [128, nbytes // 2], BF16))
        self.t32 = self.t16.bitcast(F32)
        self.off = 0

    def alloc(self, shape, dt):
        n = int(np.prod(shape))
        nb = n * (4 if dt == F32 else 2)
        o = self.off
        self.off += (nb + 63) // 64 * 64
        assert self.off <= self.nbytes, ("SBUF arena overflow", self.off, self.nbytes)
        v = self.t32[:, o // 4:o // 4 + n] if dt == F32 else self.t16[:, o // 2:o // 2 + n]
        if len(shape) == 2:
            v = v.rearrange("p (a b) -> p a b", b=shape[1])
        elif len(shape) == 3:
            v = v.rearrange("p (a b c) -> p a b c", b=shape[1], c=shape[2])
        return v


def make_consts():
    bf = ml_dtypes.bfloat16
    c = {}
    c["c_ident"] = np.eye(128, dtype=np.float32).astype(bf)
    pa = np.zeros((128, 128), np.float32)
    pb = np.zeros((128, 128), np.float32)
    for m in range(128):
        pa[(m + 32) if (m % 64) < 32 else (m - 32), m] = 1.0
        pb[(m + 64) if m < 64 else (m - 64), m] = 1.0
    c["c_permA"] = pa.astype(bf)
    c["c_permB"] = pb.astype(bf)
    pos = np.arange(S, dtype=np.float32)
    invA = (10000.0 ** (-np.arange(0, 64, 2, dtype=np.float32) / 64)).astype(np.float32)
    invB = (10000.0 ** (-np.arange(0, 128, 2, dtype=np.float32) / 128)).astype(np.float32)
    angA = (pos[None, :] * invA[:, None]).astype(np.float32)
    angB = (pos[None, :] * invB[:, None]).astype(np.float32)
    p = np.arange(128)
    c["c_cosA"] = np.cos(angA)[p % 32].astype(np.float32)
    c["c_sinA"] = (np.sin(angA)[p % 32] * np.where((p % 64) < 32, -1.0, 1.0)[:, None]).astype(np.float32)
    c["c_cosB"] = np.cos(angB)[p % 64].astype(np.float32)
    c["c_sinB"] = (np.sin(angB)[p % 64] * np.where(p < 64, -1.0, 1.0)[:, None]).astype(np.float32)
    c["c_tri"] = (p[:, None] <= p[None, :]).astype(np.float32).astype(bf)
    gam = 1.0 - 2.0 ** (-5.0 - np.arange(4, dtype=np.float64))
    sc = 128.0 ** -0.5
    j = np.arange(128, dtype=np.float64)
    m2 = np.zeros((128, 4, 128), np.float64)
    for h in range(4):
        m2[:, h, :] = (sc * gam[h] ** (-(j + 1.0)))[:, None] * (j[:, None] <= j[None, :])
    c["c_M2"] = m2.astype(np.float32)
    c["c_kdec"] = (sc * gam[None, :] ** (127.0 - j[:, None])).astype(np.float32)
    c["c_epsq"] = (EPS / gam[None, :] ** (2.0 * (j[:, None] + 1.0))).astype(np.float32)
    n = np.arange(16)
    sel = np.zeros((3, 16, 16), np.float32)
    sel[0] = np.where(n[None, :] >= n[:, None], -BIG, 0.0)
    sel[1] = (n[None, :] < n[:, None]).astype(np.float32)
    sel[2] = (n[None, :] == n[:, None]).astype(np.float32)
    c["c_sel"] = sel
    c["c_onehot"] = (np.arange(S)[None, :] // 256 == n[:, None]).astype(np.float32).astype(bf)
    return c, gam


CONST_SPECS = [("c_ident", [128, 128], BF16), ("c_permA", [128, 128], BF16), ("c_permB", [128, 128], BF16),
               ("c_cosA", [128, S], F32), ("c_sinA", [128, S], F32), ("c_cosB", [128, S], F32),
               ("c_sinB", [128, S], F32), ("c_tri", [128, 128], BF16), ("c_M2", [128, 4, 128], F32),
               ("c_kdec", [128, 4], F32), ("c_epsq", [128, 4], F32), ("c_sel", [3, 16, 16], F32),
               ("c_onehot", [16, S], BF16)]

W_SPECS = [("norm_mix", [D]), ("w_in", [D, 6656]), ("w_branch_a", [512, D]), ("w_branch_b", [D, D]),
           ("w_out", [D, D]), ("norm_cross", [D]), ("norm_mem", [D]), ("w_xq", [D, D]), ("w_xkv", [D, 2 * D]),
           ("w_xo", [D, D]), ("norm_ffn", [D]), ("w_gate", [D, DFF]), ("w_up", [D, DFF]), ("w_down", [DFF, D]),
           ("norm_final", [D])]


def build(nt0=NT, nt1=NT, nt2=NT, dbg=False):
    _, gam = make_consts()
    gamC = [float(g ** 128.0) for g in gam]
    nc = bass.Bass("TRN2", target_bir_lowering=False)
    dr = {}
    dr["x"] = nc.dram_tensor("x", [S, D], F32, kind="ExternalInput").ap()
    dr["mem"] = nc.dram_tensor("mem", [256, D], F32, kind="ExternalInput").ap()
    for name, shp in W_SPECS:
        dr[name] = nc.dram_tensor(name, shp, F32, kind="ExternalInput").ap()
    for name, shp, dt in CONST_SPECS:
        dr[name] = nc.dram_tensor(name, shp, dt, kind="ExternalInput").ap()
    out_d = nc.dram_tensor("out", [S, D], F32, kind="ExternalOutput").ap()
    skind = "ExternalOutput" if dbg else "Internal"
    ya_s = nc.dram_tensor("ya_s", [512, S], BF16, kind=skind).ap()
    x1_s = nc.dram_tensor("x1_s", [S, D], F32, kind=skind).ap()

    with contextlib.ExitStack() as st:
        st.enter_context(nc.allow_low_precision(reason="bf16 matmul operands by design (fp32 accumulate)"))
        st.enter_context(nc.allow_non_contiguous_dma(reason="tiny norm-gain transposes / strided weight blocks"))
        AR = Arena(nc, st, 206 * 1024)
        ps_t = [st.enter_context(nc.psum_tensor("ps%d" % i, [128, 512], F32)) for i in range(8)]
        ps = [p_[:, :] for p_ in ps_t]
        ps16 = [p_.bitcast(BF16)[:, :] for p_ in ps_t]
        P = Prog(nc)
        uid = [0]

        def dma(q, out, in_, reads, writes, sem):
            P.add(q, lambda e: e.dma_start(out=out, in_=in_), reads=reads, writes=writes, dma_sem=sem)

        def bk(b):
            return ("ps", b)

        ident = AR.alloc([128], BF16)
        permA = AR.alloc([128], BF16)
        permB = AR.alloc([128], BF16)
        tri = AR.alloc([128], BF16)
        onesb = AR.alloc([128], BF16)
        ones32 = AR.alloc([64], F32)
        M2 = AR.alloc([4, 128], F32)
        kdec = AR.alloc([4], F32)
        epsq = AR.alloc([4], F32)
        seltab = AR.alloc([3, 16, 16], F32)
        eps1 = AR.alloc([1], F32)
        gT = {k: AR.alloc([8], F32) for k in ("norm_mix", "norm_cross", "norm_mem", "norm_ffn")}
        gfin = AR.alloc([D], F32)
        kxT = AR.alloc([8, 256], BF16)
        vx = AR.alloc([2, D], BF16)
        ss = [AR.alloc([1], F32) for _ in range(2)]
        sd = [AR.alloc([1], F32) for _ in range(2)]
        rs = [AR.alloc([1], F32) for _ in range(2)]
        for nm, buf in (("c_ident", ident), ("c_permA", permA), ("c_permB", permB), ("c_tri", tri),
                        ("c_M2", M2), ("c_kdec", kdec), ("c_epsq", epsq)):
            dma("sp", buf, dr[nm], [], ["const"], "const")
        dma("sp", seltab.rearrange("p a b c -> p (a b c)"),
            dr["c_sel"].rearrange("a b c -> (a b c)").partition_broadcast(128), [], ["const"], "const")
        for k in gT:
            dma("sp", gT[k], dr[k].rearrange("(c p) -> p c", p=128), [], ["const"], "const")
        dma("sp", gfin, dr["norm_final"].partition_broadcast(128), [], ["const"], "const")
        P.add("pool", lambda e: e.memset(onesb, 1.0), writes=["const"])
        P.add("pool", lambda e: e.memset(ones32, 1.0), writes=["const"])
        P.add("pool", lambda e: e.memset(eps1, EPS), writes=["const"])
        persist_mark = AR.off

        class Ctx:
            pass

        cx = Ctx()

        def setup_common(nw, xt_full):
            cx.wslots = [AR.alloc([8, 512], BF16) for _ in range(nw)]
            cx.wi = 0
            cx.nw = nw
            cx.xb = [AR.alloc([D], BF16) for _ in range(2)]
            cx.junk = AR.alloc([D], BF16)
            cx.hT = AR.alloc([8, T], BF16)
            cx.xts = [AR.alloc([NST, D], F32) for _ in range(2)] if xt_full else None
            cx.xt = cx.xts[0] if xt_full else None
            cx.nrm = 0

        def load_w(src, kcs, n):
            s = cx.wi % cx.nw
            cx.wi += 1
            dst = cx.wslots[s][:, 0:kcs, 0:n]
            dma("pool", dst, src.rearrange("(kc p) n -> p kc n", p=128), [], [("w", s)], "w%d" % s)
            return dst, ("w", s)

        def norm_T(xsrc, xkey, gkey, st_i, bank, hkeys):
            i = cx.nrm % 2
            cx.nrm += 1
            xb = cx.xb[i]
            junk = cx.junk
            hT = cx.hT
            P.add("act", lambda e: e.activation(out=junk, in_=xsrc, func=AF.Square, accum_out=ss[i]),
                  reads=[xkey], writes=["junk", ("ss", i)])
            P.add("act", lambda e: e.activation(out=sd[i], in_=ss[i], func=AF.Sqrt, scale=1.0 / D, bias=eps1),
                  reads=[("ss", i), "const"], writes=[("sd", i)])
            P.add("dve", lambda e: e.reciprocal(out=rs[i], in_=sd[i]), reads=[("sd", i)], writes=[("rs", i)])
            P.add("dve", lambda e: e.tensor_scalar(out=xb, in0=xsrc, scalar1=rs[i], scalar2=None, op0=ALU.mult),
                  reads=[xkey, ("rs", i)], writes=[("xb", i)])
            pT = ps16[bank]
            for kc in range(8):
                P.add("pe", lambda e, kc=kc: e.transpose(out=pT[:, kc * 128:(kc + 1) * 128],
                                                         in_=xb[:, kc * 128:(kc + 1) * 128], identity=ident),
                      reads=[("xb", i), "const"], writes=[bk(bank)])
            g = gT[gkey]
            P.add("dve", lambda e: e.tensor_tensor(out=hT[:, :, st_i * 128:(st_i + 1) * 128],
                                                   in0=pT.rearrange("p (a b) -> p a b", b=128),
                                                   in1=g.unsqueeze(2).broadcast_to([128, 8, 128]), op=ALU.mult),
                  reads=[bk(bank), "const"], writes=hkeys)

        def fm_chunk(bank, wv, wkey, c, rhs_of_kc, nk, rkeys, ncols=T):
            for kc in range(nk):
                r_ = rhs_of_kc(kc)
                P.add("pe", lambda e, kc=kc, r_=r_: e.matmul(ps[bank][:, 0:ncols], lhsT=wv[:, kc, c * 128:(c + 1) * 128],
                                                             rhs=r_, start=(kc == 0), stop=(kc == nk - 1)),
                      reads=[wkey] + rkeys, writes=[bk(bank)])

        def tm_group(bank, wv, wkey, lhs_of_kc, nk, lkeys, ncols=512):
            for kc in range(nk):
                l_ = lhs_of_kc(kc)
                P.add("pe", lambda e, kc=kc, l_=l_: e.matmul(ps[bank][:, 0:ncols], lhsT=l_, rhs=wv[:, kc, 0:ncols],
                                                             start=(kc == 0), stop=(kc == nk - 1)),
                      reads=[wkey] + lkeys, writes=[bk(bank)])

        class RR:
            def __init__(self, ids):
                self.ids = list(ids)
                self.i = 0

            def next(self):
                b = self.ids[self.i % len(self.ids)]
                self.i += 1
                return b

        HK = [("hT", i) for i in range(NST)]

        def rotary_chunk(G, wv, wkey, c, perm, cosT, sinT, tabkey, Qb, qbkey, t1, t2, tkey, out_fn):
            b1 = G.next()
            fm_chunk(b1, wv, wkey, c, lambda kc: cx.hT[:, kc, :], 8, HK)
            P.add("act", lambda e: e.activation(out=Qb, in_=ps[b1], func=AF.Copy), reads=[bk(b1)], writes=[qbkey])
            P.add("dve", lambda e: e.tensor_tensor(out=t1, in0=ps[b1], in1=cosT, op=ALU.mult),
                  reads=[bk(b1), tabkey, qbkey], writes=[(tkey, 1)])
            b2 = G.next()
            P.add("pe", lambda e: e.matmul(ps[b2], lhsT=perm, rhs=Qb, start=True, stop=True),
                  reads=[qbkey, "const"], writes=[bk(b2)])
            P.add("dve", lambda e: e.tensor_tensor(out=t2, in0=ps[b2], in1=sinT, op=ALU.mult),
                  reads=[bk(b2), tabkey], writes=[(tkey, 2)])
            out_fn()

        mark0 = AR.off
        setup_common(3, False)
        mt = AR.alloc([2, D], F32)
        mT = AR.alloc([8, 256], BF16)
        dma("sp", mt, dr["mem"].rearrange("(a p) d -> p a d", p=128), [], ["mt"], "mt")
        G = RR([0, 1, 2, 3])
        for a in range(2):
            i = cx.nrm % 2
            cx.nrm += 1
            xb = cx.xb[i]
            src = mt[:, a, :]
            P.add("act", lambda e, src=src, i=i, junk=cx.junk: e.activation(out=junk, in_=src, func=AF.Square, accum_out=ss[i]),
                  reads=["mt"], writes=["junk", ("ss", i)])
            P.add("act", lambda e, i=i: e.activation(out=sd[i], in_=ss[i], func=AF.Sqrt, scale=1.0 / D, bias=eps1),
                  reads=[("ss", i), "const"], writes=[("sd", i)])
            P.add("dve", lambda e, i=i: e.reciprocal(out=rs[i], in_=sd[i]), reads=[("sd", i)], writes=[("rs", i)])
            P.add("dve", lambda e, src=src, i=i, xb=xb: e.tensor_scalar(out=xb, in0=src, scalar1=rs[i], scalar2=None,
                                                                      op0=ALU.mult),
                  reads=["mt", ("rs", i)], writes=[("xb", i)])
            b = G.next()
            pT = ps16[b]
            for kc in range(8):
                P.add("pe", lambda e, kc=kc, pT=pT, xb=xb: e.transpose(out=pT[:, kc * 128:(kc + 1) * 128],
                                                                    in_=xb[:, kc * 128:(kc + 1) * 128], identity=ident),
                      reads=[("xb", i), "const"], writes=[bk(b)])
            P.add("dve", lambda e, pT=pT, a=a: e.tensor_tensor(out=mT[:, :, a * 128:(a + 1) * 128],
                                                             in0=pT.rearrange("p (a b) -> p a b", b=128),
                                                             in1=gT["norm_mem"].unsqueeze(2).broadcast_to([128, 8, 128]),
                                                             op=ALU.mult),
                  reads=[bk(b), "const"], writes=["mT"])
        for g in range(2):
            wv, wk = load_w(dr["w_xkv"][:, g * 512:(g + 1) * 512], 8, 512)
            for c in range(4):
                b = G.next()
                fm_chunk(b, wv, wk, c, lambda kc: mT[:, kc, :], 8, ["mT"], ncols=256)
                P.add("act", lambda e, b=b, cc=g * 4 + c: e.activation(out=kxT[:, cc, :], in_=ps[b][:, 0:256], func=AF.Copy),
                      reads=[bk(b)], writes=["kxT"])
        for g in range(2):
            wv, wk = load_w(dr["w_xkv"][:, D + g * 512:D + (g + 1) * 512], 8, 512)
            for a in range(2):
                b = G.next()
                tm_group(b, wv, wk, lambda kc, a=a: mT[:, kc, a * 128:(a + 1) * 128], 8, ["mT"])
                P.add("act", lambda e, b=b, a=a, g=g: e.activation(out=vx[:, a, g * 512:(g + 1) * 512], in_=ps[b], func=AF.Copy),
                      reads=[bk(b)], writes=["vx"])
        P.barrier()
        AR.off = mark0

        def sweep0():
            setup_common(3, False)
            xs = [AR.alloc([D], F32) for _ in range(2)]
            Kc = AR.alloc([8, S], BF16)
            Vc = AR.alloc([32, 8, 65], BF16)
            Qa = AR.alloc([8, T], BF16)
            kmT = AR.alloc([8, 16], BF16)
            kmf = AR.alloc([8, 2], F32)
            cosA = AR.alloc([T], F32)
            sinA = AR.alloc([T], F32)
            Qb = [AR.alloc([T], BF16) for _ in range(2)]
            t1 = AR.alloc([T], F32)
            t2 = AR.alloc([T], F32)
            bsb = AR.alloc([8, 16], F32)
            top8 = AR.alloc([8, 8], F32)
            selb = AR.alloc([8, 16], F32)
            mb = AR.alloc([NST, 8, 16], BF16)
            PT = [AR.alloc([T], BF16) for _ in range(4)]
            rc = AR.alloc([T], F32)
            bcs = AR.alloc([T], F32)
            yaT = [AR.alloc([4, T], BF16) for _ in range(2)]
            for h in range(8):
                dma("sp", Kc[64:80, h, :], dr["c_onehot"], [], ["Kaux"], "const")
            P.add("pool", lambda e: e.memset(Vc[:, :, :, 64:65], 1.0), writes=["Vones"])
            P.add("pool", lambda e: e.memset(kmT[0:64], 0.0), writes=["kmT"])
            G = RR([0, 1, 2, 3])
            OB = RR([4, 5])
            pti = [0]
            for j in range(nt0):
                tok0 = j * T
                dma("sp", cosA, dr["c_cosA"][:, tok0:tok0 + T], [], ["tabA"], "tabA")
                dma("sp", sinA, dr["c_sinA"][:, tok0:tok0 + T], [], ["tabA"], "tabA")
                for s_ in range(NST):
                    xi = (j * NST + s_) % 2
                    dma("sp", xs[xi], dr["x"][tok0 + s_ * 128:tok0 + (s_ + 1) * 128, :], [], [("xs", xi)], "xs%d" % xi)
                    norm_T(xs[xi], ("xs", xi), "norm_mix", s_, G.next(), [HK[s_]])
                for which, col0 in (("q", C_AQ), ("k", C_AK)):
                    wv, wk = load_w(dr["w_in"][:, col0:col0 + 512], 8, 512)
                    for c in range(4):
                        qi = uid[0] % 2
                        uid[0] += 1

                        def comb(c=c, which=which):
                            for half in range(2):
                                h = 2 * c + half
                                if which == "q":
                                    dst = Qa[0:64, h, :]
                                    wr = [("Qa", h)]
                                else:
                                    dst = Kc[0:64, h, tok0:tok0 + T]
                                    wr = [("Kc", j, h)]
                                P.add("dve", lambda e, dst=dst, half=half: e.tensor_tensor(
                                    out=dst, in0=t1[half * 64:(half + 1) * 64, :], in1=t2[half * 64:(half + 1) * 64, :],
                                    op=ALU.add), reads=[("tA", 1), ("tA", 2)], writes=wr)

                        rotary_chunk(G, wv, wk, c, permA, cosA, sinA, "tabA", Qb[qi], ("Qb", qi), t1, t2, "tA", comb)
                wv, wk = load_w(dr["w_in"][:, C_AV:C_AV + 512], 8, 512)
                for s_ in range(NST):
                    b = G.next()
                    tm_group(b, wv, wk, lambda kc, s_=s_: cx.hT[:, kc, s_ * 128:(s_ + 1) * 128], 8, [HK[s_]])
                    kt = j * NST + s_
                    P.add("act", lambda e, b=b, kt=kt: e.activation(out=Vc[:, kt, :, 0:64],
                                                                    in_=ps[b].rearrange("p (h d) -> p h d", d=64),
                                                                    func=AF.Copy),
                          reads=[bk(b), "Vones"], writes=[("Vc", kt)])
                P.add("dve", lambda e, j=j: e.tensor_reduce(
                    out=kmf[0:64], in_=Kc[0:64, :, j * T:(j + 1) * T].rearrange("p h (n k) -> p h n k", k=256),
                    axis=AX.X, op=ALU.add), reads=[("Kc", j, h) for h in range(8)], writes=["kmf"])
                P.add("dve", lambda e, j=j: e.tensor_scalar(out=kmT[0:64, :, 2 * j:2 * j + 2], in0=kmf[0:64],
                                                            scalar1=1.0 / 256, scalar2=None, op0=ALU.mult),
                      reads=["kmf"], writes=["kmT"])
                BSB = 7
                for s_ in range(NST):
                    for h in range(8):
                        P.add("pe", lambda e, s_=s_, h=h: e.matmul(
                            ps[BSB][:, (s_ * 8 + h) * 16:(s_ * 8 + h + 1) * 16],
                            lhsT=Qa[0:64, h, s_ * 128:(s_ + 1) * 128], rhs=kmT[0:64, h, :], start=True, stop=True),
                            reads=[("Qa", h), "kmT"], writes=[bk(BSB)])
                for s_ in range(NST):
                    blk = (j * NST + s_) // 2
                    P.add("dve", lambda e, s_=s_, blk=blk: e.tensor_tensor(
                        out=bsb, in0=ps[BSB][:, s_ * 128:(s_ + 1) * 128].rearrange("p (h n) -> p h n", n=16),
                        in1=seltab[:, 0, blk, :].unsqueeze(1).broadcast_to([128, 8, 16]), op=ALU.add),
                        reads=[bk(BSB), "const"], writes=["bsb"])
                    for h in range(8):
                        P.add("dve", lambda e, h=h: e.max(out=top8[:, h, :], in_=bsb[:, h, :]),
                              reads=["bsb"], writes=["top8"])
                    P.add("dve", lambda e: e.tensor_tensor(out=selb, in0=bsb, in1=top8[:, :, 2:3].broadcast_to([128, 8, 16]),
                                                           op=ALU.is_ge), reads=["bsb", "top8"], writes=["selb"])
                    P.add("dve", lambda e, blk=blk: e.tensor_tensor(
                        out=selb, in0=selb, in1=seltab[:, 1, blk, :].unsqueeze(1).broadcast_to([128, 8, 16]), op=ALU.mult),
                        reads=["selb", "const"], writes=["selb"])
                    P.add("dve", lambda e, blk=blk: e.tensor_tensor(
                        out=selb, in0=selb, in1=seltab[:, 2, blk, :].unsqueeze(1).broadcast_to([128, 8, 16]), op=ALU.add),
                        reads=["selb", "const"], writes=["selb"])
                    P.add("dve", lambda e, s_=s_: e.tensor_scalar(out=mb[:, s_], in0=selb, scalar1=BIG, scalar2=-BIG,
                                                                  op0=ALU.mult, op1=ALU.add),
                          reads=["selb"], writes=["mb"])
                for h in range(8):
                    b = G.next()
                    for s_ in range(NST):
                        P.add("pe", lambda e, b=b, h=h, s_=s_: e.transpose(out=ps16[b][0:16, s_ * 128:(s_ + 1) * 128],
                                                                           in_=mb[:, s_, h, :], identity=ident),
                              reads=["mb", "const"], writes=[bk(b)])
                    P.add("act", lambda e, b=b, h=h: e.activation(out=Qa[64:80, h, :], in_=ps16[b][0:16, 0:T], func=AF.Copy),
                          reads=[bk(b)], writes=[("Qa", h)])
                yb_ = yaT[j % 2]
                nkt = (j + 1) * NST
                tasks = [(h, kt) for h in range(8) for kt in range(nkt)]
                LAG = 3
                obs = {}
                info = {}
                deferred = []
                BC = 6

                def emit_S(i, j=j, nkt=nkt):
                    h, kt = tasks[i]
                    if kt == 0:
                        obs[h] = OB.next()
                    r = kt - j * NST
                    c0 = 0 if r <= 0 else r * 128
                    sb_ = G.next()
                    P.add("pe", lambda e, sb_=sb_, kt=kt, c0=c0, h=h: e.matmul(
                        ps[sb_][:, c0:T], lhsT=Kc[0:80, h, kt * 128:(kt + 1) * 128], rhs=Qa[0:80, h, c0:T],
                        start=True, stop=True),
                        reads=[("Kc", kt // NST, h), "Kaux", ("Qa", h)], writes=[bk(sb_)])
                    pt = PT[pti[0] % 4]
                    ptk = ("PT", pti[0] % 4)
                    pti[0] += 1
                    P.add("act", lambda e, sb_=sb_, pt=pt, c0=c0: e.activation(out=pt[:, c0:T], in_=ps[sb_][:, c0:T],
                                                                                func=AF.Exp, scale=0.125),
                          reads=[bk(sb_)], writes=[ptk])
                    if r >= 0:
                        P.add("dve", lambda e, pt=pt, c0=c0: e.tensor_tensor(out=pt[:, c0:c0 + 128], in0=pt[:, c0:c0 + 128],
                                                                             in1=tri, op=ALU.mult),
                              reads=[ptk, "const"], writes=[ptk])
                    info[i] = (pt, ptk, c0)

                def norm_tail(h, ob, yb_=yb_, j=j):
                    P.add("pe", lambda e: e.matmul(ps[BC][0:64, :], lhsT=ones32[64:65, :], rhs=rc[64:65, :], start=True, stop=True),
                          reads=["rc", "const"], writes=[bk(BC)])
                    P.add("act", lambda e: e.activation(out=bcs[0:64, :], in_=ps[BC][0:64, :], func=AF.Copy),
                          reads=[bk(BC)], writes=["bcs"])
                    po = (h % 2) * 64
                    P.add("dve", lambda e, ob=ob, po=po, h=h, yb_=yb_: e.tensor_tensor(
                        out=yb_[po:po + 64, h // 2, :], in0=ps[ob][0:64, :], in1=bcs[0:64, :], op=ALU.mult),
                        reads=[bk(ob), "bcs"], writes=[("yaT", j % 2)])

                def emit_PV(i, nkt=nkt):
                    h, kt = tasks[i]
                    pt, ptk, c0 = info.pop(i)
                    ob = obs[h]
                    P.add("pe", lambda e, ob=ob, kt=kt, h=h, pt=pt, c0=c0, nkt=nkt: e.matmul(
                        ps[ob][0:65, c0:T], lhsT=Vc[:, kt, h, :], rhs=pt[:, c0:T],
                        start=(kt == 0), stop=(kt == nkt - 1), skip_group_check=True),
                        reads=[("Vc", kt), "Vones", ptk], writes=[bk(ob)])
                    if kt == nkt - 1:
                        P.add("dve", lambda e, ob=ob: e.reciprocal(out=rc[64:65, :], in_=ps[ob][64:65, :]),
                              reads=[bk(ob)], writes=["rc"])
                        deferred.append((i + LAG + 2, h, ob))

                ntask = len(tasks)
                for i in range(ntask + LAG + 3):
                    if i < ntask:
                        emit_S(i)
                    if 0 <= i - LAG < ntask:
                        emit_PV(i - LAG)
                    while deferred and deferred[0][0] <= i:
                        _, h_, ob_ = deferred.pop(0)
                        norm_tail(h_, ob_)
                assert not deferred and not info
                dma("sp", ya_s[:, tok0:tok0 + T].rearrange("(c p) t -> p c t", p=128), yb_,
                    [("yaT", j % 2)], [("yas", j)], "yaT%d" % (j % 2))
            P.barrier()
            AR.off = mark0

        if nt0 > 0:
            sweep0()

        def sweep1():
            setup_common(6, True)
            cosB = AR.alloc([T], F32)
            sinB = AR.alloc([T], F32)
            Qb = [AR.alloc([T], BF16) for _ in range(2)]
            t1 = AR.alloc([T], F32)
            t2 = AR.alloc([T], F32)
            qT = AR.alloc([4, T], BF16)
            kT = AR.alloc([4, T], BF16)
            ktok = AR.alloc([NST, 4, 128], BF16)
            vr = AR.alloc([NST, D], BF16)
            sil = AR.alloc([NST, D], BF16)
            Sst = AR.alloc([4, 256], F32)
            Sbf = AR.alloc([4, 256], BF16)
            attT = [AR.alloc([128], BF16) for _ in range(4)]
            st6 = AR.alloc([4, 6], F32)
            mv4 = AR.alloc([4, 2], F32)
            ve4 = AR.alloc([4], F32)
            rstd4 = AR.alloc([4], F32)
            ytmp = [AR.alloc([256], F32) for _ in range(2)]
            sgaA = AR.alloc([8, T], BF16)
            sgbA = AR.alloc([8, T], BF16)
            ybt = [AR.alloc([D], BF16) for _ in range(2)]
            ybT = AR.alloc([8, T], BF16)
            yaT1 = AR.alloc([4, T], BF16)
            mtmp = [AR.alloc([T], F32) for _ in range(2)]
            mrg = AR.alloc([8, T], BF16)
            P.add("pool", lambda e: e.memset(Sst, 0.0), writes=["S"])
            P.add("pool", lambda e: e.memset(Sbf, 0.0), writes=["Sbf"])
            G = RR([0, 1, 2, 3, 4, 5, 6, 7])
            ai = [0]
            for j in range(nt1):
                tok0 = j * T
                dma("sp", cosB, dr["c_cosB"][:, tok0:tok0 + T], [], ["tabB"], "tabB")
                dma("sp", sinB, dr["c_sinB"][:, tok0:tok0 + T], [], ["tabB"], "tabB")
                def xload1(jn):
                    bi = jn % 2
                    dma("sp", cx.xts[bi], dr["x"][jn * T:(jn + 1) * T, :].rearrange("(a p) d -> p a d", p=128), [],
                        [("xt", bi, s_) for s_ in range(NST)], "xt%d" % bi)

                if j == 0:
                    xload1(0)
                if j + 1 < nt1:
                    xload1(j + 1)
                xt = cx.xts[j % 2]
                XK = [("xt", j % 2, s_) for s_ in range(NST)]
                dma("sp", yaT1, ya_s[:, tok0:tok0 + T].rearrange("(c p) t -> p c t", p=128), [("yas", j)], ["yaT1"], "yaL")
                for s_ in range(NST):
                    norm_T(xt[:, s_, :], XK[s_], "norm_mix", s_, G.next(), [HK[s_]])
                for which, col0, dstT in (("q", C_RQ, qT), ("k", C_RK, kT)):
                    wv, wk = load_w(dr["w_in"][:, col0:col0 + 512], 8, 512)
                    for c in range(4):
                        qi = uid[0] % 2
                        uid[0] += 1

                        def comb(c=c, dstT=dstT, which=which):
                            P.add("dve", lambda e: e.tensor_tensor(out=dstT[:, c, :], in0=t1, in1=t2, op=ALU.add),
                                  reads=[("tB", 1), ("tB", 2)], writes=[(which + "T", c)])

                        rotary_chunk(G, wv, wk, c, permB, cosB, sinB, "tabB", Qb[qi], ("Qb", qi), t1, t2, "tB", comb)
                for g in range(2):
                    wv, wk = load_w(dr["w_in"][:, C_RV + g * 512:C_RV + (g + 1) * 512], 8, 512)
                    for s_ in range(NST):
                        b = G.next()
                        tm_group(b, wv, wk, lambda kc, s_=s_: cx.hT[:, kc, s_ * 128:(s_ + 1) * 128], 8, [HK[s_]])
                        P.add("act", lambda e, b=b, s_=s_, g=g: e.activation(out=vr[:, s_, g * 512:(g + 1) * 512], in_=ps[b],
                                                                             func=AF.Copy),
                              reads=[bk(b)], writes=[("vr", s_)])
                for g in range(2):
                    wv, wk = load_w(dr["w_in"][:, C_RG + g * 512:C_RG + (g + 1) * 512], 8, 512)
                    for s_ in range(NST):
                        b = G.next()
                        tm_group(b, wv, wk, lambda kc, s_=s_: cx.hT[:, kc, s_ * 128:(s_ + 1) * 128], 8, [HK[s_]])
                        P.add("act", lambda e, b=b, s_=s_, g=g: e.activation(out=sil[:, s_, g * 512:(g + 1) * 512], in_=ps[b],
                                                                             func=AF.Silu),
                              reads=[bk(b)], writes=[("sil", s_)])
                def gate_group(which, g):
                    col0 = (C_GA if which == "a" else C_GB) + g * 512
                    wv_, wk_ = load_w(dr["w_in"][:, col0:col0 + 512], 8, 512)
                    dstA = sgaA if which == "a" else sgbA
                    for c in range(4):
                        m = g * 4 + c
                        b = G.next()
                        fm_chunk(b, wv_, wk_, c, lambda kc: cx.hT[:, kc, :], 8, HK)
                        P.add("act", lambda e, b=b, m=m, dstA=dstA: e.activation(out=dstA[:, m, :], in_=ps[b], func=AF.Sigmoid),
                              reads=[bk(b)], writes=[("sg" + which, m)])

                gate_plan = [("a", 0), ("a", 1), ("b", 0), ("b", 1)]
                for s_ in range(NST):
                    cs = slice(s_ * 128, (s_ + 1) * 128)
                    b = G.next()
                    for h in range(4):
                        P.add("pe", lambda e, b=b, h=h, cs=cs: e.transpose(out=ps16[b][:, h * 128:(h + 1) * 128],
                                                                           in_=kT[:, h, cs], identity=ident),
                              reads=[("kT", h), "const"], writes=[bk(b)])
                    P.add("dve", lambda e, b=b, s_=s_: e.tensor_tensor(
                        out=ktok[:, s_], in0=ps16[b][:, 0:512].rearrange("p (h d) -> p h d", d=128),
                        in1=kdec.unsqueeze(2).broadcast_to([128, 4, 128]), op=ALU.mult),
                        reads=[bk(b), "const"], writes=[("ktok", s_)])
                    yb_ = ybt[s_ % 2]
                    ybk = ("ybt", s_ % 2)
                    bas = []
                    for h in range(4):
                        ba = G.next()
                        bas.append(ba)
                        P.add("pe", lambda e, ba=ba, h=h, cs=cs: e.matmul(ps[ba][:, 0:128], lhsT=kT[:, h, cs], rhs=qT[:, h, cs],
                                                                          start=True, stop=True),
                              reads=[("kT", h), ("qT", h)], writes=[bk(ba)])
                    ats = []
                    for h in range(4):
                        at = attT[h]
                        atk = ("attT", h)
                        ats.append((at, atk))
                        P.add("dve", lambda e, ba=bas[h], at=at, h=h: e.tensor_tensor(out=at, in0=ps[ba][:, 0:128], in1=M2[:, h, :],
                                                                                      op=ALU.mult),
                              reads=[bk(bas[h]), "const"], writes=[atk])
                    bys = []
                    for h in range(4):
                        hs = slice(h * 256, (h + 1) * 256)
                        at, atk = ats[h]
                        by = G.next()
                        bys.append(by)
                        P.add("pe", lambda e, by=by, at=at, s_=s_, hs=hs: e.matmul(ps[by][:, 0:256], lhsT=at, rhs=vr[:, s_, hs],
                                                                                   start=True, stop=False),
                              reads=[atk, ("vr", s_)], writes=[bk(by)])
                        P.add("pe", lambda e, by=by, h=h, cs=cs: e.matmul(ps[by][:, 0:256], lhsT=qT[:, h, cs], rhs=Sbf[:, h, :],
                                                                          start=False, stop=True),
                              reads=[("qT", h), ("Sbf", h)], writes=[bk(by)])
                        P.add("pe", lambda e, by=by, h=h, s_=s_, hs=hs: e.matmul(ps[by][:, 256:512], lhsT=ktok[:, s_, h, :],
                                                                                 rhs=vr[:, s_, hs], start=True, stop=True),
                              reads=[("ktok", s_), ("vr", s_)], writes=[bk(by)])
                    for h in range(4):
                        by = bys[h]
                        P.add("dve", lambda e, by=by, h=h: e.scalar_tensor_tensor(
                            out=Sst[:, h, :], in0=Sst[:, h, :], scalar=gamC[h], in1=ps[by][:, 256:512], op0=ALU.mult, op1=ALU.add),
                            reads=[bk(by), ("S", h)], writes=[("S", h)])
                        P.add("act", lambda e, h=h: e.activation(out=Sbf[:, h, :], in_=Sst[:, h, :], func=AF.Copy),
                              reads=[("S", h)], writes=[("Sbf", h)])
                    gate_group(*gate_plan[s_])
                    for h in range(4):
                        by = bys[h]
                        P.add("dve", lambda e, by=by, h=h: e.bn_stats(out=st6[:, h, :], in_=ps[by][:, 0:256]),
                              reads=[bk(by)], writes=[("st6", h)])
                        P.add("dve", lambda e, h=h: e.bn_aggr(out=mv4[:, h, :], in_=st6[:, h, :]), reads=[("st6", h)], writes=["mv4"])
                    P.add("dve", lambda e: e.tensor_tensor(out=ve4, in0=mv4[:, :, 1], in1=epsq, op=ALU.add),
                          reads=["mv4", "const"], writes=["ve4"])
                    P.add("act", lambda e: e.activation(out=rstd4, in_=ve4, func=AF.Sqrt), reads=["ve4"], writes=["rstd4"])
                    P.add("dve", lambda e: e.reciprocal(out=rstd4, in_=rstd4), reads=["rstd4"], writes=["rstd4"])
                    for h in range(4):
                        by = bys[h]
                        hs = slice(h * 256, (h + 1) * 256)
                        yt = ytmp[h % 2]
                        ytk = ("ytmp", h % 2)
                        P.add("dve", lambda e, by=by, s_=s_, hs=hs, h=h, yt=yt: e.scalar_tensor_tensor(
                            out=yt, in0=ps[by][:, 0:256], scalar=mv4[:, h, 0:1], in1=sil[:, s_, hs], op0=ALU.subtract, op1=ALU.mult),
                            reads=[bk(by), "mv4", ("sil", s_)], writes=[ytk])
                        P.add("dve", lambda e, yb_=yb_, hs=hs, h=h, yt=yt: e.tensor_scalar(out=yb_[:, hs], in0=yt, scalar1=rstd4[:, h:h + 1],
                                                                                         scalar2=None, op0=ALU.mult),
                              reads=[ytk, "rstd4"], writes=[ybk])
                    b = G.next()
                    for kc in range(8):
                        P.add("pe", lambda e, b=b, kc=kc, yb_=yb_: e.transpose(out=ps16[b][:, kc * 128:(kc + 1) * 128],
                                                                               in_=yb_[:, kc * 128:(kc + 1) * 128], identity=ident),
                              reads=[ybk, "const"], writes=[bk(b)])
                    P.add("act", lambda e, b=b, cs=cs: e.activation(out=ybT[:, :, cs], in_=ps16[b].rearrange("p (a b) -> p a b", b=128),
                                                                    func=AF.Copy),
                          reads=[bk(b)], writes=[("ybT", s_)])
                YBK = [("ybT", s_) for s_ in range(NST)]
                for g in range(2):
                    wA, kA = load_w(dr["w_branch_a"][:, g * 512:(g + 1) * 512], 4, 512)
                    wB, kB = load_w(dr["w_branch_b"][:, g * 512:(g + 1) * 512], 8, 512)
                    for c in range(4):
                        m = g * 4 + c
                        i2 = m % 2
                        b = G.next()
                        fm_chunk(b, wA, kA, c, lambda kc: yaT1[:, kc, :], 4, ["yaT1"])
                        P.add("dve", lambda e, b=b, i2=i2, m=m: e.tensor_tensor(out=mtmp[i2], in0=ps[b], in1=sgaA[:, m, :], op=ALU.mult),
                              reads=[bk(b), ("sga", m)], writes=[("mtmp", i2)])
                        b = G.next()
                        fm_chunk(b, wB, kB, c, lambda kc: ybT[:, kc, :], 8, YBK)
                        P.add("dve", lambda e, b=b, m=m: e.tensor_tensor(out=sgbA[:, m, :], in0=ps[b], in1=sgbA[:, m, :], op=ALU.mult),
                              reads=[bk(b), ("sgb", m)], writes=[("sgb", m)])
                        P.add("dve", lambda e, m=m, i2=i2: e.tensor_tensor(out=mrg[:, m, :], in0=mtmp[i2], in1=sgbA[:, m, :], op=ALU.add),
                              reads=[("mtmp", i2), ("sgb", m)], writes=[("mrg", m)])
                MK = [("mrg", mm) for mm in range(8)]
                for g in range(2):
                    wv, wk = load_w(dr["w_out"][:, g * 512:(g + 1) * 512], 8, 512)
                    for s_ in range(NST):
                        b = G.next()
                        tm_group(b, wv, wk, lambda kc, s_=s_: mrg[:, kc, s_ * 128:(s_ + 1) * 128], 8, MK)
                        P.add("dve", lambda e, b=b, s_=s_, g=g, xt=xt: e.tensor_tensor(out=xt[:, s_, g * 512:(g + 1) * 512],
                                                                                       in0=ps[b], in1=xt[:, s_, g * 512:(g + 1) * 512], op=ALU.add),
                              reads=[bk(b), XK[s_]], writes=[XK[s_]])
                dma("sp", x1_s[tok0:tok0 + T, :].rearrange("(a p) d -> p a d", p=128), xt,
                    list(XK), [("x1s", j)], "xto%d" % (j % 2))
            P.barrier()
            AR.off = mark0

        if nt1 > 0:
            sweep1()

        def sweep2():
            setup_common(6, True)
            qxT = AR.alloc([8, T], BF16)
            PTx = [AR.alloc([T], BF16) for _ in range(4)]
            rcs = [AR.alloc([T], F32) for _ in range(2)]
            oT = AR.alloc([8, T], BF16)
            sgt = [AR.alloc([T], BF16) for _ in range(3)]
            aT = AR.alloc([NFC, T], BF16)
            ot = [AR.alloc([D], F32) for _ in range(2)]
            G = RR([0, 1, 2, 3])
            pi = [0]
            gi = [0]
            for j in range(nt2):
                tok0 = j * T
                src = x1_s if nt1 > 0 else dr["x"]

                def xload2(jn):
                    bi = jn % 2
                    dma("sp", cx.xts[bi], src[jn * T:(jn + 1) * T, :].rearrange("(a p) d -> p a d", p=128), [("x1s", jn)],
                        [("xt", bi, s_) for s_ in range(NST)], "xt%d" % bi)

                if j == 0:
                    xload2(0)
                if j + 1 < nt2:
                    xload2(j + 1)
                xt = cx.xts[j % 2]
                XK = [("xt", j % 2, s_) for s_ in range(NST)]
                for s_ in range(NST):
                    norm_T(xt[:, s_, :], XK[s_], "norm_cross", s_, G.next(), [HK[s_]])
                for g in range(2):
                    wv, wk = load_w(dr["w_xq"][:, g * 512:(g + 1) * 512], 8, 512)
                    for c in range(4):
                        b = G.next()
                        m = g * 4 + c
                        fm_chunk(b, wv, wk, c, lambda kc: cx.hT[:, kc, :], 8, HK)
                        P.add("act", lambda e, b=b, m=m: e.activation(out=qxT[:, m, :], in_=ps[b], func=AF.Copy),
                              reads=[bk(b)], writes=[("qxT", m)])
                for h in range(4):
                    pts = []
                    for mc in range(2):
                        b = G.next()
                        for dc in range(2):
                            P.add("pe", lambda e, b=b, h=h, mc=mc, dc=dc: e.matmul(
                                ps[b], lhsT=kxT[:, 2 * h + dc, mc * 128:(mc + 1) * 128], rhs=qxT[:, 2 * h + dc, :],
                                start=(dc == 0), stop=(dc == 1)),
                                reads=["kxT", ("qxT", 2 * h + dc)], writes=[bk(b)])
                        pt = PTx[pi[0] % 4]
                        ptk = ("PTx", pi[0] % 4)
                        pi[0] += 1
                        P.add("act", lambda e, b=b, pt=pt: e.activation(out=pt, in_=ps[b], func=AF.Exp, scale=1.0 / 16),
                              reads=[bk(b)], writes=[ptk])
                        pts.append((pt, ptk))
                    b = G.next()
                    for mc in range(2):
                        P.add("pe", lambda e, b=b, mc=mc, pt=pts[mc][0]: e.matmul(ps[b], lhsT=onesb, rhs=pt, start=(mc == 0), stop=(mc == 1)),
                              reads=[pts[mc][1], "const"], writes=[bk(b)])
                    rcv = rcs[h % 2]
                    P.add("dve", lambda e, b=b, rcv=rcv: e.reciprocal(out=rcv, in_=ps[b]), reads=[bk(b)], writes=[("rcs", h % 2)])
                    for ec in range(2):
                        b = G.next()
                        for mc in range(2):
                            P.add("pe", lambda e, b=b, h=h, ec=ec, mc=mc, pt=pts[mc][0]: e.matmul(
                                ps[b], lhsT=vx[:, mc, h * 256 + ec * 128:h * 256 + (ec + 1) * 128], rhs=pt,
                                start=(mc == 0), stop=(mc == 1)),
                                reads=[pts[mc][1], "vx"], writes=[bk(b)])
                        mo = 2 * h + ec
                        P.add("dve", lambda e, b=b, rcv=rcv, mo=mo: e.tensor_tensor(out=oT[:, mo, :], in0=ps[b], in1=rcv, op=ALU.mult),
                              reads=[bk(b), ("rcs", h % 2)], writes=[("oT", mo)])
                OK_ = [("oT", mm) for mm in range(8)]
                for g in range(2):
                    wv, wk = load_w(dr["w_xo"][:, g * 512:(g + 1) * 512], 8, 512)
                    for s_ in range(NST):
                        b = G.next()
                        tm_group(b, wv, wk, lambda kc, s_=s_: oT[:, kc, s_ * 128:(s_ + 1) * 128], 8, OK_)
                        P.add("dve", lambda e, b=b, s_=s_, g=g, xt=xt: e.tensor_tensor(out=xt[:, s_, g * 512:(g + 1) * 512],
                                                                                       in0=ps[b], in1=xt[:, s_, g * 512:(g + 1) * 512], op=ALU.add),
                              reads=[bk(b), XK[s_]], writes=[XK[s_]])
                for s_ in range(NST):
                    norm_T(xt[:, s_, :], XK[s_], "norm_ffn", s_, G.next(), [HK[s_]])
                for fg in range(6):
                    ncol = 512 if fg < 5 else 256
                    wg, kg = load_w(dr["w_gate"][:, fg * 512:fg * 512 + ncol], 8, ncol)
                    wu, ku = load_w(dr["w_up"][:, fg * 512:fg * 512 + ncol], 8, ncol)
                    for c in range(ncol // 128):
                        fc = fg * 4 + c
                        bg = G.next()
                        fm_chunk(bg, wg, kg, c, lambda kc: cx.hT[:, kc, :], 8, HK)
                        sg_ = sgt[gi[0] % 3]
                        sgk = ("sgt", gi[0] % 3)
                        gi[0] += 1
                        P.add("act", lambda e, bg=bg, sg_=sg_: e.activation(out=sg_, in_=ps[bg], func=AF.Silu),
                              reads=[bk(bg)], writes=[sgk])
                        bu = G.next()
                        fm_chunk(bu, wu, ku, c, lambda kc: cx.hT[:, kc, :], 8, HK)
                        P.add("dve", lambda e, bu=bu, sg_=sg_, fc=fc: e.tensor_tensor(out=aT[:, fc, :], in0=ps[bu], in1=sg_, op=ALU.mult),
                              reads=[bk(bu), sgk], writes=[("aT", fc)])
                pieces = [(0, 8), (8, 8), (16, 6)]
                for g in range(2):
                    for (f0, nf) in pieces:
                        wv, wk = load_w(dr["w_down"][f0 * 128:(f0 + nf) * 128, g * 512:(g + 1) * 512], nf, 512)
                        for s_ in range(NST):
                            for fl in range(nf):
                                fc = f0 + fl
                                P.add("pe", lambda e, s_=s_, fl=fl, fc=fc, wv=wv: e.matmul(
                                    ps[4 + s_], lhsT=aT[:, fc, s_ * 128:(s_ + 1) * 128], rhs=wv[:, fl, :],
                                    start=(fc == 0), stop=(fc == NFC - 1), skip_group_check=True),
                                    reads=[wk, ("aT", fc)], writes=[bk(4 + s_)])
                    for s_ in range(NST):
                        P.add("dve", lambda e, s_=s_, g=g, xt=xt: e.tensor_tensor(out=xt[:, s_, g * 512:(g + 1) * 512], in0=ps[4 + s_],
                                                                                  in1=xt[:, s_, g * 512:(g + 1) * 512], op=ALU.add),
                              reads=[bk(4 + s_), XK[s_]], writes=[XK[s_]])
                for s_ in range(NST):
                    i = cx.nrm % 2
                    cx.nrm += 1
                    o_ = ot[s_ % 2]
                    ok_ = ("ot", s_ % 2)
                    src = xt[:, s_, :]
                    P.add("act", lambda e, src=src, i=i, junk=cx.junk: e.activation(out=junk, in_=src, func=AF.Square, accum_out=ss[i]),
                          reads=[XK[s_]], writes=["junk", ("ss", i)])
                    P.add("act", lambda e, i=i: e.activation(out=sd[i], in_=ss[i], func=AF.Sqrt, scale=1.0 / D, bias=eps1),
                          reads=[("ss", i), "const"], writes=[("sd", i)])
                    P.add("dve", lambda e, i=i: e.reciprocal(out=rs[i], in_=sd[i]), reads=[("sd", i)], writes=[("rs", i)])
                    P.add("dve", lambda e, src=src, i=i, o_=o_: e.scalar_tensor_tensor(out=o_, in0=src, scalar=rs[i], in1=gfin,
                                                                                     op0=ALU.mult, op1=ALU.mult),
                          reads=[XK[s_], ("rs", i), "const"], writes=[ok_])
                    dma("sp", out_d[tok0 + s_ * 128:tok0 + (s_ + 1) * 128, :], o_, [ok_], [], "ot%d" % (s_ % 2))
        if nt2 > 0:
            sweep2()
        P.emit()
        nc._n_ops = P.nops
    return nc


_NC_CACHE = {}


def kernel(**inputs):
    consts, _ = make_consts()
    if "nc" not in _NC_CACHE:
        _NC_CACHE["nc"] = build()
    nc = _NC_CACHE["nc"]
    x = np.ascontiguousarray(np.asarray(inputs["x"], dtype=np.float32))
    mem = np.ascontiguousarray(np.asarray(inputs["mem"], dtype=np.float32))
    shared = {}
    for name, shp in W_SPECS:
        shared[name] = np.ascontiguousarray(np.asarray(inputs[name], dtype=np.float32).reshape(shp))
    shared.update(consts)
    in_maps = []
    for b in range(8):
        m = dict(shared)
        m["x"] = x[b]
        m["mem"] = mem[b]
        in_maps.append(m)
    res = run_bass_kernel_spmd(nc, in_maps, core_ids=list(range(8)))
    return np.stack([np.asarray(r["out"], dtype=np.float32) for r in res.results], axis=0)
```
